# Optimizing a Trainium2 kernel written in Bass

```python
import math
import jax
import jax.numpy as jnp
from jax import lax
import numpy as np

D_MODEL = 1024
BATCH = 8
SEQ = 4096
DEPTH = 2

HEAD_DIM = 64
N_HEADS_A = 8
N_KV_A = 2
HPG_A = N_HEADS_A // N_KV_A
N_HEADS_B = 8
N_HEADS = N_HEADS_A + N_HEADS_B
WIDTH_A = N_HEADS_A * HEAD_DIM
WIDTH_B = N_HEADS_B * HEAD_DIM
MIX_WIDTH = WIDTH_A + WIDTH_B
KV_A = N_KV_A * HEAD_DIM
IN_SPLITS = (WIDTH_A, KV_A, KV_A, KV_A, KV_A, KV_A, KV_A, N_HEADS_A * 3, WIDTH_B, WIDTH_B, WIDTH_B)
IN_WIDTH = sum(IN_SPLITS)

CMP_LEN = 32
CMP_STRIDE = 16
CMP_HIDDEN = 256
SEL_BLOCK = 64
SEL_TOPK = 16
WIN_A = 512
NSA_QBLOCK = 64

DIL_PAIRS = ((128, 1), (512, 4), (2048, 16))

N_BUCKETS = 32
MAX_DISTANCE = 2048

D_FF = 2816
N_EXPERTS = 8
TOP_K = 2
D_FF_EXPERT = 3584
MOE_CHUNK = 256

RMS_EPS = 1e-6
NEG = -1e30
SCALE = HEAD_DIM ** -0.5

kernel_name = 'hymba_nsa_dilated_moe_trunk'


def rmsnorm(x, g):
    xf = x.astype(jnp.float32)
    y = xf * lax.rsqrt(jnp.mean(xf * xf, axis=-1, keepdims=True) + RMS_EPS)
    return (y * g.astype(jnp.float32)).astype(x.dtype)


def t5_bucket(dist):
    dist = jnp.maximum(dist, 0)
    max_exact = N_BUCKETS // 2
    scaled = jnp.log(jnp.maximum(dist, 1).astype(jnp.float32) / max_exact) / math.log(MAX_DISTANCE / max_exact)
    large = jnp.minimum(max_exact + (scaled * (N_BUCKETS - max_exact)).astype(jnp.int32), N_BUCKETS - 1)
    return jnp.where(dist < max_exact, dist, large)


def masked_softmax(s, mask):
    p = jax.nn.softmax(jnp.where(mask, s, NEG), axis=-1)
    return jnp.where(mask, p, 0.0)


def swiglu(h, w_gate, w_up, w_down):
    return (jax.nn.silu(h @ w_gate) * (h @ w_up)) @ w_down


def compress_tokens(kv, pos_emb, w1, b1, w2):
    B, S, G, dh = kv.shape
    n_cmp = (S - CMP_LEN) // CMP_STRIDE + 1
    idx = jnp.arange(n_cmp)[:, None] * CMP_STRIDE + jnp.arange(CMP_LEN)[None, :]
    blocks = kv[:, idx] + pos_emb[:, None, :]
    flat = blocks.transpose(0, 3, 1, 2, 4).reshape(B, G, n_cmp, CMP_LEN * dh)
    return jax.nn.gelu(flat @ w1 + b1) @ w2


def nsa_mixer(q, k_c, v_c, k_s, v_s, k_w, v_w, gate_logits, q_norm, k_norm,
              cmp_pos, cmp_w1, cmp_b1, cmp_w2, tbl):
    B, S = q.shape[0], q.shape[1]
    G, H, dh, QA = N_KV_A, HPG_A, HEAD_DIM, NSA_QBLOCK
    qh = rmsnorm(q, q_norm).reshape(B, S, G, H, dh).transpose(0, 2, 3, 1, 4)
    kc = rmsnorm(compress_tokens(k_c, cmp_pos[0], cmp_w1[0], cmp_b1[0], cmp_w2[0]), k_norm[0])
    vc = compress_tokens(v_c, cmp_pos[1], cmp_w1[1], cmp_b1[1], cmp_w2[1])
    n_cmp = kc.shape[2]
    n_sb = S // SEL_BLOCK
    ks = rmsnorm(k_s, k_norm[1]).reshape(B, n_sb, SEL_BLOCK, G, dh).transpose(0, 3, 1, 2, 4)
    vs = v_s.reshape(B, n_sb, SEL_BLOCK, G, dh).transpose(0, 3, 1, 2, 4)
    pad = ((0, 0), (0, 0), (WIN_A, 0), (0, 0))
    kw = jnp.pad(rmsnorm(k_w, k_norm[2]).transpose(0, 2, 1, 3), pad)
    vw = jnp.pad(v_w.transpose(0, 2, 1, 3), pad)
    gates = jax.nn.sigmoid(gate_logits.astype(jnp.float32)).reshape(B, S, G, H, 3).transpose(0, 2, 3, 1, 4)
    tbl_g = tbl.reshape(G, H, N_BUCKETS)

    c_start = jnp.arange(n_cmp)[:, None] * CMP_STRIDE
    s_start = jnp.arange(n_sb)[None, :] * SEL_BLOCK
    overlap = jnp.clip(jnp.minimum(c_start + CMP_LEN, s_start + SEL_BLOCK) - jnp.maximum(c_start, s_start), 0, None)
    overlap = overlap.astype(jnp.float32) / CMP_LEN
    c_end = jnp.arange(n_cmp) * CMP_STRIDE + CMP_LEN - 1
    blk_j = jnp.arange(n_sb)
    k_sel = min(SEL_TOPK, n_sb)
    b_ix = jnp.arange(B)[:, None, None, None]
    g_ix = jnp.arange(G)[None, :, None, None]
    g5 = jnp.arange(G)[None, :, None, None, None]
    h5 = jnp.arange(H)[None, None, :, None, None]

    def block(i):
        t0 = i * QA
        t = t0 + jnp.arange(QA)
        qi = lax.dynamic_slice_in_dim(qh, t0, QA, axis=3)
        dist_c = t[:, None] - c_end[None, :]
        bias_c = tbl[:, t5_bucket(dist_c)].reshape(G, H, QA, n_cmp)
        s_c = jnp.einsum('bghqd,bgnd->bghqn', qi, kc).astype(jnp.float32) * SCALE + bias_c
        p_c = masked_softmax(s_c, dist_c >= 0)
        o_c = jnp.einsum('bghqn,bgnd->bghqd', p_c, vc)
        imp = jnp.einsum('bghqn,nj->bgqj', p_c, overlap)
        cur = t // SEL_BLOCK
        forced = (blk_j[None, :] == 0) | (blk_j[None, :] == cur[:, None]) | (blk_j[None, :] == cur[:, None] - 1)
        future = blk_j[None, :] * SEL_BLOCK > t[:, None]
        imp = jnp.where(forced, 1e6, jnp.where(future, -1e6, imp))
        _, sel = lax.top_k(imp, k_sel)
        k_g = ks[b_ix, g_ix, sel].reshape(B, G, QA, k_sel * SEL_BLOCK, dh)
        v_g = vs[b_ix, g_ix, sel].reshape(B, G, QA, k_sel * SEL_BLOCK, dh)
        pos = (sel[..., None] * SEL_BLOCK + jnp.arange(SEL_BLOCK)).reshape(B, G, QA, k_sel * SEL_BLOCK)
        dist_s = t[:, None] - pos
        bias_s = tbl_g[g5, h5, t5_bucket(dist_s)[:, :, None]]
        s_s = jnp.einsum('bghqd,bgqnd->bghqn', qi, k_g).astype(jnp.float32) * SCALE + bias_s
        p_s = masked_softmax(s_s, (dist_s >= 0)[:, :, None])
        o_s = jnp.einsum('bghqn,bgqnd->bghqd', p_s, v_g)
        k_wi = lax.dynamic_slice_in_dim(kw, t0, WIN_A + QA, axis=2)
        v_wi = lax.dynamic_slice_in_dim(vw, t0, WIN_A + QA, axis=2)
        pos_w = t0 - WIN_A + jnp.arange(WIN_A + QA)
        dist_w = t[:, None] - pos_w[None, :]
        mask_w = (dist_w >= 0) & (dist_w < WIN_A) & (pos_w[None, :] >= 0)
        bias_w = tbl[:, t5_bucket(dist_w)].reshape(G, H, QA, WIN_A + QA)
        s_w = jnp.einsum('bghqd,bgkd->bghqk', qi, k_wi).astype(jnp.float32) * SCALE + bias_w
        p_w = masked_softmax(s_w, mask_w)
        o_w = jnp.einsum('bghqk,bgkd->bghqd', p_w, v_wi)
        g = lax.dynamic_slice_in_dim(gates, t0, QA, axis=3)
        return g[..., 0:1] * o_c + g[..., 1:2] * o_s + g[..., 2:3] * o_w

    out = lax.map(block, jnp.arange(S // QA))
    return out.transpose(1, 0, 4, 2, 3, 5).reshape(B, S, WIDTH_A).astype(q.dtype)


def dilated_branch(q, k, v, tbl, window, dil):
    B, S, H, dh = q.shape
    L = S // dil
    band = window // dil
    nb = -(-L // band)
    Lp = nb * band

    def to_blocks(a):
        a = a.reshape(B, L, dil, H, dh).transpose(0, 2, 3, 1, 4)
        a = jnp.pad(a, ((0, 0), (0, 0), (0, 0), (0, Lp - L), (0, 0)))
        return a.reshape(B, dil, H, nb, band, dh)

    def with_prev(a):
        prev = jnp.pad(a[:, :, :, :-1], ((0, 0), (0, 0), (0, 0), (1, 0), (0, 0), (0, 0)))
        return jnp.concatenate([prev, a], axis=4)

    qb = to_blocks(q)
    kk = with_prev(to_blocks(k))
    vv = with_prev(to_blocks(v))
    a_ix = jnp.arange(band)[:, None]
    c_ix = jnp.arange(2 * band)[None, :]
    n = band + a_ix - c_ix
    key_idx = (jnp.arange(nb)[:, None, None] - 1) * band + c_ix[None]
    mask = (n >= 0) & (n <= band) & (key_idx >= 0)
    bias = tbl[:, t5_bucket(n * dil)]
    s = jnp.einsum('brhnqd,brhnkd->brhnqk', qb, kk).astype(jnp.float32) * SCALE + bias[:, None]
    s = jnp.where(mask, s, NEG)
    m = jnp.max(s, axis=-1, keepdims=True)
    e = jnp.exp(s - m)
    den = jnp.sum(e, axis=-1)
    o = jnp.einsum('brhnqk,brhnkd->brhnqd', e, vv) / den[..., None]
    lse = m[..., 0] + jnp.log(den)
    o = o.reshape(B, dil, H, Lp, dh)[:, :, :, :L].transpose(0, 3, 1, 2, 4).reshape(B, S, H, dh)
    lse = lse.reshape(B, dil, H, Lp)[..., :L].transpose(0, 3, 1, 2).reshape(B, S, H)
    return o, lse


def dilated_mixer(q, k, v, q_norm, k_norm, tbl):
    B, S = q.shape[0], q.shape[1]
    qn = rmsnorm(q, q_norm)
    kn = rmsnorm(k, k_norm)
    outs, lses = [], []
    for window, dil in DIL_PAIRS:
        o, lse = dilated_branch(qn, kn, v, tbl, window, dil)
        outs.append(o)
        lses.append(lse)
    w = jax.nn.softmax(jnp.stack(lses), axis=0)
    o = jnp.einsum('ibsh,ibshd->bshd', w, jnp.stack(outs))
    return o.reshape(B, S, WIDTH_B).astype(q.dtype)


def moe_swiglu(h, router_w, w_gate, w_up, w_down):
    B, S, D = h.shape
    hf = h.reshape(-1, D)
    N = hf.shape[0]
    NK = N * TOP_K
    logits = (hf @ router_w).astype(jnp.float32)
    top_logit, top_e = lax.top_k(logits, TOP_K)
    gate = jax.nn.softmax(top_logit, axis=-1)
    e_flat = top_e.reshape(-1)
    tok_flat = jnp.arange(NK) // TOP_K
    order = jnp.argsort(e_flat)
    e_sorted = e_flat[order]
    counts = jnp.bincount(e_flat, length=N_EXPERTS)
    padded = (counts + MOE_CHUNK - 1) // MOE_CHUNK * MOE_CHUNK
    start = jnp.cumsum(counts) - counts
    pend = jnp.cumsum(padded)
    pstart = pend - padded
    dest = pstart[e_sorted] + jnp.arange(NK) - start[e_sorted]
    n_chunks = -(-NK // MOE_CHUNK) + N_EXPERTS
    rows = n_chunks * MOE_CHUNK
    row_tok = jnp.zeros((rows,), jnp.int32).at[dest].set(tok_flat[order])
    row_gate = jnp.zeros((rows,), jnp.float32).at[dest].set(gate.reshape(-1)[order])
    chunk_e = jnp.minimum(jnp.sum(jnp.arange(n_chunks)[:, None] * MOE_CHUNK >= pend[None, :], axis=1), N_EXPERTS - 1)

    def run_chunk(args):
        tok, e = args
        return swiglu(hf[tok], w_gate[e], w_up[e], w_down[e])

    y = lax.map(run_chunk, (row_tok.reshape(n_chunks, MOE_CHUNK), chunk_e))
    out = jnp.zeros((N, D), jnp.float32).at[row_tok].add(y.reshape(rows, D).astype(jnp.float32) * row_gate[:, None])
    return out.astype(h.dtype).reshape(B, S, D)


def setup_inputs(seed: int = 0) -> dict:
    key = jax.random.key(seed)
    k = jax.random.split(key, 24)
    n_dense = (DEPTH + 1) // 2
    n_moe = DEPTH // 2

    def nrm(kk, shape, scale):
        return jax.random.normal(kk, shape, jnp.float32) * scale

    def gain(kk, shape):
        return 1.0 + 0.02 * jax.random.normal(kk, shape, jnp.float32)

    return {
        'x': nrm(k[0], (BATCH, SEQ, D_MODEL), 1.0),
        'rel_bias': nrm(k[1], (N_BUCKETS, N_HEADS), 0.5),
        'attn_norm': gain(k[2], (DEPTH, D_MODEL)),
        'w_in': nrm(k[3], (DEPTH, D_MODEL, IN_WIDTH), D_MODEL ** -0.5),
        'nsa_q_norm': gain(k[4], (DEPTH, HEAD_DIM)),
        'nsa_k_norm': gain(k[5], (DEPTH, 3, HEAD_DIM)),
        'cmp_pos': nrm(k[6], (DEPTH, 2, CMP_LEN, HEAD_DIM), 0.1),
        'cmp_w1': nrm(k[7], (DEPTH, 2, CMP_LEN * HEAD_DIM, CMP_HIDDEN), (CMP_LEN * HEAD_DIM) ** -0.5),
        'cmp_b1': nrm(k[8], (DEPTH, 2, CMP_HIDDEN), 0.01),
        'cmp_w2': nrm(k[9], (DEPTH, 2, CMP_HIDDEN, HEAD_DIM), CMP_HIDDEN ** -0.5),
        'dil_q_norm': gain(k[10], (DEPTH, HEAD_DIM)),
        'dil_k_norm': gain(k[11], (DEPTH, HEAD_DIM)),
        'out_norm': gain(k[12], (DEPTH, MIX_WIDTH)),
        'w_out': nrm(k[13], (DEPTH, MIX_WIDTH, D_MODEL), MIX_WIDTH ** -0.5),
        'ffn_norm': gain(k[14], (DEPTH, D_MODEL)),
        'ffn_w_gate': nrm(k[15], (n_dense, D_MODEL, D_FF), D_MODEL ** -0.5),
        'ffn_w_up': nrm(k[16], (n_dense, D_MODEL, D_FF), D_MODEL ** -0.5),
        'ffn_w_down': nrm(k[17], (n_dense, D_FF, D_MODEL), D_FF ** -0.5),
        'router_w': nrm(k[18], (n_moe, D_MODEL, N_EXPERTS), D_MODEL ** -0.5),
        'exp_w_gate': nrm(k[19], (n_moe, N_EXPERTS, D_MODEL, D_FF_EXPERT), D_MODEL ** -0.5),
        'exp_w_up': nrm(k[20], (n_moe, N_EXPERTS, D_MODEL, D_FF_EXPERT), D_MODEL ** -0.5),
        'exp_w_down': nrm(k[21], (n_moe, N_EXPERTS, D_FF_EXPERT, D_MODEL), D_FF_EXPERT ** -0.5),
    }


def reference(x, rel_bias, attn_norm, w_in, nsa_q_norm, nsa_k_norm, cmp_pos, cmp_w1, cmp_b1, cmp_w2,
              dil_q_norm, dil_k_norm, out_norm, w_out, ffn_norm, ffn_w_gate, ffn_w_up, ffn_w_down,
              router_w, exp_w_gate, exp_w_up, exp_w_down):
    B, S, _ = x.shape
    tbl = rel_bias.astype(jnp.float32).T
    tbl_a = tbl[:N_HEADS_A]
    tbl_b = tbl[N_HEADS_A:]
    split_points = [int(v) for v in np.cumsum(IN_SPLITS)[:-1]]

    def heads(a):
        return a.reshape(B, S, -1, HEAD_DIM)

    h = x
    for layer in range(DEPTH):
        u = rmsnorm(h, attn_norm[layer])
        proj = u @ w_in[layer]
        qa, kca, vca, ksa, vsa, kwa, vwa, ga, qb, kb, vb = jnp.split(proj, split_points, axis=-1)
        o_a = nsa_mixer(heads(qa), heads(kca), heads(vca), heads(ksa), heads(vsa), heads(kwa), heads(vwa), ga,
                        nsa_q_norm[layer], nsa_k_norm[layer], cmp_pos[layer], cmp_w1[layer], cmp_b1[layer],
                        cmp_w2[layer], tbl_a)
        o_b = dilated_mixer(heads(qb), heads(kb), heads(vb), dil_q_norm[layer], dil_k_norm[layer], tbl_b)
        o = jnp.concatenate([rmsnorm(o_a, out_norm[layer, :WIDTH_A]), rmsnorm(o_b, out_norm[layer, WIDTH_A:])], axis=-1)
        h = h + o @ w_out[layer]
        v = rmsnorm(h, ffn_norm[layer])
        if layer % 2 == 0:
            h = h + swiglu(v, ffn_w_gate[layer // 2], ffn_w_up[layer // 2], ffn_w_down[layer // 2])
        else:
            h = h + moe_swiglu(v, router_w[layer // 2], exp_w_gate[layer // 2], exp_w_up[layer // 2], exp_w_down[layer // 2])
    return h
```

```python
import contextlib
import numpy as np
import ml_dtypes
import concourse.bass as bass
import concourse.mybir as mybir
from concourse.ap import AP
from concourse.bass_utils import run_bass_kernel_spmd

F32 = mybir.dt.float32
BF16 = mybir.dt.bfloat16
I32 = mybir.dt.int32
AF = mybir.ActivationFunctionType
ALU = mybir.AluOpType
AX = mybir.AxisListType

S = 4096
D = 1024
NT = 32
DEPTH = 2
INW = 2840
DFF = 2816
DFE = 3584
NE = 8
EPS = 1e-6
XA = 3072
XC = 6144
C_QA, C_KC, C_VC, C_KS, C_VS, C_KW, C_VW, C_GA, C_QB, C_KB, C_VB = 0, 512, 640, 768, 896, 1024, 1152, 1280, 1304, 1816, 2328


class Buf:
    __slots__ = ("name", "writers", "readers")

    def __init__(self, name=""):
        self.name = name
        self.writers = []
        self.readers = []


class Tl:
    __slots__ = ("ap", "buf", "psum")

    def __init__(self, ap, buf=None, psum=False):
        self.ap = ap
        self.buf = buf if buf is not None else Buf()
        self.psum = psum

    def __getitem__(self, k):
        return Tl(self.ap[k], self.buf, self.psum)

    def v(self, ap):
        return Tl(ap, self.buf, self.psum)


class Ctx:
    COMPUTE = ("pe", "act", "dve", "pool")

    def __init__(self, nc):
        self.nc = nc
        self.ops = {e: [] for e in ("pe", "act", "dve", "pool", "sp")}
        self.sems = {}
        self.count = {}
        self.known = {e: {} for e in self.ops}
        for e in self.COMPUTE:
            self.sems[e] = nc.alloc_semaphore(name=f"sem_{e}")
            self.count[e] = 0
        self.free_dsems = []
        self.nsem = 0
        self.region = None
        self.rec = None

    def dma_sem(self):
        if self.free_dsems:
            return self.free_dsems.pop()
        self.nsem += 1
        k = f"dma{self.nsem}"
        self.sems[k] = self.nc.alloc_semaphore(name=k)
        self.count[k] = 0
        return k

    def release(self, ks):
        self.free_dsems.extend(ks)

    def record(self, body):
        assert self.rec is None
        self.rec = []
        body()
        r, self.rec = self.rec, None
        return r

    def play(self, *lists):
        n = max(len(l) for l in lists)
        for i in range(n):
            for l in lists:
                if i < len(l):
                    self.emit(*l[i])

    def emit(self, eng, fn, reads=(), writes=(), pwrites=(), dsem=None):
        if self.rec is not None:
            self.rec.append((eng, fn, list(reads), list(writes), list(pwrites), dsem))
            return None
        deps = {}

        def add(ev, kind):
            sk, v = ev
            if sk == eng:
                if eng == "pe" or kind != "raw":
                    return
            if deps.get(sk, 0) < v:
                deps[sk] = v

        for b in reads:
            for ev in b.writers:
                add(ev, "raw")
        for b in writes:
            for ev in b.writers:
                add(ev, "waw")
            for ev in b.readers:
                add(ev, "war")
        for b in pwrites:
            for ev in b.readers:
                add(ev, "war")
        waits = []
        kn = self.known[eng]
        for sk, v in deps.items():
            if sk not in self.COMPUTE:
                v = self.count[sk]
            if kn.get(sk, 0) < v:
                kn[sk] = v
                waits.append((sk, v))
        if dsem is not None:
            self.count[dsem] += 16
            ev = (dsem, self.count[dsem])
            inc = (dsem, 16)
        else:
            self.count[eng] += 1
            ev = (eng, self.count[eng])
            inc = (eng, 1)
        self.ops[eng].append((waits, fn, inc))
        if self.region is not None and dsem is not None:
            self.region["dq"].setdefault((eng, dsem), 0)
            self.region["dq"][(eng, dsem)] += 16
        for b in reads:
            b.readers.append(ev)
            if len(b.readers) > 48:
                b.readers = self._compact(b.readers)
        for b in writes:
            b.writers = [ev]
            b.readers = []
        for b in pwrites:
            b.writers.append(ev)
            if len(b.writers) > 48:
                b.writers = self._compact(b.writers)
        return ev

    @staticmethod
    def _compact(evs):
        d = {}
        for sk, v in evs:
            if d.get(sk, 0) < v:
                d[sk] = v
        return list(d.items())

    def barrier(self):
        for eng in self.ops:
            waits = []
            kn = self.known[eng]
            for sk, v in self.count.items():
                if v > 0 and sk != eng and kn.get(sk, 0) < v:
                    kn[sk] = v
                    waits.append((sk, v))
            if waits:
                self.ops[eng].append((waits, None, None))

    def cond_begin(self, flag_ap, flag_buf):
        assert self.region is None
        self.region = {"start": dict(self.count), "known": {e: dict(k) for e, k in self.known.items()}, "dq": {}}
        for eng in self.ops:
            waits = []
            kn = self.known[eng]
            for sk, v in flag_buf.writers:
                if sk not in self.COMPUTE:
                    v = self.count[sk]
                if kn.get(sk, 0) < v:
                    kn[sk] = v
                    waits.append((sk, v))
            self.ops[eng].append(("begin", waits, flag_ap))

    def cond_end(self):
        r = self.region
        self.region = None
        for eng in self.ops:
            fix = []
            if eng in self.COMPUTE:
                n = self.count[eng] - r["start"][eng]
                if n > 0:
                    fix.append((eng, r["start"][eng], n))
            for (q, dsem), n in r["dq"].items():
                if q == eng:
                    fix.append((dsem, r["start"].get(dsem, 0), n))
            self.ops[eng].append(("end", fix, None))
            self.known[eng] = r["known"][eng]

    def flush(self):
        nc = self.nc
        sems = self.sems
        with nc.Block() as block:
            def mk(ename):
                def run(engine):
                    ops = self.ops[ename]
                    stack = []
                    i = 0
                    while i < len(ops):
                        a, b, c3 = ops[i]
                        if isinstance(a, str) and a == "begin":
                            for sk, v in b:
                                engine.wait_ge(sems[sk], v)
                            if isinstance(ops[i + 1][0], str) and ops[i + 1][0] == "end" and not ops[i + 1][1]:
                                i += 2
                                continue
                            val = engine.value_load(c3)
                            guard = engine.If(val)
                            guard.__enter__()
                            stack.append((guard, val))
                        elif isinstance(a, str) and a == "end":
                            guard, val = stack.pop()
                            guard.__exit__(None, None, None)
                            with engine.Else():
                                for sk, prior, n in b:
                                    if prior > 0:
                                        engine.wait_ge(sems[sk], prior)
                                    engine.sem_inc(sems[sk], n)
                            engine.free_register(val.val)
                        else:
                            for sk, v in a:
                                engine.wait_ge(sems[sk], v)
                            if b is not None:
                                b(engine).then_inc(sems[c3[0]], c3[1])
                        i += 1
                return run
            block.tensor(mk("pe"))
            block.scalar(mk("act"))
            block.vector(mk("dve"))
            block.gpsimd(mk("pool"))
            block.sync(mk("sp"))


def bc_last(ap, n):
    return AP(ap.tensor, ap.offset, [list(x) for x in ap.ap] + [[0, n]])


def bc_mid(ap, nh):
    a = [list(x) for x in ap.ap]
    return AP(ap.tensor, ap.offset, [a[0], [0, nh]] + a[1:])


class KB:
    def __init__(self, nc):
        self.nc = nc
        self.c = Ctx(nc)
        self.uid = 0

    def name(self, p):
        self.uid += 1
        return f"{p}_{self.uid}"

    def sb(self, es, shape, dt, name="t"):
        h = es.enter_context(self.nc.sbuf_tensor(self.name(name), list(shape), dt))
        return Tl(h.ap())

    def sbn(self, es, n, shape, dt, name="t"):
        return [self.sb(es, shape, dt, name) for _ in range(n)]

    def _rw(self, outs, ins):
        reads, writes = [], []
        for t in ins:
            if t is None:
                continue
            (writes if t.psum else reads).append(t.buf)
        for t in outs:
            writes.append(t.buf)
        return reads, writes

    def op(self, eng, fn, outs, ins, pw=()):
        reads, writes = self._rw(outs, ins)
        pwb = [t.buf for t in pw]
        return self.c.emit(eng, fn, reads=reads, writes=writes, pwrites=pwb)

    def dma(self, out, in_, sem, q="sp", pw=False):
        o, i = out.ap, in_.ap
        reads = [in_.buf]
        if pw:
            return self.c.emit(q, lambda e: e.dma_start(out=o, in_=i), reads=reads, pwrites=[out.buf], dsem=sem)
        return self.c.emit(q, lambda e: e.dma_start(out=o, in_=i), reads=reads, writes=[out.buf], dsem=sem)

    def mm(self, out, lhsT, rhs, start=True, stop=True):
        o, l, r = out.ap, lhsT.ap, rhs.ap
        return self.op("pe", lambda e: e.matmul(o, lhsT=l, rhs=r, start=start, stop=stop, skip_group_check=True), [out], [lhsT, rhs])

    def tr(self, out, in_, ident):
        o, i, d = out.ap, in_.ap, ident.ap
        return self.op("pe", lambda e: e.transpose(out=o, in_=i, identity=d), [out], [in_, ident])

    def act(self, out, in_, func, bias=None, scale=None, accum=None, eng="act"):
        o, i = out.ap, in_.ap
        kw = {}
        ins = [in_]
        if bias is not None:
            if isinstance(bias, Tl):
                kw["bias"] = bias.ap
                ins.append(bias)
            else:
                kw["bias"] = bias
        if scale is not None:
            if isinstance(scale, Tl):
                kw["scale"] = scale.ap
                ins.append(scale)
            else:
                kw["scale"] = scale
        outs = [out]
        if accum is not None:
            kw["accum_out"] = accum.ap
            outs.append(accum)
        return self.op("act", lambda e: e.activation(out=o, in_=i, func=func, **kw), outs, ins)

    def tt(self, eng, out, in0, in1, op):
        o, a, b = out.ap, in0.ap, in1.ap
        return self.op(eng, lambda e: e.tensor_tensor(out=o, in0=a, in1=b, op=op), [out], [in0, in1])

    def ts(self, eng, out, in0, s1, op0, s2=None, op1=None):
        o, a = out.ap, in0.ap
        ins = [in0]
        v1 = s1
        if isinstance(s1, Tl):
            ins.append(s1)
            v1 = s1.ap
        v2 = s2
        if isinstance(s2, Tl):
            ins.append(s2)
            v2 = s2.ap
        if op1 is None:
            return self.op(eng, lambda e: e.tensor_scalar(out=o, in0=a, scalar1=v1, scalar2=None, op0=op0), [out], ins)
        return self.op(eng, lambda e: e.tensor_scalar(out=o, in0=a, scalar1=v1, scalar2=v2, op0=op0, op1=op1), [out], ins)

    def stt(self, out, in0, scalar, in1, op0, op1):
        o, a, b = out.ap, in0.ap, in1.ap
        ins = [in0, in1]
        sv = scalar
        if isinstance(scalar, Tl):
            ins.append(scalar)
            sv = scalar.ap
        return self.op("dve", lambda e: e.scalar_tensor_tensor(out=o, in0=a, scalar=sv, in1=b, op0=op0, op1=op1), [out], ins)

    def copy(self, eng, out, in_):
        o, i = out.ap, in_.ap
        if eng == "act":
            return self.op("act", lambda e: e.copy(out=o, in_=i), [out], [in_])
        return self.op(eng, lambda e: e.tensor_copy(out=o, in_=i), [out], [in_])

    def memset(self, eng, out, val):
        o = out.ap
        return self.op(eng, lambda e: e.memset(o, val), [out], [])

    def recip(self, out, in_):
        o, i = out.ap, in_.ap
        return self.op("dve", lambda e: e.reciprocal(out=o, in_=i), [out], [in_])

    def reduce(self, out, in_, op=ALU.add):
        o, i = out.ap, in_.ap
        return self.op("dve", lambda e: e.tensor_reduce(out=o, in_=i, axis=AX.X, op=op), [out], [in_])

    def rstd(self, es_tmp, out, ssq, n, tmp):
        self.act(tmp, ssq, AF.Sqrt, bias=self.epsb[0:ssq.ap.shape[0], :], scale=1.0 / n)
        self.recip(out, tmp)


def t5_bucket_np(dist):
    dist = np.maximum(dist, 0)
    max_exact = 16
    scaled = np.log(np.maximum(dist, 1).astype(np.float32) / np.float32(max_exact)) / np.float32(np.log(2048 / 16))
    large = np.minimum(max_exact + (scaled.astype(np.float32) * np.float32(16)).astype(np.int32), 31)
    return np.where(dist < max_exact, dist, large)


def host_consts():
    bf = ml_dtypes.bfloat16
    cst = {}
    cst["ident_bf"] = np.eye(128, dtype=np.float32).astype(bf)
    cst["anti_bf"] = np.eye(128, dtype=np.float32)[::-1].copy().astype(bf)
    cst["ident_f"] = np.eye(128, dtype=np.float32)
    da = np.arange(XA) - 511
    oh = np.zeros((32, XA + XC), np.float32)
    ba = t5_bucket_np(da)
    oh[ba, np.arange(XA)] = 1.0
    dc = np.arange(XC) - 2063
    bc = t5_bucket_np(dc)
    oh[bc, XA + np.arange(XC)] = 1.0
    cst["onehot"] = oh
    mult = np.zeros((4, XC), np.float32)
    mult[0, :XA] = (da >= 0)
    mult[1, :XA] = (da >= 0) & (da < 512)
    mult[2, :XA] = ((da >= 0) & (da <= 128)).astype(np.float32) + ((da >= 0) & (da % 4 == 0) & (da <= 512)) + ((da >= 0) & (da % 16 == 0) & (da <= 2048))
    mult[3, :] = (dc >= 0)
    with np.errstate(divide="ignore"):
        mult[:] = np.where(mult > 0, np.log(np.maximum(mult, 1e-30)), -30000.0)
    mult[0:3, XA:] = 0
    cst["mult"] = np.ascontiguousarray(np.broadcast_to(mult[:, None, :], (4, 16, XC))).astype(np.float32)
    n = 128 * (np.arange(256) // 128) + 127 - (np.arange(256) % 128)
    c_start = n[:, None] * 16
    s_start = np.arange(64)[None, :] * 64
    ov = np.clip(np.minimum(c_start + 32, s_start + 64) - np.maximum(c_start, s_start), 0, None).astype(np.float32) / 32
    ov[n == 255] = 0
    cst["overlap"] = ov.astype(bf)
    key = 128 * (np.arange(S) // 128) + 127 - (np.arange(S) % 128)
    ex = np.zeros((64, S), np.float32)
    ex[key // 64, np.arange(S)] = 1
    cst["exr"] = ex.astype(bf)
    t = np.arange(S)[:, None]
    j = np.arange(64)[None, :]
    cur = t // 64
    fm = np.where((j == 0) | (j == cur) | (j == cur - 1), 1e6, np.where(j * 64 > t, -1e6, 0.0)).astype(np.float32)
    cst["fm"] = fm
    cst["ecst"] = np.ascontiguousarray(np.broadcast_to((np.arange(NE) * S).astype(np.float32)[None, :], (128, NE)))
    cst["utri"] = np.triu(np.ones((128, 128), np.float32), k=1).astype(bf)
    cst["ones_bf"] = np.ones((128, 128), np.float32).astype(bf)
    thr = np.broadcast_to((np.arange(8) * 512).astype(np.float32)[None, None, :], (128, NE, 8))
    cst["jthr"] = np.ascontiguousarray(thr).reshape(128, NE * 8)
    return cst


class Prog:
    def __init__(self, debug=()):
        self.debug = set(debug)
        nc = self.nc = bass.Bass("TRN2", target_bir_lowering=False)
        self.kb = KB(nc)
        self.es = contextlib.ExitStack()
        self.inp = {}
        self.scr = {}

    def din(self, name, shape, dt=F32):
        t = self.nc.dram_tensor(name, list(shape), dt, kind="ExternalInput").ap()
        self.inp[name] = Tl(t)
        return self.inp[name]

    def dscr(self, name, shape, dt):
        kind = "ExternalOutput" if name in self.debug else "Internal"
        t = self.nc.dram_tensor(name, list(shape), dt, kind=kind).ap()
        self.scr[name] = Tl(t)
        return self.scr[name]

    def declare(self):
        d = self.din
        d("x", [S, D]); d("rel_bias", [32, 16]); d("attn_norm", [DEPTH, D]); d("w_in", [DEPTH, D, INW])
        d("nsa_q_norm", [DEPTH, 64]); d("nsa_k_norm", [DEPTH, 3, 64]); d("cmp_pos", [DEPTH, 2, 32, 64])
        d("cmp_w1", [DEPTH, 2, 2048, 256]); d("cmp_b1", [DEPTH, 2, 256]); d("cmp_w2", [DEPTH, 2, 256, 64])
        d("dil_q_norm", [DEPTH, 64]); d("dil_k_norm", [DEPTH, 64]); d("out_norm", [DEPTH, D]); d("w_out", [DEPTH, D, D])
        d("ffn_norm", [DEPTH, D]); d("ffn_w_gate", [1, D, DFF]); d("ffn_w_up", [1, D, DFF]); d("ffn_w_down", [1, DFF, D])
        d("router_w", [1, D, NE]); d("exp_w_gate", [1, NE, D, DFE]); d("exp_w_up", [1, NE, D, DFE]); d("exp_w_down", [1, NE, DFE, D])
        d("ident_bf", [128, 128], BF16); d("anti_bf", [128, 128], BF16); d("ident_f", [128, 128])
        d("onehot", [32, XA + XC]); d("mult", [4, 16, XC]); d("overlap", [256, 64], BF16); d("exr", [64, S], BF16); d("fm", [S, 64])
        d("ecst", [128, NE]); d("utri", [128, 128], BF16); d("ones_bf", [128, 128], BF16); d("jthr", [128, NE * 8])
        self.out = Tl(self.nc.dram_tensor("out", [S, D], F32, kind="ExternalOutput").ap())
        s = self.dscr
        s("wtab", [4, 16, XC], BF16)
        s("qaT", [8, 64, S], BF16); s("kcvT", [2, 2, 64, S], BF16); s("kswT", [2, 2, 64, S], BF16)
        s("vsw", [S, 256], BF16); s("gates", [S, 24], F32)
        s("qbT", [8, 64, S], BF16); s("kbT", [8, 64, S], BF16); s("vb", [S, 512], BF16)
        s("OC", [S, 512], F32); s("O", [S, D], F32); s("H1", [S, D], F32)
        s("HP", [S, D], F32); s("XE", [NE * S, D], BF16); s("YE", [NE * S, D], F32)
        if "dbg" in self.debug:
            s("dbg", [128, 4096], F32)

    def setup_globals(self):
        kb, es = self.kb, self.es
        nc = self.nc
        self.banks = []
        for i in range(8):
            h = es.enter_context(nc.psum_tensor(f"bank{i}", [128, 512], F32))
            self.banks.append(Tl(h.ap(), psum=True))
        self.ident_bf = kb.sb(es, [128, 128], BF16, "identbf")
        self.anti_bf = kb.sb(es, [128, 128], BF16, "antibf")
        self.ident_f = kb.sb(es, [128, 128], F32, "identf")
        kb.epsb = kb.sb(es, [128, 1], F32, "epsb")
        self.one11 = kb.sb(es, [1, 1], F32, "one11")
        self.kcmpT = kb.sb(es, [64, 2, 2, 128], BF16, "kcmpT")
        self.vcaug = kb.sb(es, [128, 2, 2, 128], BF16, "vcaug")
        sem = kb.c.dma_sem()
        kb.dma(self.ident_bf, self.inp["ident_bf"], sem)
        kb.dma(self.anti_bf, self.inp["anti_bf"], sem)
        kb.dma(self.ident_f, self.inp["ident_f"], sem)
        kb.memset("dve", kb.epsb, EPS)
        kb.memset("dve", self.one11, 1.0)
        kb.c.barrier()

    def phase_tables(self):
        kb = self.kb
        with contextlib.ExitStack() as es:
            tbl = kb.sb(es, [32, 16], F32, "tbl")
            oh = kb.sbn(es, 2, [32, 512], F32, "oh")
            e32 = kb.sbn(es, 2, [16, 512], F32, "e32")
            mt = kb.sbn(es, 2, [16, 512], F32, "mt")
            wb = kb.sbn(es, 2, [16, 512], BF16, "wb")
            sems = [kb.c.dma_sem() for _ in range(7)]
            kb.dma(tbl, self.inp["rel_bias"], sems[0])
            wtab = self.scr["wtab"]
            k = 0
            for ci in range((XA + XC) // 512):
                x0 = ci * 512
                o = oh[ci % 2]
                kb.dma(o, self.inp["onehot"][:, x0:x0 + 512], sems[1 + ci % 2])
                bank = self.banks[ci % 2]
                kb.mm(bank[0:16, :], tbl, o)
                e = e32[ci % 2]
                kb.copy("act", e, bank[0:16, :])
                tabs = [(0, x0), (1, x0), (2, x0)] if x0 < XA else [(3, x0 - XA)]
                for tb, xx in tabs:
                    m = mt[k % 2]
                    w = wb[k % 2]
                    kb.dma(m, self.inp["mult"][tb, :, xx:xx + 512], sems[3 + k % 2])
                    kb.tt("dve", w, e, m, ALU.add)
                    kb.dma(wtab[tb, :, xx:xx + 512], w, sems[5 + k % 2], pw=True)
                    k += 1
            kb.c.barrier()
            kb.c.release(sems)

    def phase_inproj(self, layer, hin_dram):
        kb = self.kb
        I = self.inp
        with contextlib.ExitStack() as es:
            W = kb.sb(es, [128, 8, INW], BF16, "win")
            gA = kb.sb(es, [128, D], F32, "gA")
            g6 = kb.sb(es, [128, 6, 64], F32, "g6")
            hin = kb.sbn(es, 2, [128, D], F32, "hin")
            junk = kb.sb(es, [128, INW], F32, "junk")
            junkA = kb.sb(es, [128, D], F32, "junkA")
            ssq = kb.sbn(es, 2, [128, 1], F32, "ssq")
            rms = kb.sbn(es, 2, [128, 1], F32, "rms")
            rstd = kb.sbn(es, 2, [128, 1], F32, "rstd")
            u = kb.sbn(es, 2, [128, D], BF16, "u")
            uT = kb.sbn(es, 2, [128, 8, 128], BF16, "uT")
            pj = kb.sbn(es, 2, [128, INW], F32, "pj")
            ssh = kb.sbn(es, 2, [128, 28], F32, "ssh")
            rmh = kb.sbn(es, 2, [128, 28], F32, "rmh")
            rsh = kb.sbn(es, 2, [128, 28], F32, "rsh")
            t1 = kb.sbn(es, 2, [128, 1024], F32, "t1")
            nb = kb.sbn(es, 2, [128, 2328], BF16, "nb")
            vall = kb.sbn(es, 2, [128, 768], BF16, "vall")
            gt = kb.sbn(es, 2, [128, 24], F32, "gt")
            stg_q = kb.sbn(es, 2, [128, 4, 128], BF16, "stgq")
            stg_c = kb.sbn(es, 2, [128, 2, 128], BF16, "stgc")
            stg_k = kb.sbn(es, 2, [128, 2, 128], BF16, "stgk")
            stg_qb = kb.sbn(es, 2, [128, 4, 128], BF16, "stgqb")
            stg_kb = kb.sbn(es, 2, [128, 4, 128], BF16, "stgkb")
            stg_v = kb.sbn(es, 2, [128, 768], BF16, "stgv")
            sems = [kb.c.dma_sem() for _ in range(20)]
            wv = I["w_in"][layer].v(I["w_in"].ap[layer].rearrange("(kc p) n -> p kc n", p=128))
            for kc in range(8):
                kb.dma(W[:, kc, :], wv[:, kc, :], sems[0], q="pool", pw=True)
            kb.dma(gA, I["attn_norm"].v(I["attn_norm"].ap[layer].partition_broadcast(128)), sems[1])
            gsrc = [I["nsa_q_norm"].ap[layer], I["nsa_k_norm"].ap[layer, 1], I["nsa_k_norm"].ap[layer, 2],
                    I["dil_q_norm"].ap[layer], I["dil_k_norm"].ap[layer], I["nsa_k_norm"].ap[layer, 0]]
            for i, a in enumerate(gsrc):
                kb.dma(g6[:, i, :], Tl(a.partition_broadcast(128), I["nsa_q_norm"].buf), sems[1], pw=True)
            kb.ts("dve", g6[:, 0, :], g6[:, 0, :], 0.125, ALU.mult)
            kb.ts("dve", g6[:, 3, :], g6[:, 3, :], 0.125, ALU.mult)
            self.g6_k0 = None
            bk = self.banks
            hv = hin_dram.v(hin_dram.ap.rearrange("(t p) d -> t p d", p=128))
            qaT2 = self.scr["qaT"].v(self.scr["qaT"].ap.rearrange("h d s -> (h d) s").rearrange("(a p) s -> p a s", p=128))
            kcvT2 = self.scr["kcvT"].v(self.scr["kcvT"].ap.rearrange("k g d s -> (k g d) s").rearrange("(a p) s -> p a s", p=128))
            kswT2 = self.scr["kswT"].v(self.scr["kswT"].ap.rearrange("k g d s -> (k g d) s").rearrange("(a p) s -> p a s", p=128))
            qbT2 = self.scr["qbT"].v(self.scr["qbT"].ap.rearrange("h d s -> (h d) s").rearrange("(a p) s -> p a s", p=128))
            kbT2 = self.scr["kbT"].v(self.scr["kbT"].ap.rearrange("h d s -> (h d) s").rearrange("(a p) s -> p a s", p=128))
            def pre(t):
                s = t % 2
                ts_ = slice(t * 128, (t + 1) * 128)
                kb.dma(hin[s], hv[t], sems[2 + s], q="pool")
                h = hin[s]
                kb.act(junkA, h, AF.Square, accum=ssq[s])
                kb.act(rms[s], ssq[s], AF.Sqrt, bias=kb.epsb, scale=1.0 / D)
                kb.recip(rstd[s], rms[s])
                kb.stt(u[s], h, rstd[s], gA, ALU.mult, ALU.mult)
                b2 = bk[2].v(bk[2].ap.bitcast(BF16))
                for kc in range(8):
                    kb.tr(b2[:, kc * 128:(kc + 1) * 128], u[s][:, kc * 128:(kc + 1) * 128], self.ident_bf)
                kb.copy("act", uT[s], b2.v(b2.ap.rearrange("p (a b) -> p a b", b=128)))
            def mmf(t):
                s = t % 2
                for cg in range(6):
                    c0 = cg * 512
                    cw = min(512, INW - c0)
                    bank = bk[cg % 2]
                    for kc in range(8):
                        kb.mm(bank[:, 0:cw], uT[s][:, kc, :], W[:, kc, c0:c0 + cw], start=(kc == 0), stop=(kc == 7))
                    kb.copy("act" if cg % 2 == 0 else "dve", pj[s][:, c0:c0 + cw], bank[:, 0:cw])
            def back(t):
                s = t % 2
                ts_ = slice(t * 128, (t + 1) * 128)
                p = pj[s]
                kb.act(junk, p, AF.Square)
                for (c0, nh, r0) in ((C_QA, 8, 0), (C_KS, 2, 8), (C_KW, 2, 10), (C_QB, 16, 12)):
                    kb.reduce(ssh[s][:, r0:r0 + nh], junk.v(junk.ap[:, c0:c0 + nh * 64].rearrange("p (h d) -> p h d", d=64)))
                kb.act(rmh[s], ssh[s], AF.Sqrt, bias=kb.epsb, scale=1.0 / 64)
                kb.recip(rsh[s], rmh[s])
                n_ = nb[s]
                for (c0, nh, r0, gi) in ((C_QA, 8, 0, 0), (C_KS, 2, 8, 1), (C_KW, 2, 10, 2), (C_QB, 8, 12, 3), (C_KB, 8, 20, 4)):
                    tv = t1[s].v(t1[s].ap[:, 0:nh * 64].rearrange("p (h d) -> p h d", d=64))
                    pv = p.v(p.ap[:, c0:c0 + nh * 64].rearrange("p (h d) -> p h d", d=64))
                    kb.tt("dve", tv, pv, rsh[s].v(bc_last(rsh[s].ap[:, r0:r0 + nh], 64)), ALU.mult)
                    nv = n_.v(n_.ap[:, c0:c0 + nh * 64].rearrange("p (h d) -> p h d", d=64))
                    kb.tt("pool", nv, tv, g6.v(bc_mid(g6.ap[:, gi, :], nh)), ALU.mult)
                kb.copy("pool", n_[:, C_KC:C_KC + 256], p[:, C_KC:C_KC + 256])
                kb.copy("act", vall[s][:, 0:128], p[:, C_VS:C_VS + 128])
                kb.copy("act", vall[s][:, 128:256], p[:, C_VW:C_VW + 128])
                kb.copy("act", vall[s][:, 256:768], p[:, C_VB:C_VB + 512])
                kb.act(gt[s], p[:, C_GA:C_GA + 24], AF.Sigmoid)
                kb.dma(self.scr["gates"][ts_, :], gt[s], sems[4 + s], pw=True)
                b3 = bk[3].v(bk[3].ap.bitcast(BF16))
                for a in range(4):
                    kb.tr(b3[:, a * 128:(a + 1) * 128], n_[:, C_QA + a * 128:C_QA + (a + 1) * 128], self.ident_bf)
                for a in range(4):
                    kb.tr(b3[:, 512 + a * 128:512 + (a + 1) * 128], n_[:, C_QB + a * 128:C_QB + (a + 1) * 128], self.ident_bf)
                kb.copy("dve", stg_q[s], b3.v(b3.ap[:, 0:512].rearrange("p (a b) -> p a b", b=128)))
                kb.copy("act", stg_qb[s], b3.v(b3.ap[:, 512:1024].rearrange("p (a b) -> p a b", b=128)))
                kb.dma(qaT2[:, :, ts_], stg_q[s], sems[6 + s], pw=True)
                kb.dma(qbT2[:, :, ts_], stg_qb[s], sems[8 + s], pw=True)
                b4 = bk[4].v(bk[4].ap.bitcast(BF16))
                for a in range(2):
                    kb.tr(b4[:, a * 128:(a + 1) * 128], n_[:, C_KC + a * 128:C_KC + (a + 1) * 128], self.ident_bf)
                kb.copy("dve", stg_c[s], b4.v(b4.ap[:, 0:256].rearrange("p (a b) -> p a b", b=128)))
                kb.dma(kcvT2[:, :, ts_], stg_c[s], sems[10 + s], pw=True)
                for a, c0 in enumerate((C_KS, C_KW)):
                    kb.mm(bk[5][:, a * 128:(a + 1) * 128], n_[:, c0:c0 + 128], self.anti_bf)
                kb.copy("act", stg_k[s], bk[5].v(bk[5].ap[:, 0:256].rearrange("p (a b) -> p a b", b=128)))
                kb.dma(kswT2[:, :, ts_], stg_k[s], sems[12 + s], pw=True)
                for a in range(4):
                    kb.mm(bk[6][:, a * 128:(a + 1) * 128], n_[:, C_KB + a * 128:C_KB + (a + 1) * 128], self.anti_bf)
                kb.copy("dve", stg_kb[s], bk[6].v(bk[6].ap.rearrange("p (a b) -> p a b", b=128)))
                kb.dma(kbT2[:, :, ts_], stg_kb[s], sems[14 + s], pw=True)
                kb.mm(bk[7], self.anti_bf, vall[s][:, 256:768])
                kb.copy("act", stg_v[s][:, 256:768], bk[7])
                kb.mm(bk[5][:, 256:512], self.anti_bf, vall[s][:, 0:256])
                kb.copy("dve", stg_v[s][:, 0:256], bk[5][:, 256:512])
                kb.dma(self.scr["vsw"][ts_, :], stg_v[s][:, 0:256], sems[16 + s], pw=True)
                kb.dma(self.scr["vb"][ts_, :], stg_v[s][:, 256:768], sems[18 + s], pw=True)

            kb.c.play(kb.c.record(lambda: pre(0)))
            kb.c.play(kb.c.record(lambda: pre(1)), kb.c.record(lambda: mmf(0)))
            for t in range(NT):
                lb = kb.c.record(lambda: back(t))
                k = next(i for i, o in enumerate(lb) if o[0] == "pe")
                lists = [lb[:k]]
                if t + 1 < NT:
                    lists.insert(0, kb.c.record(lambda: mmf(t + 1)))
                if t + 2 < NT:
                    lists.insert(0, kb.c.record(lambda: pre(t + 2)))
                kb.c.play(*lists)
                kb.c.play(lb[k:])
            kb.c.barrier()
            kb.c.release(sems)

    def phase_compress(self, layer):
        kb = self.kb
        I = self.inp
        bk = self.banks
        with contextlib.ExitStack() as es:
            kvT = kb.sbn(es, 2, [64, S], BF16, "kvT")
            w1 = kb.sbn(es, 2, [64, 32, 256], BF16, "w1")
            w2 = kb.sbn(es, 2, [128, 2, 64], BF16, "w2")
            pos = kb.sbn(es, 2, [32, 64], F32, "pos")
            posT = kb.sbn(es, 2, [64, 32], BF16, "posT")
            b1 = kb.sbn(es, 2, [1, 256], F32, "b1")
            bias = kb.sbn(es, 2, [128, 2], F32, "bias")
            hid = kb.sbn(es, 2, [128, 2, 256], BF16, "hid")
            gk = kb.sb(es, [128, 64], F32, "gk")
            xk = kb.sbn(es, 2, [128, 64], F32, "xk")
            jk = kb.sb(es, [128, 64], F32, "jk")
            sq1 = kb.sbn(es, 2, [128, 1], F32, "sq1")
            rm1 = kb.sbn(es, 2, [128, 1], F32, "rm1")
            rs1 = kb.sbn(es, 2, [128, 1], F32, "rs1")
            xb = kb.sbn(es, 2, [128, 64], BF16, "xb")
            sems = [kb.c.dma_sem() for _ in range(8)]
            kb.dma(gk, Tl(I["nsa_k_norm"].ap[layer, 0].partition_broadcast(128), I["nsa_k_norm"].buf), sems[0])
            it = 0
            for g in range(2):
                for kv in range(2):
                    s = it % 2
                    it += 1
                    kb.dma(kvT[s], self.scr["kcvT"][kv, g], sems[1 + s])
                    w1src = I["cmp_w1"].v(I["cmp_w1"].ap[layer, kv].rearrange("(l d) c -> d l c", d=64))
                    for lq in range(4):
                        kb.dma(w1[s][:, lq * 8:(lq + 1) * 8, :], w1src[:, lq * 8:(lq + 1) * 8, :], sems[3 + s], q="pool", pw=True)
                    kb.dma(w2[s], I["cmp_w2"].v(I["cmp_w2"].ap[layer, kv].rearrange("(hh p) c -> p hh c", p=128)), sems[3 + s], q="pool", pw=True)
                    kb.dma(pos[s], I["cmp_pos"][layer, kv], sems[5 + s], pw=True)
                    kb.dma(b1[s], I["cmp_b1"][layer, kv:kv + 1, :], sems[5 + s], pw=True)
                    kb.tr(bk[2][0:64, 0:32], pos[s], self.ident_f[0:32, 0:32])
                    kb.copy("dve", posT[s], bk[2][0:64, 0:32])
                    for hh in range(2):
                        hs = slice(hh * 128, (hh + 1) * 128)
                        for l in range(32):
                            kb.mm(bk[3][:, hh:hh + 1], w1[s][:, l, hs], posT[s][:, l:l + 1], start=(l == 0), stop=False)
                        kb.mm(bk[3][:, hh:hh + 1], b1[s][0:1, hs], self.one11, start=False, stop=True)
                    kb.copy("dve", bias[s], bk[3][:, 0:2])
                    kb.memset("pool", hid[s][:, :, 255:256], 0.0)
                    for hh in range(2):
                        hs = slice(hh * 128, (hh + 1) * 128)
                        bank = bk[hh]
                        ka = kvT[s].ap
                        for l in range(32):
                            rhs = kvT[s].v(AP(ka.tensor, ka.offset + l, [list(ka.ap[0]), [16, 255]]))
                            kb.mm(bank[:, 0:255], w1[s][:, l, hs], rhs, start=(l == 0), stop=(l == 31))
                        kb.act(hid[s][:, hh, 0:255], bank[:, 0:255], AF.Gelu_apprx_tanh, bias=bias[s][:, hh:hh + 1])
                    for nt in range(2):
                        ns = slice(nt * 128, (nt + 1) * 128)
                        bank = bk[4 + nt]
                        for hh in range(2):
                            kb.mm(bank[:, 0:64], hid[s][:, hh, ns], w2[s][:, hh, :], start=(hh == 0), stop=(hh == 1))
                        j = (it + nt) % 2
                        if kv == 0:
                            kb.copy("dve", xk[j], bank[:, 0:64])
                            kb.act(jk, xk[j], AF.Square, accum=sq1[j])
                            kb.act(rm1[j], sq1[j], AF.Sqrt, bias=kb.epsb, scale=1.0 / 64)
                            kb.recip(rs1[j], rm1[j])
                            kb.stt(xb[j], xk[j], rs1[j], gk, ALU.mult, ALU.mult)
                            kb.mm(bk[6 + nt][0:64, 0:128], xb[j], self.anti_bf)
                            kb.copy("act", self.kcmpT[:, g, nt, :], bk[6 + nt][0:64, 0:128])
                        else:
                            kb.copy("dve", xb[j], bank[:, 0:64])
                            kb.mm(bk[6 + nt][:, 0:64], self.anti_bf, xb[j])
                            kb.copy("act", self.vcaug[:, g, nt, 0:64], bk[6 + nt][:, 0:64])
            for g in range(2):
                for nt in range(2):
                    kb.dma(self.vcaug[:, g, nt, 64:128], self.inp["overlap"][nt * 128:(nt + 1) * 128, :], sems[7], pw=True)
            kb.c.barrier()
            kb.c.release(sems)


    def run_attn(self, items, ebuf, tbuf, pbuf):
        kb = self.kb
        bk = self.banks
        n = len(items)
        if n == 0:
            return

        LA = 3
        pending = []

        def score(i):
            it = items[i]
            c0, c1 = it["qa"] * 128, it["qb"] * 128
            kb.mm(bk[i % 4][:, c0:c1], it["kT"], it["q"][:, c0:c1], start=True, stop=False)
            kb.mm(bk[i % 4][:, c0:c1], self.ident_bf, it["strip"][:, c0:c1], start=False, stop=True)
        for i in range(min(LA, n)):
            score(i)
        for i in range(n):
            if i + LA < n:
                score(i + LA)
            it = items[i]
            c0, c1 = it["qa"] * 128, it["qb"] * 128
            p = pbuf[i % len(pbuf)]
            kb.act(p[:, c0:c1], bk[i % 4][:, c0:c1], AF.Exp)
            kb.mm(it["pv"][:, c0:c1], it["vaug"], p[:, c0:c1], start=it["first"], stop=it["last"])
            if it["last"]:
                pending.append((i + 2, it["fin"]))
            while pending and pending[0][0] <= i:
                pending.pop(0)[1]()
        while pending:
            pending.pop(0)[1]()

    def hankel(self, tb, hd, pstep, ncols):
        w = self.scr["wtab"]
        a = w.ap[tb, hd]
        return w.v(AP(a.tensor, a.offset, [[pstep, 128], [1, ncols]]))

    def phase_nsa(self, layer):
        kb = self.kb
        I = self.inp
        bk = self.banks
        with contextlib.ExitStack() as es0:
            gates = kb.sb(es0, [128, NT, 24], F32, "gates")
            selT = kb.sb(es0, [128, 2, S], BF16, "selT")
            sem0 = kb.c.dma_sem()
            kb.dma(gates, self.scr["gates"].v(self.scr["gates"].ap.rearrange("(t p) c -> p t c", p=128)), sem0)
            kb.c.barrier()
            OCv = self.scr["OC"].v(self.scr["OC"].ap.rearrange("(t p) c -> p t c", p=128))
            Ov = self.scr["O"].v(self.scr["O"].ap.rearrange("(t p) c -> p t c", p=128))
            with contextlib.ExitStack() as es:
                fm = kb.sb(es, [128, NT, 64], F32, "fm")
                imp = kb.sb(es, [128, NT, 64], F32, "imp")
                stripc = kb.sbn(es, 2, [128, S], BF16, "stripc")
                qTh = kb.sbn(es, 2, [64, S], BF16, "qTh")
                eb = kb.sbn(es, 3, [128, 512], BF16, "eb")
                pb = kb.sbn(es, 3, [128, 512], BF16, "pb")
                den = kb.sbn(es, 2, [128, 4], F32, "den")
                rd = kb.sbn(es, 2, [128, 4], F32, "rd")
                sc = kb.sbn(es, 2, [128, 4], F32, "sc")
                itmp = kb.sbn(es, 2, [128, 4, 64], F32, "itmp")
                ocs = kb.sbn(es, 2, [128, 4, 64], F32, "ocs")
                impf = kb.sbn(es, 2, [128, 64], F32, "impf")
                imp2 = kb.sbn(es, 2, [128, 64], F32, "imp2")
                m8a = kb.sbn(es, 2, [128, 8], F32, "m8a")
                m8b = kb.sbn(es, 2, [128, 8], F32, "m8b")
                selm = kb.sbn(es, 2, [128, 128], BF16, "selm")
                sems = [kb.c.dma_sem() for _ in range(7)]
                kb.dma(fm, I["fm"].v(I["fm"].ap.rearrange("(t p) c -> p t c", p=128)), sems[0])
                kb.memset("dve", selm[0], 0.0)
                kb.memset("dve", selm[1], 0.0)
                kb.c.barrier()
                it = 0
                cnt = 0
                for g in range(2):
                    for h in range(4):
                        hd = 4 * g + h
                        s = it % 2
                        it += 1
                        kb.dma(stripc[s], self.hankel(3, hd, 16, S), sems[1 + s])
                        kb.dma(qTh[s], self.scr["qaT"][hd], sems[3 + s])
                        for QG in range(8):
                            qs = slice(QG * 512, (QG + 1) * 512)
                            ps = []
                            for nt in range(2 if QG >= 4 else 1):
                                bank = bk[nt]
                                x0 = QG * 512 - nt * 2048
                                kb.mm(bank, self.kcmpT[:, g, nt, :], qTh[s][:, qs], start=True, stop=False)
                                kb.mm(bank, self.ident_bf, stripc[s][:, x0:x0 + 512], start=False, stop=True)
                                p = pb[cnt % 3]
                                cnt += 1
                                kb.act(p, bank, AF.Exp)
                                ps.append(p)
                            u_ = (it * 8 + QG) % 2
                            oc = ocs[u_]
                            ob = bk[2 + u_]
                            for qt in range(4):
                                for nt, p in enumerate(ps):
                                    kb.mm(ob[:, qt * 128:(qt + 1) * 128], p[:, qt * 128:(qt + 1) * 128], self.vcaug[:, g, nt, :], start=(nt == 0), stop=(nt == len(ps) - 1))
                            obv = ob.v(ob.ap.rearrange("p (a b) -> p a b", b=128))
                            kb.reduce(den[u_], obv[:, :, 64:128])
                            kb.ts("dve", den[u_], den[u_], 1e-30, ALU.max)
                            kb.recip(rd[u_], den[u_])
                            kb.tt("dve", sc[u_], rd[u_], gates[:, QG * 4:QG * 4 + 4, hd * 3], ALU.mult)
                            kb.tt("dve", oc, obv[:, :, 0:64], sc[u_].v(bc_last(sc[u_].ap, 64)), ALU.mult)
                            iv = imp[:, QG * 4:QG * 4 + 4, :]
                            if h == 0:
                                kb.tt("dve", iv, obv[:, :, 64:128], rd[u_].v(bc_last(rd[u_].ap, 64)), ALU.mult)
                            else:
                                kb.tt("dve", itmp[u_], obv[:, :, 64:128], rd[u_].v(bc_last(rd[u_].ap, 64)), ALU.mult)
                                kb.tt("pool", iv, iv, itmp[u_], ALU.add)
                            kb.dma(OCv[:, QG * 4:QG * 4 + 4, hd * 64:(hd + 1) * 64], oc, sems[5 + u_], pw=True)
                    b4 = bk[4].v(bk[4].ap.bitcast(BF16))
                    for tile in range(NT):
                        j = tile % 2
                        kb.tt("dve", impf[j], imp[:, tile, :], fm[:, tile, :], ALU.add)
                        a, b, c_ = impf[j].ap, m8a[j].ap, imp2[j].ap
                        kb.op("dve", lambda e, a=a, b=b: e.max(out=b, in_=a), [m8a[j]], [impf[j]])
                        kb.op("dve", lambda e, a=a, b=b, c_=c_: e.match_replace(out=c_, in_to_replace=b, in_values=a, imm_value=-3.0e6), [imp2[j]], [impf[j], m8a[j]])
                        d_ = m8b[j].ap
                        kb.op("dve", lambda e, c_=c_, d_=d_: e.max(out=d_, in_=c_), [m8b[j]], [imp2[j]])
                        kb.ts("dve", selm[j][:, 64:128], impf[j], m8b[j][:, 7:8], ALU.is_ge)
                        kb.tr(b4[:, j * 128:(j + 1) * 128], selm[j], self.ident_bf)
                        kb.ts("dve", selT[64:128, g, tile * 128:(tile + 1) * 128], b4[64:128, j * 128:(j + 1) * 128], -1.0, ALU.add, 30000.0, ALU.mult)
                kb.c.barrier()
                kb.c.release(sems)
            if "selT" in self.debug:
                semd = kb.c.dma_sem()
                kb.dma(self.scr["selT"], selT, semd)
                kb.c.barrier()
            with contextlib.ExitStack() as es:
                if getattr(self, "skip_p3b", False):
                    kb.c.release([sem0])
                    return
                ksT = kb.sbn(es, 2, [128, S], BF16, "ksx")
                kwT = kb.sbn(es, 2, [128, S], BF16, "kwT")
                vsa = kb.sbn(es, 2, [128, NT, 65], BF16, "vsa")
                vwa = kb.sbn(es, 2, [128, NT, 65], BF16, "vwa")
                ssel = kb.sbn(es, 4, [128, 2688], BF16, "ssel")
                swin = kb.sbn(es, 4, [128, 1408], BF16, "swin")
                qT4 = kb.sbn(es, 2, [128, 4, 512], BF16, "qsx")
                oct_ = kb.sbn(es, 2, [128, 4, 256], F32, "oct")
                eb = kb.sbn(es, 5, [128, 512], BF16, "eb")
                tb = kb.sbn(es, 5, [128, 512], BF16, "tb")
                pb = kb.sbn(es, 5, [128, 512], BF16, "pb")
                osb = kb.sbn(es, 2, [65, 512], F32, "osb")
                rd4 = kb.sbn(es, 2, [128, 4], F32, "rd4")
                sc4 = kb.sbn(es, 2, [128, 4], F32, "sc4")
                tmp4 = kb.sbn(es, 2, [128, 4, 64], F32, "tmp4")
                sems = [kb.c.dma_sem() for _ in range(12)]
                kb.memset("pool", kwT[0][64:128, :], 0.0)
                kb.memset("pool", kwT[1][64:128, :], 0.0)
                kb.dma(ksT[0][64:128, :], I["exr"], sems[0], pw=True)
                kb.dma(ksT[1][64:128, :], I["exr"], sems[0], pw=True)
                vswv = self.scr["vsw"].v(self.scr["vsw"].ap.rearrange("(t p) c -> p t c", p=128))
                fcnt = [0]
                for g in range(2):
                    sg = g % 2
                    kb.dma(ksT[sg][0:64, :], self.scr["kswT"][0, g], sems[1 + sg], pw=True)
                    kb.dma(kwT[sg][0:64, :], self.scr["kswT"][1, g], sems[1 + sg], pw=True)
                    kb.dma(vsa[sg][:, :, 0:64], vswv[:, :, g * 64:(g + 1) * 64], sems[1 + sg], pw=True)
                    kb.dma(vwa[sg][:, :, 0:64], vswv[:, :, 128 + g * 64:128 + (g + 1) * 64], sems[1 + sg], pw=True)
                    kb.memset("pool", vsa[sg][:, :, 64:65], 1.0)
                    kb.memset("pool", vwa[sg][:, :, 64:65], 1.0)
                    for h in range(4):
                        kb.dma(ssel[h], self.hankel(0, 4 * g + h, 1, 2688), sems[3], pw=True)
                        kb.dma(swin[h], self.hankel(1, 4 * g + h, 1, 1408), sems[3], pw=True)
                    qsrc = self.scr["qaT"].v(self.scr["qaT"].ap[4 * g:4 * g + 4].rearrange("h d s -> d h s"))
                    for QG in range(8):
                        sq = QG % 2
                        qs = slice(QG * 512, (QG + 1) * 512)
                        nK = 4 * QG + 4
                        kb.dma(qT4[sq][0:64, :, :], qsrc[:, :, qs], sems[4 + sq], pw=True)
                        kb.op("pool", (lambda e, o=qT4[sq].ap[64:128, :, :], i=bc_mid(selT.ap[64:128, g, qs], 4): e.tensor_copy(out=o, in_=i)), [], [selT], pw=[qT4[sq]])
                        acc = oct_[sq]
                        kb.dma(acc, OCv[:, QG * 4:QG * 4 + 4, g * 256:(g + 1) * 256], sems[6 + sq])
                        items = []
                        for h in range(4):
                            hd = 4 * g + h
                            for br in range(2):
                                k0 = 0 if br == 0 else max(0, 4 * QG - 4)
                                pvb = bk[4 + br][0:65, :]

                                def fin(h=h, hd=hd, br=br, pvb=pvb, acc=acc, QG=QG):
                                    f = fcnt[0]
                                    fcnt[0] += 1
                                    o = osb[f % 2]
                                    kb.copy("dve", o, pvb)
                                    tbk = bk[6 + f % 2]
                                    for qt in range(4):
                                        kb.tr(tbk[:, qt * 65:qt * 65 + 65], o[:, qt * 128:(qt + 1) * 128], self.ident_f[0:65, 0:65])
                                    ta = tbk.ap
                                    dens = tbk.v(AP(ta.tensor, ta.offset + 64, [list(ta.ap[0]), [65, 4]]))
                                    vals = tbk.v(AP(ta.tensor, ta.offset, [list(ta.ap[0]), [65, 4], [1, 64]]))
                                    r4, s4, tm = rd4[f % 2], sc4[f % 2], tmp4[f % 2]
                                    kb.recip(r4, dens)
                                    kb.tt("dve", s4, r4, gates[:, QG * 4:QG * 4 + 4, hd * 3 + 1 + br], ALU.mult)
                                    kb.tt("dve", tm, vals, s4.v(bc_last(s4.ap, 64)), ALU.mult)
                                    av = acc[:, :, h * 64:(h + 1) * 64]
                                    kb.tt("pool", av, av, tm, ALU.add)
                                for Kt in range(k0, nK):
                                    if br == 0:
                                        c0 = min(4 * QG - Kt + 3, 16) * 128
                                        items.append(dict(kT=ksT[sg][:, Kt * 128:(Kt + 1) * 128], q=qT4[sq][:, h, :], strip=ssel[h][:, c0:c0 + 512],
                                                          mask=None, vaug=vsa[sg][:, Kt, :], pv=pvb, first=(Kt == k0), last=(Kt == nK - 1), fin=fin,
                                                          qa=max(0, Kt - 4 * QG), qb=4))
                                    else:
                                        c0 = (4 * QG - Kt + 3) * 128
                                        items.append(dict(kT=kwT[sg][:, Kt * 128:(Kt + 1) * 128], q=qT4[sq][:, h, :], strip=swin[h][:, c0:c0 + 512],
                                                          mask=None, vaug=vwa[sg][:, Kt, :], pv=pvb, first=(Kt == k0), last=(Kt == nK - 1), fin=fin,
                                                          qa=max(0, Kt - 4 * QG), qb=min(4, Kt - 4 * QG + 5)))
                        self.run_attn(items, eb, tb, pb)
                        kb.dma(Ov[:, QG * 4:QG * 4 + 4, g * 256:(g + 1) * 256], acc, sems[8 + sq], pw=True)
                kb.c.barrier()
                kb.c.release(sems)
            kb.c.release([sem0])

    def phase_dil(self, layer):
        kb = self.kb
        bk = self.banks
        with contextlib.ExitStack() as es:
            kT = kb.sbn(es, 2, [128, S], BF16, "kbT")
            qT = kb.sbn(es, 2, [128, S], BF16, "qbT")
            for t_ in (kT[0], kT[1], qT[0], qT[1]):
                kb.memset("pool", t_[64:128, :], 0.0)
            va = kb.sbn(es, 2, [128, NT, 65], BF16, "vba")
            sd = kb.sbn(es, 2, [128, 2944], BF16, "sdil")
            eb = kb.sbn(es, 5, [128, 512], BF16, "eb")
            pb = kb.sbn(es, 5, [128, 512], BF16, "pb")
            osb = kb.sbn(es, 2, [65, 512], F32, "osb")
            rd4 = kb.sbn(es, 2, [128, 4], F32, "rd4")
            obs = kb.sbn(es, 2, [128, 4, 64], F32, "obs")
            sems = [kb.c.dma_sem() for _ in range(4)]
            vbv = self.scr["vb"].v(self.scr["vb"].ap.rearrange("(t p) c -> p t c", p=128))
            Ov = self.scr["O"].v(self.scr["O"].ap.rearrange("(t p) c -> p t c", p=128))
            fcnt = [0]

            def load(hd):
                s = hd % 2
                kb.dma(kT[s][0:64, :], self.scr["kbT"][hd], sems[s], pw=True)
                kb.dma(qT[s][0:64, :], self.scr["qbT"][hd], sems[s], pw=True)
                kb.dma(va[s][:, :, 0:64], vbv[:, :, hd * 64:(hd + 1) * 64], sems[s], pw=True)
                kb.memset("pool", va[s][:, :, 64:65], 1.0)
                kb.dma(sd[s], self.hankel(2, 8 + hd, 1, 2944), sems[s])
            load(0)
            for hd in range(8):
                s = hd % 2
                if hd + 1 < 8:
                    load(hd + 1)
                items = []
                for QG in range(8):
                    k0 = max(0, 4 * QG - 16)
                    nK = 4 * QG + 4
                    pvb = bk[4 + QG % 2][0:65, :]

                    def fin(hd=hd, QG=QG, pvb=pvb):
                        f = fcnt[0]
                        fcnt[0] += 1
                        o = osb[f % 2]
                        kb.copy("dve", o, pvb)
                        tbk = bk[6 + f % 2]
                        ob = obs[f % 2]
                        for qt in range(4):
                            kb.tr(tbk[:, qt * 65:qt * 65 + 65], o[:, qt * 128:(qt + 1) * 128], self.ident_f[0:65, 0:65])
                        ta = tbk.ap
                        dens = tbk.v(AP(ta.tensor, ta.offset + 64, [list(ta.ap[0]), [65, 4]]))
                        vals = tbk.v(AP(ta.tensor, ta.offset, [list(ta.ap[0]), [65, 4], [1, 64]]))
                        r4 = rd4[f % 2]
                        kb.recip(r4, dens)
                        kb.tt("dve", ob, vals, r4.v(bc_last(r4.ap, 64)), ALU.mult)
                        kb.dma(Ov[:, QG * 4:QG * 4 + 4, 512 + hd * 64:512 + (hd + 1) * 64], ob, sems[2 + f % 2], pw=True)
                    for Kt in range(k0, nK):
                        c0 = (4 * QG - Kt + 3) * 128
                        items.append(dict(kT=kT[s][:, Kt * 128:(Kt + 1) * 128], q=qT[s][:, QG * 512:(QG + 1) * 512], strip=sd[s][:, c0:c0 + 512],
                                          mask=None, vaug=va[s][:, Kt, :], pv=pvb, first=(Kt == k0), last=(Kt == nK - 1), fin=fin,
                                          qa=max(0, Kt - 4 * QG), qb=min(4, Kt - 4 * QG + 17)))
                self.run_attn(items, eb, None, pb)
            kb.c.barrier()
            kb.c.release(sems)


    def phase_ffn(self, layer, hin_dram, hout_dram):
        kb = self.kb
        I = self.inp
        bk = self.banks
        moe = (layer % 2 == 1)
        li = layer // 2
        dff = DFE if moe else DFF
        nfc = dff // 128
        nfb = dff // 256
        with contextlib.ExitStack() as es:
            Wo = kb.sb(es, [128, 8, D], BF16, "Wo")
            gO = kb.sb(es, [128, D], F32, "gO")
            gF = kb.sb(es, [128, D], F32, "gF")
            hp = kb.sb(es, [128, 4, D], F32, "hp")
            hpt = [Tl(hp.ap[:, i, :]) for i in range(4)]
            vT = kb.sb(es, [128, 8, 512], BF16, "vT")
            hid = kb.sb(es, [128, nfc, 512], BF16, "hid")
            hidc = [Tl(hid.ap[:, i, :]) for i in range(nfc)]
            stg = kb.sbn(es, 2, [128, 8, 256], F32, "stg")
            wgu = kb.sbn(es, 4, [128, 8, 256], BF16, "wgu")
            stgd = kb.sbn(es, 2, [128, 4, 512], F32, "stgd")
            wd = kb.sbn(es, 2, [128, 4, 512], BF16, "wd")
            Ot = kb.sbn(es, 2, [128, D], F32, "Ot")
            ht = kb.sbn(es, 2, [128, D], F32, "ht")
            junk = kb.sb(es, [128, D], F32, "junk")
            on = kb.sbn(es, 2, [128, D], BF16, "on")
            onT = kb.sbn(es, 2, [128, 8, 128], BF16, "onT")
            vb_ = kb.sbn(es, 2, [128, D], BF16, "vb_")
            ss2 = kb.sbn(es, 2, [128, 2], F32, "ss2")
            rm2 = kb.sbn(es, 2, [128, 2], F32, "rm2")
            rs2 = kb.sbn(es, 2, [128, 2], F32, "rs2")
            ss1 = kb.sbn(es, 2, [128, 1], F32, "ss1")
            rm1 = kb.sbn(es, 2, [128, 1], F32, "rm1")
            rs1 = kb.sbn(es, 2, [128, 1], F32, "rs1")
            sg = kb.sbn(es, 2, [128, 512], F32, "sg")
            sems = [kb.c.dma_sem() for _ in range(12)]
            if moe:
                v32 = kb.sbn(es, 2, [128, D], F32, "v32")
                v32T = kb.sb(es, [128, 8, 128], F32, "v32T")
                rw = kb.sb(es, [128, 8, NE], F32, "rw")
                gate = kb.sb(es, [128, 4, NE], F32, "gate")
                lg = kb.sbn(es, 2, [128, NE], F32, "lg")
                m8 = kb.sbn(es, 2, [128, 8], F32, "m8")
                msk = kb.sbn(es, 2, [128, NE], F32, "msk")
                nm1 = kb.sbn(es, 2, [128, 1], F32, "nm1")
                ex = kb.sbn(es, 2, [128, NE], F32, "ex")
                gu = kb.sbn(es, 2, [128, NE], F32, "gu")
                dn = kb.sbn(es, 2, [128, 1], F32, "dn")
                rdn = kb.sbn(es, 2, [128, 1], F32, "rdn")
                kb.dma(rw, I["router_w"].v(I["router_w"].ap[li].rearrange("(kc p) e -> p kc e", p=128)), sems[0])
            wov = I["w_out"].v(I["w_out"].ap[layer].rearrange("(kc p) n -> p kc n", p=128))
            for kc in range(8):
                kb.dma(Wo[:, kc, :], wov[:, kc, :], sems[0], q="pool", pw=True)
            kb.dma(gO, Tl(I["out_norm"].ap[layer].partition_broadcast(128), I["out_norm"].buf), sems[0])
            kb.dma(gF, Tl(I["ffn_norm"].ap[layer].partition_broadcast(128), I["ffn_norm"].buf), sems[0])
            kb.c.barrier()
            Ov = self.scr["O"].v(self.scr["O"].ap.rearrange("(t p) c -> t p c", p=128))
            hv = hin_dram.v(hin_dram.ap.rearrange("(t p) d -> t p d", p=128))
            hov = hout_dram.v(hout_dram.ap.rearrange("(t p) d -> p t d", p=128))
            if moe:
                experts = [(I["exp_w_gate"][li, e], I["exp_w_up"][li, e], I["exp_w_down"][li, e], e) for e in range(NE)]
            else:
                experts = [(I["ffn_w_gate"][li], I["ffn_w_up"][li], I["ffn_w_down"][li], None)]
            cnt = {"ld": 0, "ldd": 0, "fc": 0}
            for blk in range(8):
                def tile_body(ti, blk=blk):
                    t = blk * 4 + ti
                    s = t % 2
                    kb.dma(Ot[s], Ov[t], sems[1 + s])
                    kb.dma(ht[s], hv[t], sems[3 + s])
                    o = Ot[s]
                    kb.act(junk[:, 0:512], o[:, 0:512], AF.Square, accum=ss2[s][:, 0:1])
                    kb.act(junk[:, 512:1024], o[:, 512:1024], AF.Square, accum=ss2[s][:, 1:2])
                    kb.act(rm2[s], ss2[s], AF.Sqrt, bias=kb.epsb, scale=1.0 / 512)
                    kb.recip(rs2[s], rm2[s])
                    for gq in range(2):
                        cs = slice(gq * 512, (gq + 1) * 512)
                        kb.stt(on[s][:, cs], o[:, cs], rs2[s][:, gq:gq + 1], gO[:, cs], ALU.mult, ALU.mult)
                    b0 = bk[s].v(bk[s].ap.bitcast(BF16))
                    for kc in range(8):
                        kb.tr(b0[:, kc * 128:(kc + 1) * 128], on[s][:, kc * 128:(kc + 1) * 128], self.ident_bf)
                    kb.copy("act", onT[s], b0.v(b0.ap.rearrange("p (a b) -> p a b", b=128)))
                    for half in range(2):
                        bank = bk[2 + 2 * s + half]
                        for kc in range(8):
                            kb.mm(bank, onT[s][:, kc, :], Wo[:, kc, half * 512:(half + 1) * 512], start=(kc == 0), stop=(kc == 7))
                        kb.tt("dve", hpt[ti][:, half * 512:(half + 1) * 512], bank, ht[s][:, half * 512:(half + 1) * 512], ALU.add)
                    kb.act(junk, hpt[ti], AF.Square, accum=ss1[s])
                    kb.act(rm1[s], ss1[s], AF.Sqrt, bias=kb.epsb, scale=1.0 / D)
                    kb.recip(rs1[s], rm1[s])
                    if moe:
                        kb.stt(v32[s], hpt[ti], rs1[s], gF, ALU.mult, ALU.mult)
                        kb.copy("pool", vb_[s], v32[s])
                    else:
                        kb.stt(vb_[s], hpt[ti], rs1[s], gF, ALU.mult, ALU.mult)
                    for kc in range(8):
                        kb.tr(b0[:, kc * 128:(kc + 1) * 128], vb_[s][:, kc * 128:(kc + 1) * 128], self.ident_bf)
                    kb.copy("act", vT[:, :, ti * 128:(ti + 1) * 128], b0.v(b0.ap.rearrange("p (a b) -> p a b", b=128)))
                    if moe:
                        for kc in range(8):
                            kb.tr(bk[2 + kc // 4][:, (kc % 4) * 128:(kc % 4 + 1) * 128], v32[s][:, kc * 128:(kc + 1) * 128], self.ident_f)
                        kb.copy("dve", v32T[:, 0:4, :], bk[2].v(bk[2].ap.rearrange("p (a b) -> p a b", b=128)))
                        kb.copy("act", v32T[:, 4:8, :], bk[3].v(bk[3].ap.rearrange("p (a b) -> p a b", b=128)))
                        for kc in range(8):
                            kb.mm(bk[1][:, 0:NE], v32T[:, kc, :], rw[:, kc, :], start=(kc == 0), stop=(kc == 7))
                        kb.copy("dve", lg[s], bk[1][:, 0:NE])
                        a_, b_ = lg[s].ap, m8[s].ap
                        kb.op("dve", lambda e, a_=a_, b_=b_: e.max(out=b_, in_=a_), [m8[s]], [lg[s]])
                        kb.ts("dve", msk[s], lg[s], m8[s][:, 1:2], ALU.is_ge)
                        kb.ts("dve", nm1[s], m8[s][:, 0:1], -1.0, ALU.mult)
                        kb.act(ex[s], lg[s], AF.Exp, bias=nm1[s])
                        kb.tt("dve", gu[s], ex[s], msk[s], ALU.mult)
                        kb.reduce(dn[s], gu[s])
                        kb.recip(rdn[s], dn[s])
                        kb.ts("dve", gate[:, ti, :], gu[s], rdn[s], ALU.mult)
                for pa in (0, 2):
                    la = kb.c.record(lambda: tile_body(pa))
                    lb = kb.c.record(lambda: tile_body(pa + 1))
                    kb.c.play(la, lb)
                for (Wg, Wu, Wd, e) in experts:
                    stages = []
                    wgv = Wg.v(Wg.ap.rearrange("(kc p) f -> p kc f", p=128))
                    wuv = Wu.v(Wu.ap.rearrange("(kc p) f -> p kc f", p=128))
                    wdv = Wd.v(Wd.ap.rearrange("(fc p) n -> p fc n", p=128))
                    for fb in range(nfb):
                        def ld(fb=fb, wgv=wgv, wuv=wuv):
                            res = []
                            for src in (wgv, wuv):
                                k = cnt["ld"]
                                cnt["ld"] += 1
                                st = stg[k % 2]
                                w = wgu[k % 4]
                                kb.dma(st, src[:, :, fb * 256:(fb + 1) * 256], sems[5 + k % 2])
                                kb.copy("act" if k % 2 == 0 else "dve", w, st)
                                res.append(w)
                            return res

                        def comp(ws, fb=fb):
                            wg_, wu_ = ws
                            for j in range(2):
                                fc = fb * 2 + j
                                k = cnt["fc"]
                                cnt["fc"] += 1
                                gb, ub = bk[(k % 2) * 2], bk[(k % 2) * 2 + 1]
                                for kc in range(8):
                                    kb.mm(gb, wg_[:, kc, j * 128:(j + 1) * 128], vT[:, kc, :], start=(kc == 0), stop=(kc == 7))
                                for kc in range(8):
                                    kb.mm(ub, wu_[:, kc, j * 128:(j + 1) * 128], vT[:, kc, :], start=(kc == 0), stop=(kc == 7))
                                kb.act(sg[k % 2], gb, AF.Silu)
                                kb.tt("dve", hidc[fc], sg[k % 2], ub, ALU.mult)
                        stages.append((ld, comp))
                    ngr = (nfc + 3) // 4
                    for half in range(2):
                        for fg in range(ngr):
                            f0 = fg * 4
                            nf = min(4, nfc - f0)

                            def ld(half=half, f0=f0, nf=nf, wdv=wdv):
                                k = cnt["ldd"]
                                cnt["ldd"] += 1
                                st = stgd[k % 2]
                                w = wd[k % 2]
                                kb.dma(st[:, 0:nf, :], wdv[:, f0:f0 + nf, half * 512:(half + 1) * 512], sems[7 + k % 2])
                                kb.copy("act" if k % 2 == 0 else "dve", w[:, 0:nf, :], st[:, 0:nf, :])
                                return w

                            def comp(w, half=half, f0=f0, nf=nf, fg=fg, e=e):
                                for j in range(nf):
                                    fc = f0 + j
                                    for ti in range(4):
                                        kb.mm(bk[4 + ti], hidc[fc][:, ti * 128:(ti + 1) * 128], w[:, j, :], start=(fc == 0), stop=(fc == nfc - 1))
                                if fg == ngr - 1:
                                    for ti in range(4):
                                        dst = hpt[ti][:, half * 512:(half + 1) * 512]
                                        if e is None:
                                            kb.tt("dve", dst, bk[4 + ti], dst, ALU.add)
                                        else:
                                            kb.stt(dst, bk[4 + ti], gate[:, ti, e:e + 1], dst, ALU.mult, ALU.add)
                            stages.append((ld, comp))
                    nxt = stages[0][0]()
                    for i in range(len(stages)):
                        cur = nxt
                        if i + 1 < len(stages):
                            nxt = stages[i + 1][0]()
                        stages[i][1](cur)
                for ti in range(4):
                    kb.dma(hov[:, blk * 4 + ti, :], hpt[ti], sems[9], pw=True)
            kb.c.barrier()
            kb.c.release(sems)

    def phase_moe(self, layer, hin_dram, hout_dram):
        kb = self.kb
        c = kb.c
        I = self.inp
        bk = self.banks
        li = layer // 2
        dff = DFE
        nfc = dff // 128
        nfb = dff // 256
        XE = self.scr["XE"]
        YE = self.scr["YE"]
        HP = self.scr["HP"]
        with contextlib.ExitStack() as es0:
            idx_all = kb.sb(es0, [128, NT, 2], I32, "idx_all")
            g_all = kb.sb(es0, [128, NT, 2], F32, "g_all")
            run = kb.sb(es0, [128, NE], F32, "run")
            flags_i = kb.sb(es0, [128, NE * 8], I32, "flags_i")
            HPv = HP.v(HP.ap.rearrange("(t p) d -> t p d", p=128))
            with contextlib.ExitStack() as es:
                Wo = kb.sb(es, [128, 8, D], BF16, "Wo")
                gO = kb.sb(es, [128, D], F32, "gO")
                gF = kb.sb(es, [128, D], F32, "gF")
                ecst = kb.sb(es, [128, NE], F32, "ecst")
                utri = kb.sb(es, [128, 128], BF16, "utri")
                ones_bf = kb.sb(es, [128, 128], BF16, "ones_bf")
                jthr = kb.sb(es, [128, NE * 8], F32, "jthr")
                rw = kb.sb(es, [128, 8, NE], F32, "rw")
                Ot = kb.sbn(es, 2, [128, D], F32, "Ot")
                ht = kb.sbn(es, 2, [128, D], F32, "ht")
                hpt = kb.sbn(es, 2, [128, D], F32, "hpt")
                junk = kb.sb(es, [128, D], F32, "junk")
                on = kb.sbn(es, 2, [128, D], BF16, "on")
                onT = kb.sbn(es, 2, [128, 8, 128], BF16, "onT")
                vb_ = kb.sbn(es, 2, [128, D], BF16, "vb_")
                v32 = kb.sbn(es, 2, [128, D], F32, "v32")
                v32T = kb.sbn(es, 2, [128, 8, 128], F32, "v32T")
                ss2 = kb.sbn(es, 2, [128, 2], F32, "ss2")
                rm2 = kb.sbn(es, 2, [128, 2], F32, "rm2")
                rs2 = kb.sbn(es, 2, [128, 2], F32, "rs2")
                ss1 = kb.sbn(es, 2, [128, 1], F32, "ss1")
                rm1 = kb.sbn(es, 2, [128, 1], F32, "rm1")
                rs1 = kb.sbn(es, 2, [128, 1], F32, "rs1")
                lg = kb.sbn(es, 2, [128, NE], F32, "lg")
                m8 = kb.sbn(es, 2, [128, 8], F32, "m8")
                msk = kb.sbn(es, 2, [128, NE], F32, "msk")
                mskb = kb.sbn(es, 2, [128, NE], BF16, "mskb")
                nm1 = kb.sbn(es, 2, [128, 1], F32, "nm1")
                ex = kb.sbn(es, 2, [128, NE], F32, "ex")
                gu = kb.sbn(es, 2, [128, NE], F32, "gu")
                gate = kb.sbn(es, 2, [128, NE], F32, "gate")
                dn = kb.sbn(es, 2, [128, 1], F32, "dn")
                rdn = kb.sbn(es, 2, [128, 1], F32, "rdn")
                flat = kb.sbn(es, 2, [128, NE], F32, "flat")
                A = kb.sbn(es, 2, [128, NE], F32, "A")
                Bm = kb.sbn(es, 2, [128, NE], F32, "Bm")
                eq = kb.sbn(es, 2, [128, NE], F32, "eq")
                f2 = kb.sbn(es, 2, [128, 2], F32, "f2")
                amax = kb.sbn(es, 2, [128, 1], F32, "amax")
                bmax = kb.sbn(es, 2, [128, 1], F32, "bmax")
                nrep = kb.sb(es, [128, NE * 8], F32, "nrep")
                flags_f = kb.sb(es, [128, NE * 8], F32, "flags_f")
                sems = [kb.c.dma_sem() for _ in range(10)]
                wov = I["w_out"].v(I["w_out"].ap[layer].rearrange("(kc p) n -> p kc n", p=128))
                for kc in range(8):
                    kb.dma(Wo[:, kc, :], wov[:, kc, :], sems[0], q="pool", pw=True)
                kb.dma(gO, Tl(I["out_norm"].ap[layer].partition_broadcast(128), I["out_norm"].buf), sems[0])
                kb.dma(gF, Tl(I["ffn_norm"].ap[layer].partition_broadcast(128), I["ffn_norm"].buf), sems[0])
                kb.dma(rw, I["router_w"].v(I["router_w"].ap[li].rearrange("(kc p) e -> p kc e", p=128)), sems[0])
                kb.dma(ecst, I["ecst"], sems[0])
                kb.dma(utri, I["utri"], sems[0])
                kb.dma(ones_bf, I["ones_bf"], sems[0])
                kb.dma(jthr, I["jthr"], sems[0])
                kb.memset("dve", run, 0.0)
                kb.c.barrier()
                Ov = self.scr["O"].v(self.scr["O"].ap.rearrange("(t p) c -> t p c", p=128))
                hv = hin_dram.v(hin_dram.ap.rearrange("(t p) d -> t p d", p=128))
                def part1(t):
                    s = t % 2
                    kb.dma(Ot[s], Ov[t], sems[1 + s])
                    kb.dma(ht[s], hv[t], sems[3 + s])
                    o = Ot[s]
                    kb.act(junk[:, 0:512], o[:, 0:512], AF.Square, accum=ss2[s][:, 0:1])
                    kb.act(junk[:, 512:1024], o[:, 512:1024], AF.Square, accum=ss2[s][:, 1:2])
                    kb.act(rm2[s], ss2[s], AF.Sqrt, bias=kb.epsb, scale=1.0 / 512)
                    kb.recip(rs2[s], rm2[s])
                    for gq in range(2):
                        cs = slice(gq * 512, (gq + 1) * 512)
                        kb.stt(on[s][:, cs], o[:, cs], rs2[s][:, gq:gq + 1], gO[:, cs], ALU.mult, ALU.mult)
                    b0 = bk[s].v(bk[s].ap.bitcast(BF16))
                    for kc in range(8):
                        kb.tr(b0[:, kc * 128:(kc + 1) * 128], on[s][:, kc * 128:(kc + 1) * 128], self.ident_bf)
                    kb.copy("act", onT[s], b0.v(b0.ap.rearrange("p (a b) -> p a b", b=128)))
                    hp = hpt[s]
                    for half in range(2):
                        bank = bk[2 + s]
                        for kc in range(8):
                            kb.mm(bank, onT[s][:, kc, :], Wo[:, kc, half * 512:(half + 1) * 512], start=(kc == 0), stop=(kc == 7))
                        kb.tt("dve", hp[:, half * 512:(half + 1) * 512], bank, ht[s][:, half * 512:(half + 1) * 512], ALU.add)
                    kb.dma(HPv[t], hp, sems[5 + s])
                    kb.act(junk, hp, AF.Square, accum=ss1[s])
                    kb.act(rm1[s], ss1[s], AF.Sqrt, bias=kb.epsb, scale=1.0 / D)
                    kb.recip(rs1[s], rm1[s])
                    kb.stt(v32[s], hp, rs1[s], gF, ALU.mult, ALU.mult)
                    kb.copy("pool", vb_[s], v32[s])
                    for kc in range(8):
                        kb.tr(bk[4 + s][:, (kc % 4) * 128:(kc % 4 + 1) * 128], v32[s][:, kc * 128:(kc + 1) * 128], self.ident_f)
                        if kc % 4 == 3:
                            kb.copy("dve" if kc == 3 else "act", v32T[s][:, kc - 3:kc + 1, :], bk[4 + s].v(bk[4 + s].ap.rearrange("p (a b) -> p a b", b=128)))
                    for kc in range(8):
                        kb.mm(bk[6 + s][:, 0:NE], v32T[s][:, kc, :], rw[:, kc, :], start=(kc == 0), stop=(kc == 7))
                    kb.copy("dve", lg[s], bk[6 + s][:, 0:NE])
                    a_, b_ = lg[s].ap, m8[s].ap
                    kb.op("dve", lambda e, a_=a_, b_=b_: e.max(out=b_, in_=a_), [m8[s]], [lg[s]])
                    kb.ts("dve", msk[s], lg[s], m8[s][:, 1:2], ALU.is_ge)
                    kb.copy("dve", mskb[s], msk[s])
                    kb.ts("dve", nm1[s], m8[s][:, 0:1], -1.0, ALU.mult)
                    kb.act(ex[s], lg[s], AF.Exp, bias=nm1[s])
                    kb.tt("dve", gu[s], ex[s], msk[s], ALU.mult)
                    kb.reduce(dn[s], gu[s])
                    kb.recip(rdn[s], dn[s])
                    kb.ts("dve", gate[s], gu[s], rdn[s], ALU.mult)
                def part2(t):
                    s = t % 2
                    kb.mm(bk[6][:, 0:NE], utri, mskb[s])
                    kb.mm(bk[7][:, 0:NE], ones_bf, mskb[s])
                    kb.tt("dve", flat[s], bk[6][:, 0:NE], run, ALU.add)
                    kb.tt("dve", run, bk[7][:, 0:NE], run, ALU.add)
                    kb.tt("dve", flat[s], flat[s], ecst, ALU.add)
                    kb.stt(A[s], flat[s], 1.0, msk[s], ALU.add, ALU.mult)
                    kb.reduce(amax[s], A[s], op=ALU.max)
                    kb.ts("dve", f2[s][:, 0:1], amax[s], -1.0, ALU.add)
                    kb.ts("dve", Bm[s], flat[s], -1.0, ALU.mult, 40000.0, ALU.add)
                    kb.tt("dve", Bm[s], Bm[s], msk[s], ALU.mult)
                    kb.reduce(bmax[s], Bm[s], op=ALU.max)
                    kb.ts("dve", f2[s][:, 1:2], bmax[s], -1.0, ALU.mult, 40000.0, ALU.add)
                    kb.copy("dve", idx_all[:, t, :], f2[s])
                    kb.ts("dve", eq[s], A[s], amax[s], ALU.is_equal)
                    kb.tt("dve", eq[s], eq[s], gate[s], ALU.mult)
                    kb.reduce(g_all[:, t, 0:1], eq[s])
                    kb.ts("dve", g_all[:, t, 1:2], g_all[:, t, 0:1], -1.0, ALU.mult, 1.0, ALU.add)
                    for k in range(2):
                        xo, ia, va = XE.ap, idx_all.ap[:, t, k:k + 1], vb_[s].ap
                        c.emit("pool", lambda e, xo=xo, ia=ia, va=va: e.indirect_dma_start(
                            out=xo, out_offset=bass.IndirectOffsetOnAxis(ap=ia, axis=0), in_=va, in_offset=None),
                            reads=[idx_all.buf, vb_[s].buf], pwrites=[XE.buf], dsem=sems[7 + s])
                for t in range(0, NT, 2):
                    la = kb.c.record(lambda: part1(t))
                    lb = kb.c.record(lambda: part1(t + 1))
                    kb.c.play(la, lb)
                    part2(t)
                    part2(t + 1)
                kb.copy("dve", nrep.v(nrep.ap.rearrange("p (e j) -> p e j", j=8)), run.v(bc_last(run.ap, 8)))
                kb.tt("dve", flags_f, nrep, jthr, ALU.is_gt)
                kb.copy("dve", flags_i, flags_f)
                kb.c.barrier()
                kb.c.release(sems)
            with contextlib.ExitStack() as es:
                xt_ = kb.sbn(es, 4, [128, D], BF16, "xt_")
                XT = kb.sb(es, [128, 8, 512], BF16, "XT")
                hid = kb.sb(es, [128, nfc, 512], BF16, "hid")
                hidc = [Tl(hid.ap[:, i, :]) for i in range(nfc)]
                stg = kb.sbn(es, 2, [128, 8, 256], F32, "stg")
                wgu = kb.sbn(es, 4, [128, 8, 256], BF16, "wgu")
                stgd = kb.sbn(es, 2, [128, 4, 512], F32, "stgd")
                wd = kb.sbn(es, 2, [128, 4, 512], BF16, "wd")
                sg = kb.sbn(es, 2, [128, 512], F32, "sg")
                yst = kb.sbn(es, 2, [128, 4, 512], F32, "yst")
                sems = [kb.c.dma_sem() for _ in range(8)]
                cnt = {"ld": 0, "ldd": 0, "fc": 0, "x": 0, "y": 0}
                XEv = XE.v(XE.ap.rearrange("(t p) d -> t p d", p=128))
                YEv = YE.v(YE.ap.rearrange("(t p) d -> p t d", p=128))
                for e in range(NE):
                    Wg, Wu, Wd = I["exp_w_gate"][li, e], I["exp_w_up"][li, e], I["exp_w_down"][li, e]
                    wgv = Wg.v(Wg.ap.rearrange("(kc p) f -> p kc f", p=128))
                    wuv = Wu.v(Wu.ap.rearrange("(kc p) f -> p kc f", p=128))
                    wdv = Wd.v(Wd.ap.rearrange("(fc p) n -> p fc n", p=128))
                    for j in range(8):
                        c.cond_begin(flags_i.ap[0:1, e * 8 + j:e * 8 + j + 1], flags_i.buf)
                        t0 = (e * S + j * 512) // 128
                        b0 = bk[0].v(bk[0].ap.bitcast(BF16))
                        for ti in range(4):
                            kb.dma(xt_[ti], XEv[t0 + ti], sems[ti % 2])
                        for ti in range(4):
                            bt = bk[ti % 2].v(bk[ti % 2].ap.bitcast(BF16))
                            for kc in range(8):
                                kb.tr(bt[:, kc * 128:(kc + 1) * 128], xt_[ti][:, kc * 128:(kc + 1) * 128], self.ident_bf)
                            kb.copy("act" if ti % 2 == 0 else "dve", XT[:, :, ti * 128:(ti + 1) * 128], bt.v(bt.ap.rearrange("p (a b) -> p a b", b=128)))
                        stages = []
                        for fb in range(nfb):
                            def ld(fb=fb, wgv=wgv, wuv=wuv):
                                res = []
                                for src in (wgv, wuv):
                                    k = cnt["ld"]
                                    cnt["ld"] += 1
                                    st = stg[k % 2]
                                    w = wgu[k % 4]
                                    kb.dma(st, src[:, :, fb * 256:(fb + 1) * 256], sems[2 + k % 2])
                                    kb.copy("act" if k % 2 == 0 else "dve", w, st)
                                    res.append(w)
                                return res

                            def comp(ws, fb=fb):
                                wg_, wu_ = ws
                                for jj in range(2):
                                    fc = fb * 2 + jj
                                    k = cnt["fc"]
                                    cnt["fc"] += 1
                                    gb, ub = bk[(k % 2) * 2], bk[(k % 2) * 2 + 1]
                                    for kc in range(8):
                                        kb.mm(gb, wg_[:, kc, jj * 128:(jj + 1) * 128], XT[:, kc, :], start=(kc == 0), stop=(kc == 7))
                                    for kc in range(8):
                                        kb.mm(ub, wu_[:, kc, jj * 128:(jj + 1) * 128], XT[:, kc, :], start=(kc == 0), stop=(kc == 7))
                                    kb.act(sg[k % 2], gb, AF.Silu)
                                    kb.tt("dve", hidc[fc], sg[k % 2], ub, ALU.mult)
                            stages.append((ld, comp))
                        ngr = (nfc + 3) // 4
                        for half in range(2):
                            for fg in range(ngr):
                                f0 = fg * 4
                                nf = min(4, nfc - f0)

                                def ld(half=half, f0=f0, nf=nf, wdv=wdv):
                                    k = cnt["ldd"]
                                    cnt["ldd"] += 1
                                    st = stgd[k % 2]
                                    w = wd[k % 2]
                                    kb.dma(st[:, 0:nf, :], wdv[:, f0:f0 + nf, half * 512:(half + 1) * 512], sems[4 + k % 2])
                                    kb.copy("act" if k % 2 == 0 else "dve", w[:, 0:nf, :], st[:, 0:nf, :])
                                    return w

                                def comp(w, half=half, f0=f0, nf=nf, fg=fg, t0=t0):
                                    for jj in range(nf):
                                        fc = f0 + jj
                                        for ti in range(4):
                                            kb.mm(bk[4 + ti], hidc[fc][:, ti * 128:(ti + 1) * 128], w[:, jj, :], start=(fc == 0), stop=(fc == nfc - 1))
                                    if fg == ngr - 1:
                                        k = cnt["y"]
                                        cnt["y"] += 1
                                        y = yst[k % 2]
                                        for ti in range(4):
                                            kb.copy("act" if ti % 2 == 0 else "dve", y[:, ti, :], bk[4 + ti])
                                        kb.dma(YEv[:, t0:t0 + 4, half * 512:(half + 1) * 512], y, sems[6 + k % 2], pw=True)
                                stages.append((ld, comp))
                        nxt = stages[0][0]()
                        for i in range(len(stages)):
                            cur = nxt
                            if i + 1 < len(stages):
                                nxt = stages[i + 1][0]()
                            stages[i][1](cur)
                        c.cond_end()
                kb.c.barrier()
                kb.c.release(sems)
            with contextlib.ExitStack() as es:
                hpb = kb.sbn(es, 2, [128, D], F32, "hpb")
                yh = kb.sbn(es, 2, [128, D], F32, "yh")
                yl = kb.sbn(es, 2, [128, D], F32, "yl")
                ob = kb.sbn(es, 2, [128, D], F32, "ob")
                sems = [kb.c.dma_sem() for _ in range(8)]
                hov = hout_dram.v(hout_dram.ap.rearrange("(t p) d -> t p d", p=128))
                for t in range(NT):
                    s = t % 2
                    kb.dma(hpb[s], HPv[t], sems[s])
                    for k, dst in enumerate((yh[s], yl[s])):
                        yi, ia, da = YE.ap, idx_all.ap[:, t, k:k + 1], dst.ap
                        c.emit("pool", lambda e, yi=yi, ia=ia, da=da: e.indirect_dma_start(
                            out=da, out_offset=None, in_=yi, in_offset=bass.IndirectOffsetOnAxis(ap=ia, axis=0)),
                            reads=[idx_all.buf, YE.buf], writes=[dst.buf], dsem=sems[2 + 2 * k + s])
                    kb.stt(ob[s], yh[s], g_all[:, t, 0:1], hpb[s], ALU.mult, ALU.add)
                    kb.stt(ob[s], yl[s], g_all[:, t, 1:2], ob[s], ALU.mult, ALU.add)
                    kb.dma(hov[t], ob[s], sems[6 + s], pw=True)
                kb.c.barrier()
                kb.c.release(sems)

    def build_all(self):
        self.declare()
        self.setup_globals()
        self.phase_tables()
        hin = self.inp["x"]
        for layer in range(DEPTH):
            hout = self.scr["H1"] if layer == 0 else self.out
            self.phase_inproj(layer, hin)
            self.phase_compress(layer)
            self.phase_nsa(layer)
            self.phase_dil(layer)
            if layer % 2 == 1:
                self.phase_moe(layer, hin, hout)
            else:
                self.phase_ffn(layer, hin, hout)
            hin = hout
        self.kb.c.barrier()
        self.kb.c.flush()
        return self.nc


INPUT_NAMES = ["x", "rel_bias", "attn_norm", "w_in", "nsa_q_norm", "nsa_k_norm", "cmp_pos", "cmp_w1", "cmp_b1", "cmp_w2",
               "dil_q_norm", "dil_k_norm", "out_norm", "w_out", "ffn_norm", "ffn_w_gate", "ffn_w_up", "ffn_w_down",
               "router_w", "exp_w_gate", "exp_w_up", "exp_w_down"]


def make_in_maps(inputs, cores):
    cst = host_consts()
    maps = []
    shared = {k: np.ascontiguousarray(np.asarray(inputs[k], dtype=np.float32)) for k in INPUT_NAMES if k != "x"}
    x = np.asarray(inputs["x"], dtype=np.float32)
    for b in cores:
        m = dict(shared)
        m["x"] = np.ascontiguousarray(x[b])
        m.update(cst)
        maps.append(m)
    return maps


_CACHE = {}


def kernel(**inputs):
    cores = list(range(8))
    if "nc" not in _CACHE:
        _CACHE["nc"] = Prog().build_all()
    nc = _CACHE["nc"]
    maps = make_in_maps(inputs, cores)
    res = run_bass_kernel_spmd(nc, maps, core_ids=cores)
    out = np.stack([np.asarray(r["out"], dtype=np.float32) for r in res.results], axis=0)
    return out
```

```python
import contextlib
import numpy as np
import ml_dtypes
import concourse.bass as bass
import concourse.mybir as mybir
from concourse.ap import AP
from concourse.bass_utils import run_bass_kernel_spmd

F32 = mybir.dt.float32
BF16 = mybir.dt.bfloat16
I32 = mybir.dt.int32
AF = mybir.ActivationFunctionType
ALU = mybir.AluOpType
AX = mybir.AxisListType

S = 4096
D = 1024
NT = 32
DEPTH = 2
INW = 2840
DFF = 2816
DFE = 3584
NE = 8
EPS = 1e-6
XA = 3072
XC = 6144
C_QA, C_KC, C_VC, C_KS, C_VS, C_KW, C_VW, C_GA, C_QB, C_KB, C_VB = 0, 512, 640, 768, 896, 1024, 1152, 1280, 1304, 1816, 2328


class Buf:
    __slots__ = ("name", "writers", "readers")

    def __init__(self, name=""):
        self.name = name
        self.writers = []
        self.readers = []


class Tl:
    __slots__ = ("ap", "buf", "psum")

    def __init__(self, ap, buf=None, psum=False):
        self.ap = ap
        self.buf = buf if buf is not None else Buf()
        self.psum = psum

    def __getitem__(self, k):
        return Tl(self.ap[k], self.buf, self.psum)

    def v(self, ap):
        return Tl(ap, self.buf, self.psum)


class Ctx:
    COMPUTE = ("pe", "act", "dve", "pool")

    def __init__(self, nc):
        self.nc = nc
        self.ops = {e: [] for e in ("pe", "act", "dve", "pool", "sp")}
        self.sems = {}
        self.count = {}
        self.known = {e: {} for e in self.ops}
        for e in self.COMPUTE:
            self.sems[e] = nc.alloc_semaphore(name=f"sem_{e}")
            self.count[e] = 0
        self.free_dsems = []
        self.nsem = 0
        self.region = None
        self.rec = None

    def dma_sem(self):
        if self.free_dsems:
            return self.free_dsems.pop()
        self.nsem += 1
        k = f"dma{self.nsem}"
        self.sems[k] = self.nc.alloc_semaphore(name=k)
        self.count[k] = 0
        return k

    def release(self, ks):
        self.free_dsems.extend(ks)

    def record(self, body):
        assert self.rec is None
        self.rec = []
        body()
        r, self.rec = self.rec, None
        return r

    def play(self, *lists):
        n = max(len(l) for l in lists)
        for i in range(n):
            for l in lists:
                if i < len(l):
                    self.emit(*l[i])

    def emit(self, eng, fn, reads=(), writes=(), pwrites=(), dsem=None):
        if self.rec is not None:
            self.rec.append((eng, fn, list(reads), list(writes), list(pwrites), dsem))
            return None
        deps = {}

        def add(ev, kind):
            sk, v = ev
            if sk == eng:
                if eng == "pe" or kind != "raw":
                    return
            if deps.get(sk, 0) < v:
                deps[sk] = v

        for b in reads:
            for ev in b.writers:
                add(ev, "raw")
        for b in writes:
            for ev in b.writers:
                add(ev, "waw")
            for ev in b.readers:
                add(ev, "war")
        for b in pwrites:
            for ev in b.readers:
                add(ev, "war")
        waits = []
        kn = self.known[eng]
        for sk, v in deps.items():
            if sk not in self.COMPUTE:
                v = self.count[sk]
            if kn.get(sk, 0) < v:
                kn[sk] = v
                waits.append((sk, v))
        if dsem is not None:
            self.count[dsem] += 16
            ev = (dsem, self.count[dsem])
            inc = (dsem, 16)
        else:
            self.count[eng] += 1
            ev = (eng, self.count[eng])
            inc = (eng, 1)
        self.ops[eng].append((waits, fn, inc))
        if self.region is not None and dsem is not None:
            self.region["dq"].setdefault((eng, dsem), 0)
            self.region["dq"][(eng, dsem)] += 16
        for b in reads:
            b.readers.append(ev)
            if len(b.readers) > 48:
                b.readers = self._compact(b.readers)
        for b in writes:
            b.writers = [ev]
            b.readers = []
        for b in pwrites:
            b.writers.append(ev)
            if len(b.writers) > 48:
                b.writers = self._compact(b.writers)
        return ev

    @staticmethod
    def _compact(evs):
        d = {}
        for sk, v in evs:
            if d.get(sk, 0) < v:
                d[sk] = v
        return list(d.items())

    def barrier(self):
        for eng in self.ops:
            waits = []
            kn = self.known[eng]
            for sk, v in self.count.items():
                if v > 0 and sk != eng and kn.get(sk, 0) < v:
                    kn[sk] = v
                    waits.append((sk, v))
            if waits:
                self.ops[eng].append((waits, None, None))

    def cond_begin(self, flag_ap, flag_buf):
        assert self.region is None
        self.region = {"start": dict(self.count), "known": {e: dict(k) for e, k in self.known.items()}, "dq": {}}
        for eng in self.ops:
            waits = []
            kn = self.known[eng]
            for sk, v in flag_buf.writers:
                if sk not in self.COMPUTE:
                    v = self.count[sk]
                if kn.get(sk, 0) < v:
                    kn[sk] = v
                    waits.append((sk, v))
            self.ops[eng].append(("begin", waits, flag_ap))

    def cond_end(self):
        r = self.region
        self.region = None
        for eng in self.ops:
            fix = []
            if eng in self.COMPUTE:
                n = self.count[eng] - r["start"][eng]
                if n > 0:
                    fix.append((eng, r["start"][eng], n))
            for (q, dsem), n in r["dq"].items():
                if q == eng:
                    fix.append((dsem, r["start"].get(dsem, 0), n))
            self.ops[eng].append(("end", fix, None))
            self.known[eng] = r["known"][eng]

    def flush(self):
        nc = self.nc
        sems = self.sems
        with nc.Block() as block:
            def mk(ename):
                def run(engine):
                    ops = self.ops[ename]
                    stack = []
                    i = 0
                    while i < len(ops):
                        a, b, c3 = ops[i]
                        if isinstance(a, str) and a == "begin":
                            for sk, v in b:
                                engine.wait_ge(sems[sk], v)
                            if isinstance(ops[i + 1][0], str) and ops[i + 1][0] == "end" and not ops[i + 1][1]:
                                i += 2
                                continue
                            val = engine.value_load(c3)
                            guard = engine.If(val)
                            guard.__enter__()
                            stack.append((guard, val))
                        elif isinstance(a, str) and a == "end":
                            guard, val = stack.pop()
                            guard.__exit__(None, None, None)
                            with engine.Else():
                                for sk, prior, n in b:
                                    if prior > 0:
                                        engine.wait_ge(sems[sk], prior)
                                    engine.sem_inc(sems[sk], n)
                            engine.free_register(val.val)
                        else:
                            for sk, v in a:
                                engine.wait_ge(sems[sk], v)
                            if b is not None:
                                b(engine).then_inc(sems[c3[0]], c3[1])
                        i += 1
                return run
            block.tensor(mk("pe"))
            block.scalar(mk("act"))
            block.vector(mk("dve"))
            block.gpsimd(mk("pool"))
            block.sync(mk("sp"))


def bc_last(ap, n):
    return AP(ap.tensor, ap.offset, [list(x) for x in ap.ap] + [[0, n]])


def bc_mid(ap, nh):
    a = [list(x) for x in ap.ap]
    return AP(ap.tensor, ap.offset, [a[0], [0, nh]] + a[1:])


class KB:
    def __init__(self, nc):
        self.nc = nc
        self.c = Ctx(nc)
        self.uid = 0

    def name(self, p):
        self.uid += 1
        return f"{p}_{self.uid}"

    def sb(self, es, shape, dt, name="t"):
        h = es.enter_context(self.nc.sbuf_tensor(self.name(name), list(shape), dt))
        return Tl(h.ap())

    def sbn(self, es, n, shape, dt, name="t"):
        return [self.sb(es, shape, dt, name) for _ in range(n)]

    def _rw(self, outs, ins):
        reads, writes = [], []
        for t in ins:
            if t is None:
                continue
            (writes if t.psum else reads).append(t.buf)
        for t in outs:
            writes.append(t.buf)
        return reads, writes

    def op(self, eng, fn, outs, ins, pw=()):
        reads, writes = self._rw(outs, ins)
        pwb = [t.buf for t in pw]
        return self.c.emit(eng, fn, reads=reads, writes=writes, pwrites=pwb)

    def dma(self, out, in_, sem, q="sp", pw=False):
        o, i = out.ap, in_.ap
        reads = [in_.buf]
        if pw:
            return self.c.emit(q, lambda e: e.dma_start(out=o, in_=i), reads=reads, pwrites=[out.buf], dsem=sem)
        return self.c.emit(q, lambda e: e.dma_start(out=o, in_=i), reads=reads, writes=[out.buf], dsem=sem)

    def mm(self, out, lhsT, rhs, start=True, stop=True):
        o, l, r = out.ap, lhsT.ap, rhs.ap
        return self.op("pe", lambda e: e.matmul(o, lhsT=l, rhs=r, start=start, stop=stop, skip_group_check=True), [out], [lhsT, rhs])

    def tr(self, out, in_, ident):
        o, i, d = out.ap, in_.ap, ident.ap
        return self.op("pe", lambda e: e.transpose(out=o, in_=i, identity=d), [out], [in_, ident])

    def act(self, out, in_, func, bias=None, scale=None, accum=None, eng="act"):
        o, i = out.ap, in_.ap
        kw = {}
        ins = [in_]
        if bias is not None:
            if isinstance(bias, Tl):
                kw["bias"] = bias.ap
                ins.append(bias)
            else:
                kw["bias"] = bias
        if scale is not None:
            if isinstance(scale, Tl):
                kw["scale"] = scale.ap
                ins.append(scale)
            else:
                kw["scale"] = scale
        outs = [out]
        if accum is not None:
            kw["accum_out"] = accum.ap
            outs.append(accum)
        return self.op("act", lambda e: e.activation(out=o, in_=i, func=func, **kw), outs, ins)

    def tt(self, eng, out, in0, in1, op):
        o, a, b = out.ap, in0.ap, in1.ap
        return self.op(eng, lambda e: e.tensor_tensor(out=o, in0=a, in1=b, op=op), [out], [in0, in1])

    def ts(self, eng, out, in0, s1, op0, s2=None, op1=None):
        o, a = out.ap, in0.ap
        ins = [in0]
        v1 = s1
        if isinstance(s1, Tl):
            ins.append(s1)
            v1 = s1.ap
        v2 = s2
        if isinstance(s2, Tl):
            ins.append(s2)
            v2 = s2.ap
        if op1 is None:
            return self.op(eng, lambda e: e.tensor_scalar(out=o, in0=a, scalar1=v1, scalar2=None, op0=op0), [out], ins)
        return self.op(eng, lambda e: e.tensor_scalar(out=o, in0=a, scalar1=v1, scalar2=v2, op0=op0, op1=op1), [out], ins)

    def stt(self, out, in0, scalar, in1, op0, op1):
        o, a, b = out.ap, in0.ap, in1.ap
        ins = [in0, in1]
        sv = scalar
        if isinstance(scalar, Tl):
            ins.append(scalar)
            sv = scalar.ap
        return self.op("dve", lambda e: e.scalar_tensor_tensor(out=o, in0=a, scalar=sv, in1=b, op0=op0, op1=op1), [out], ins)

    def copy(self, eng, out, in_):
        o, i = out.ap, in_.ap
        if eng == "act":
            return self.op("act", lambda e: e.copy(out=o, in_=i), [out], [in_])
        return self.op(eng, lambda e: e.tensor_copy(out=o, in_=i), [out], [in_])

    def memset(self, eng, out, val):
        o = out.ap
        return self.op(eng, lambda e: e.memset(o, val), [out], [])

    def recip(self, out, in_):
        o, i = out.ap, in_.ap
        return self.op("dve", lambda e: e.reciprocal(out=o, in_=i), [out], [in_])

    def reduce(self, out, in_, op=ALU.add):
        o, i = out.ap, in_.ap
        return self.op("dve", lambda e: e.tensor_reduce(out=o, in_=i, axis=AX.X, op=op), [out], [in_])

    def rstd(self, es_tmp, out, ssq, n, tmp):
        self.act(tmp, ssq, AF.Sqrt, bias=self.epsb[0:ssq.ap.shape[0], :], scale=1.0 / n)
        self.recip(out, tmp)


def t5_bucket_np(dist):
    dist = np.maximum(dist, 0)
    max_exact = 16
    scaled = np.log(np.maximum(dist, 1).astype(np.float32) / np.float32(max_exact)) / np.float32(np.log(2048 / 16))
    large = np.minimum(max_exact + (scaled.astype(np.float32) * np.float32(16)).astype(np.int32), 31)
    return np.where(dist < max_exact, dist, large)


def host_consts():
    bf = ml_dtypes.bfloat16
    cst = {}
    cst["ident_bf"] = np.eye(128, dtype=np.float32).astype(bf)
    cst["anti_bf"] = np.eye(128, dtype=np.float32)[::-1].copy().astype(bf)
    cst["ident_f"] = np.eye(128, dtype=np.float32)
    da = np.arange(XA) - 511
    oh = np.zeros((32, XA + XC), np.float32)
    ba = t5_bucket_np(da)
    oh[ba, np.arange(XA)] = 1.0
    dc = np.arange(XC) - 2063
    bc = t5_bucket_np(dc)
    oh[bc, XA + np.arange(XC)] = 1.0
    cst["onehot"] = oh
    mult = np.zeros((4, XC), np.float32)
    mult[0, :XA] = (da >= 0)
    mult[1, :XA] = (da >= 0) & (da < 512)
    mult[2, :XA] = ((da >= 0) & (da <= 128)).astype(np.float32) + ((da >= 0) & (da % 4 == 0) & (da <= 512)) + ((da >= 0) & (da % 16 == 0) & (da <= 2048))
    mult[3, :] = (dc >= 0)
    with np.errstate(divide="ignore"):
        mult[:] = np.where(mult > 0, np.log(np.maximum(mult, 1e-30)), -30000.0)
    mult[0:3, XA:] = 0
    cst["mult"] = np.ascontiguousarray(np.broadcast_to(mult[:, None, :], (4, 16, XC))).astype(np.float32)
    n = 128 * (np.arange(256) // 128) + 127 - (np.arange(256) % 128)
    c_start = n[:, None] * 16
    s_start = np.arange(64)[None, :] * 64
    ov = np.clip(np.minimum(c_start + 32, s_start + 64) - np.maximum(c_start, s_start), 0, None).astype(np.float32) / 32
    ov[n == 255] = 0
    cst["overlap"] = ov.astype(bf)
    key = 128 * (np.arange(S) // 128) + 127 - (np.arange(S) % 128)
    ex = np.zeros((64, S), np.float32)
    ex[key // 64, np.arange(S)] = 1
    cst["exr"] = ex.astype(bf)
    t = np.arange(S)[:, None]
    j = np.arange(64)[None, :]
    cur = t // 64
    fm = np.where((j == 0) | (j == cur) | (j == cur - 1), 1e6, np.where(j * 64 > t, -1e6, 0.0)).astype(np.float32)
    cst["fm"] = fm
    cst["ecst"] = np.ascontiguousarray(np.broadcast_to((np.arange(NE) * S).astype(np.float32)[None, :], (128, NE)))
    cst["utri"] = np.triu(np.ones((128, 128), np.float32), k=1).astype(bf)
    cst["ones_bf"] = np.ones((128, 128), np.float32).astype(bf)
    thr = np.broadcast_to((np.arange(8) * 512).astype(np.float32)[None, None, :], (128, NE, 8))
    cst["jthr"] = np.ascontiguousarray(thr).reshape(128, NE * 8)
    return cst


class Prog:
    def __init__(self, debug=()):
        self.debug = set(debug)
        nc = self.nc = bass.Bass("TRN2", target_bir_lowering=False)
        self.kb = KB(nc)
        self.es = contextlib.ExitStack()
        self.inp = {}
        self.scr = {}

    def din(self, name, shape, dt=F32):
        t = self.nc.dram_tensor(name, list(shape), dt, kind="ExternalInput").ap()
        self.inp[name] = Tl(t)
        return self.inp[name]

    def dscr(self, name, shape, dt):
        kind = "ExternalOutput" if name in self.debug else "Internal"
        t = self.nc.dram_tensor(name, list(shape), dt, kind=kind).ap()
        self.scr[name] = Tl(t)
        return self.scr[name]

    def declare(self):
        d = self.din
        d("x", [S, D]); d("rel_bias", [32, 16]); d("attn_norm", [DEPTH, D]); d("w_in", [DEPTH, D, INW])
        d("nsa_q_norm", [DEPTH, 64]); d("nsa_k_norm", [DEPTH, 3, 64]); d("cmp_pos", [DEPTH, 2, 32, 64])
        d("cmp_w1", [DEPTH, 2, 2048, 256]); d("cmp_b1", [DEPTH, 2, 256]); d("cmp_w2", [DEPTH, 2, 256, 64])
        d("dil_q_norm", [DEPTH, 64]); d("dil_k_norm", [DEPTH, 64]); d("out_norm", [DEPTH, D]); d("w_out", [DEPTH, D, D])
        d("ffn_norm", [DEPTH, D]); d("ffn_w_gate", [1, D, DFF]); d("ffn_w_up", [1, D, DFF]); d("ffn_w_down", [1, DFF, D])
        d("router_w", [1, D, NE]); d("exp_w_gate", [1, NE, D, DFE]); d("exp_w_up", [1, NE, D, DFE]); d("exp_w_down", [1, NE, DFE, D])
        d("ident_bf", [128, 128], BF16); d("anti_bf", [128, 128], BF16); d("ident_f", [128, 128])
        d("onehot", [32, XA + XC]); d("mult", [4, 16, XC]); d("overlap", [256, 64], BF16); d("exr", [64, S], BF16); d("fm", [S, 64])
        d("ecst", [128, NE]); d("utri", [128, 128], BF16); d("ones_bf", [128, 128], BF16); d("jthr", [128, NE * 8])
        self.out = Tl(self.nc.dram_tensor("out", [S, D], F32, kind="ExternalOutput").ap())
        s = self.dscr
        s("wtab", [4, 16, XC], BF16)
        s("qaT", [8, 64, S], BF16); s("kcvT", [2, 2, 64, S], BF16); s("kswT", [2, 2, 64, S], BF16)
        s("vsw", [S, 256], BF16); s("gates", [S, 24], F32)
        s("qbT", [8, 64, S], BF16); s("kbT", [8, 64, S], BF16); s("vb", [S, 512], BF16)
        s("OC", [S, 512], F32); s("O", [S, D], F32); s("H1", [S, D], F32)
        s("HP", [S, D], F32); s("XE", [NE * S, D], BF16); s("YE", [NE * S, D], F32)
        if "dbg" in self.debug:
            s("dbg", [128, 4096], F32)

    def setup_globals(self):
        kb, es = self.kb, self.es
        nc = self.nc
        self.banks = []
        for i in range(8):
            h = es.enter_context(nc.psum_tensor(f"bank{i}", [128, 512], F32))
            self.banks.append(Tl(h.ap(), psum=True))
        self.ident_bf = kb.sb(es, [128, 128], BF16, "identbf")
        self.anti_bf = kb.sb(es, [128, 128], BF16, "antibf")
        self.ident_f = kb.sb(es, [128, 128], F32, "identf")
        kb.epsb = kb.sb(es, [128, 1], F32, "epsb")
        self.one11 = kb.sb(es, [1, 1], F32, "one11")
        self.kcmpT = kb.sb(es, [64, 2, 2, 128], BF16, "kcmpT")
        self.vcaug = kb.sb(es, [128, 2, 2, 128], BF16, "vcaug")
        sem = kb.c.dma_sem()
        kb.dma(self.ident_bf, self.inp["ident_bf"], sem)
        kb.dma(self.anti_bf, self.inp["anti_bf"], sem)
        kb.dma(self.ident_f, self.inp["ident_f"], sem)
        kb.memset("dve", kb.epsb, EPS)
        kb.memset("dve", self.one11, 1.0)
        kb.c.barrier()

    def phase_tables(self):
        kb = self.kb
        with contextlib.ExitStack() as es:
            tbl = kb.sb(es, [32, 16], F32, "tbl")
            oh = kb.sbn(es, 2, [32, 512], F32, "oh")
            e32 = kb.sbn(es, 2, [16, 512], F32, "e32")
            mt = kb.sbn(es, 2, [16, 512], F32, "mt")
            wb = kb.sbn(es, 2, [16, 512], BF16, "wb")
            sems = [kb.c.dma_sem() for _ in range(7)]
            kb.dma(tbl, self.inp["rel_bias"], sems[0])
            wtab = self.scr["wtab"]
            k = 0
            for ci in range((XA + XC) // 512):
                x0 = ci * 512
                o = oh[ci % 2]
                kb.dma(o, self.inp["onehot"][:, x0:x0 + 512], sems[1 + ci % 2])
                bank = self.banks[ci % 2]
                kb.mm(bank[0:16, :], tbl, o)
                e = e32[ci % 2]
                kb.copy("act", e, bank[0:16, :])
                tabs = [(0, x0), (1, x0), (2, x0)] if x0 < XA else [(3, x0 - XA)]
                for tb, xx in tabs:
                    m = mt[k % 2]
                    w = wb[k % 2]
                    kb.dma(m, self.inp["mult"][tb, :, xx:xx + 512], sems[3 + k % 2])
                    kb.tt("dve", w, e, m, ALU.add)
                    kb.dma(wtab[tb, :, xx:xx + 512], w, sems[5 + k % 2], pw=True)
                    k += 1
            kb.c.barrier()
            kb.c.release(sems)

    def phase_inproj(self, layer, hin_dram):
        kb = self.kb
        I = self.inp
        with contextlib.ExitStack() as es:
            W = kb.sb(es, [128, 8, INW], BF16, "win")
            gA = kb.sb(es, [128, D], F32, "gA")
            g6 = kb.sb(es, [128, 6, 64], F32, "g6")
            hin = kb.sbn(es, 2, [128, D], F32, "hin")
            junk = kb.sb(es, [128, INW], F32, "junk")
            junkA = kb.sb(es, [128, D], F32, "junkA")
            ssq = kb.sbn(es, 2, [128, 1], F32, "ssq")
            rms = kb.sbn(es, 2, [128, 1], F32, "rms")
            rstd = kb.sbn(es, 2, [128, 1], F32, "rstd")
            u = kb.sbn(es, 2, [128, D], BF16, "u")
            uT = kb.sbn(es, 2, [128, 8, 128], BF16, "uT")
            pj = kb.sbn(es, 2, [128, INW], F32, "pj")
            ssh = kb.sbn(es, 2, [128, 28], F32, "ssh")
            rmh = kb.sbn(es, 2, [128, 28], F32, "rmh")
            rsh = kb.sbn(es, 2, [128, 28], F32, "rsh")
            t1 = kb.sbn(es, 2, [128, 1024], F32, "t1")
            nb = kb.sbn(es, 2, [128, 2328], BF16, "nb")
            vall = kb.sbn(es, 2, [128, 768], BF16, "vall")
            gt = kb.sbn(es, 2, [128, 24], F32, "gt")
            stg_q = kb.sbn(es, 2, [128, 4, 128], BF16, "stgq")
            stg_c = kb.sbn(es, 2, [128, 2, 128], BF16, "stgc")
            stg_k = kb.sbn(es, 2, [128, 2, 128], BF16, "stgk")
            stg_qb = kb.sbn(es, 2, [128, 4, 128], BF16, "stgqb")
            stg_kb = kb.sbn(es, 2, [128, 4, 128], BF16, "stgkb")
            stg_v = kb.sbn(es, 2, [128, 768], BF16, "stgv")
            sems = [kb.c.dma_sem() for _ in range(20)]
            wv = I["w_in"][layer].v(I["w_in"].ap[layer].rearrange("(kc p) n -> p kc n", p=128))
            for kc in range(8):
                kb.dma(W[:, kc, :], wv[:, kc, :], sems[0], q="pool", pw=True)
            kb.dma(gA, I["attn_norm"].v(I["attn_norm"].ap[layer].partition_broadcast(128)), sems[1])
            gsrc = [I["nsa_q_norm"].ap[layer], I["nsa_k_norm"].ap[layer, 1], I["nsa_k_norm"].ap[layer, 2],
                    I["dil_q_norm"].ap[layer], I["dil_k_norm"].ap[layer], I["nsa_k_norm"].ap[layer, 0]]
            for i, a in enumerate(gsrc):
                kb.dma(g6[:, i, :], Tl(a.partition_broadcast(128), I["nsa_q_norm"].buf), sems[1], pw=True)
            kb.ts("dve", g6[:, 0, :], g6[:, 0, :], 0.125, ALU.mult)
            kb.ts("dve", g6[:, 3, :], g6[:, 3, :], 0.125, ALU.mult)
            self.g6_k0 = None
            bk = self.banks
            hv = hin_dram.v(hin_dram.ap.rearrange("(t p) d -> t p d", p=128))
            qaT2 = self.scr["qaT"].v(self.scr["qaT"].ap.rearrange("h d s -> (h d) s").rearrange("(a p) s -> p a s", p=128))
            kcvT2 = self.scr["kcvT"].v(self.scr["kcvT"].ap.rearrange("k g d s -> (k g d) s").rearrange("(a p) s -> p a s", p=128))
            kswT2 = self.scr["kswT"].v(self.scr["kswT"].ap.rearrange("k g d s -> (k g d) s").rearrange("(a p) s -> p a s", p=128))
            qbT2 = self.scr["qbT"].v(self.scr["qbT"].ap.rearrange("h d s -> (h d) s").rearrange("(a p) s -> p a s", p=128))
            kbT2 = self.scr["kbT"].v(self.scr["kbT"].ap.rearrange("h d s -> (h d) s").rearrange("(a p) s -> p a s", p=128))
            def pre(t):
                s = t % 2
                ts_ = slice(t * 128, (t + 1) * 128)
                kb.dma(hin[s], hv[t], sems[2 + s], q="pool")
                h = hin[s]
                kb.act(junkA, h, AF.Square, accum=ssq[s])
                kb.act(rms[s], ssq[s], AF.Sqrt, bias=kb.epsb, scale=1.0 / D)
                kb.recip(rstd[s], rms[s])
                kb.stt(u[s], h, rstd[s], gA, ALU.mult, ALU.mult)
                b2 = bk[2].v(bk[2].ap.bitcast(BF16))
                for kc in range(8):
                    kb.tr(b2[:, kc * 128:(kc + 1) * 128], u[s][:, kc * 128:(kc + 1) * 128], self.ident_bf)
                kb.copy("act", uT[s], b2.v(b2.ap.rearrange("p (a b) -> p a b", b=128)))
            def mmf(t):
                s = t % 2
                for cg in range(6):
                    c0 = cg * 512
                    cw = min(512, INW - c0)
                    bank = bk[cg % 2]
                    for kc in range(8):
                        kb.mm(bank[:, 0:cw], uT[s][:, kc, :], W[:, kc, c0:c0 + cw], start=(kc == 0), stop=(kc == 7))
                    kb.copy("act" if cg % 2 == 0 else "dve", pj[s][:, c0:c0 + cw], bank[:, 0:cw])
            def back(t):
                s = t % 2
                ts_ = slice(t * 128, (t + 1) * 128)
                p = pj[s]
                kb.act(junk, p, AF.Square)
                for (c0, nh, r0) in ((C_QA, 8, 0), (C_KS, 2, 8), (C_KW, 2, 10), (C_QB, 16, 12)):
                    kb.reduce(ssh[s][:, r0:r0 + nh], junk.v(junk.ap[:, c0:c0 + nh * 64].rearrange("p (h d) -> p h d", d=64)))
                kb.act(rmh[s], ssh[s], AF.Sqrt, bias=kb.epsb, scale=1.0 / 64)
                kb.recip(rsh[s], rmh[s])
                n_ = nb[s]
                for (c0, nh, r0, gi) in ((C_QA, 8, 0, 0), (C_KS, 2, 8, 1), (C_KW, 2, 10, 2), (C_QB, 8, 12, 3), (C_KB, 8, 20, 4)):
                    tv = t1[s].v(t1[s].ap[:, 0:nh * 64].rearrange("p (h d) -> p h d", d=64))
                    pv = p.v(p.ap[:, c0:c0 + nh * 64].rearrange("p (h d) -> p h d", d=64))
                    kb.tt("dve", tv, pv, rsh[s].v(bc_last(rsh[s].ap[:, r0:r0 + nh], 64)), ALU.mult)
                    nv = n_.v(n_.ap[:, c0:c0 + nh * 64].rearrange("p (h d) -> p h d", d=64))
                    kb.tt("pool", nv, tv, g6.v(bc_mid(g6.ap[:, gi, :], nh)), ALU.mult)
                kb.copy("pool", n_[:, C_KC:C_KC + 256], p[:, C_KC:C_KC + 256])
                kb.copy("act", vall[s][:, 0:128], p[:, C_VS:C_VS + 128])
                kb.copy("act", vall[s][:, 128:256], p[:, C_VW:C_VW + 128])
                kb.copy("act", vall[s][:, 256:768], p[:, C_VB:C_VB + 512])
                kb.act(gt[s], p[:, C_GA:C_GA + 24], AF.Sigmoid)
                kb.dma(self.scr["gates"][ts_, :], gt[s], sems[4 + s], pw=True)
                b3 = bk[3].v(bk[3].ap.bitcast(BF16))
                for a in range(4):
                    kb.tr(b3[:, a * 128:(a + 1) * 128], n_[:, C_QA + a * 128:C_QA + (a + 1) * 128], self.ident_bf)
                for a in range(4):
                    kb.tr(b3[:, 512 + a * 128:512 + (a + 1) * 128], n_[:, C_QB + a * 128:C_QB + (a + 1) * 128], self.ident_bf)
                kb.copy("dve", stg_q[s], b3.v(b3.ap[:, 0:512].rearrange("p (a b) -> p a b", b=128)))
                kb.copy("act", stg_qb[s], b3.v(b3.ap[:, 512:1024].rearrange("p (a b) -> p a b", b=128)))
                kb.dma(qaT2[:, :, ts_], stg_q[s], sems[6 + s], pw=True)
                kb.dma(qbT2[:, :, ts_], stg_qb[s], sems[8 + s], pw=True)
                b4 = bk[4].v(bk[4].ap.bitcast(BF16))
                for a in range(2):
                    kb.tr(b4[:, a * 128:(a + 1) * 128], n_[:, C_KC + a * 128:C_KC + (a + 1) * 128], self.ident_bf)
                kb.copy("dve", stg_c[s], b4.v(b4.ap[:, 0:256].rearrange("p (a b) -> p a b", b=128)))
                kb.dma(kcvT2[:, :, ts_], stg_c[s], sems[10 + s], pw=True)
                for a, c0 in enumerate((C_KS, C_KW)):
                    kb.mm(bk[5][:, a * 128:(a + 1) * 128], n_[:, c0:c0 + 128], self.anti_bf)
                kb.copy("act", stg_k[s], bk[5].v(bk[5].ap[:, 0:256].rearrange("p (a b) -> p a b", b=128)))
                kb.dma(kswT2[:, :, ts_], stg_k[s], sems[12 + s], pw=True)
                for a in range(4):
                    kb.mm(bk[6][:, a * 128:(a + 1) * 128], n_[:, C_KB + a * 128:C_KB + (a + 1) * 128], self.anti_bf)
                kb.copy("dve", stg_kb[s], bk[6].v(bk[6].ap.rearrange("p (a b) -> p a b", b=128)))
                kb.dma(kbT2[:, :, ts_], stg_kb[s], sems[14 + s], pw=True)
                kb.mm(bk[7], self.anti_bf, vall[s][:, 256:768])
                kb.copy("act", stg_v[s][:, 256:768], bk[7])
                kb.mm(bk[5][:, 256:512], self.anti_bf, vall[s][:, 0:256])
                kb.copy("dve", stg_v[s][:, 0:256], bk[5][:, 256:512])
                kb.dma(self.scr["vsw"][ts_, :], stg_v[s][:, 0:256], sems[16 + s], pw=True)
                kb.dma(self.scr["vb"][ts_, :], stg_v[s][:, 256:768], sems[18 + s], pw=True)

            kb.c.play(kb.c.record(lambda: pre(0)))
            kb.c.play(kb.c.record(lambda: pre(1)), kb.c.record(lambda: mmf(0)))
            for t in range(NT):
                lb = kb.c.record(lambda: back(t))
                k = next(i for i, o in enumerate(lb) if o[0] == "pe")
                lists = [lb[:k]]
                if t + 1 < NT:
                    lists.insert(0, kb.c.record(lambda: mmf(t + 1)))
                if t + 2 < NT:
                    lists.insert(0, kb.c.record(lambda: pre(t + 2)))
                kb.c.play(*lists)
                kb.c.play(lb[k:])
            kb.c.barrier()
            kb.c.release(sems)

    def phase_compress(self, layer):
        kb = self.kb
        I = self.inp
        bk = self.banks
        with contextlib.ExitStack() as es:
            kvT = kb.sbn(es, 2, [64, S], BF16, "kvT")
            w1 = kb.sbn(es, 2, [64, 32, 256], BF16, "w1")
            w2 = kb.sbn(es, 2, [128, 2, 64], BF16, "w2")
            pos = kb.sbn(es, 2, [32, 64], F32, "pos")
            posT = kb.sbn(es, 2, [64, 32], BF16, "posT")
            b1 = kb.sbn(es, 2, [1, 256], F32, "b1")
            bias = kb.sbn(es, 2, [128, 2], F32, "bias")
            hid = kb.sbn(es, 2, [128, 2, 256], BF16, "hid")
            gk = kb.sb(es, [128, 64], F32, "gk")
            xk = kb.sbn(es, 2, [128, 64], F32, "xk")
            jk = kb.sb(es, [128, 64], F32, "jk")
            sq1 = kb.sbn(es, 2, [128, 1], F32, "sq1")
            rm1 = kb.sbn(es, 2, [128, 1], F32, "rm1")
            rs1 = kb.sbn(es, 2, [128, 1], F32, "rs1")
            xb = kb.sbn(es, 2, [128, 64], BF16, "xb")
            sems = [kb.c.dma_sem() for _ in range(8)]
            kb.dma(gk, Tl(I["nsa_k_norm"].ap[layer, 0].partition_broadcast(128), I["nsa_k_norm"].buf), sems[0])
            it = 0
            for g in range(2):
                for kv in range(2):
                    s = it % 2
                    it += 1
                    kb.dma(kvT[s], self.scr["kcvT"][kv, g], sems[1 + s])
                    w1src = I["cmp_w1"].v(I["cmp_w1"].ap[layer, kv].rearrange("(l d) c -> d l c", d=64))
                    for lq in range(4):
                        kb.dma(w1[s][:, lq * 8:(lq + 1) * 8, :], w1src[:, lq * 8:(lq + 1) * 8, :], sems[3 + s], q="pool", pw=True)
                    kb.dma(w2[s], I["cmp_w2"].v(I["cmp_w2"].ap[layer, kv].rearrange("(hh p) c -> p hh c", p=128)), sems[3 + s], q="pool", pw=True)
                    kb.dma(pos[s], I["cmp_pos"][layer, kv], sems[5 + s], pw=True)
                    kb.dma(b1[s], I["cmp_b1"][layer, kv:kv + 1, :], sems[5 + s], pw=True)
                    kb.tr(bk[2][0:64, 0:32], pos[s], self.ident_f[0:32, 0:32])
                    kb.copy("dve", posT[s], bk[2][0:64, 0:32])
                    for hh in range(2):
                        hs = slice(hh * 128, (hh + 1) * 128)
                        for l in range(32):
                            kb.mm(bk[3][:, hh:hh + 1], w1[s][:, l, hs], posT[s][:, l:l + 1], start=(l == 0), stop=False)
                        kb.mm(bk[3][:, hh:hh + 1], b1[s][0:1, hs], self.one11, start=False, stop=True)
                    kb.copy("dve", bias[s], bk[3][:, 0:2])
                    kb.memset("pool", hid[s][:, :, 255:256], 0.0)
                    for hh in range(2):
                        hs = slice(hh * 128, (hh + 1) * 128)
                        bank = bk[hh]
                        ka = kvT[s].ap
                        for l in range(32):
                            rhs = kvT[s].v(AP(ka.tensor, ka.offset + l, [list(ka.ap[0]), [16, 255]]))
                            kb.mm(bank[:, 0:255], w1[s][:, l, hs], rhs, start=(l == 0), stop=(l == 31))
                        kb.act(hid[s][:, hh, 0:255], bank[:, 0:255], AF.Gelu_apprx_tanh, bias=bias[s][:, hh:hh + 1])
                    for nt in range(2):
                        ns = slice(nt * 128, (nt + 1) * 128)
                        bank = bk[4 + nt]
                        for hh in range(2):
                            kb.mm(bank[:, 0:64], hid[s][:, hh, ns], w2[s][:, hh, :], start=(hh == 0), stop=(hh == 1))
                        j = (it + nt) % 2
                        if kv == 0:
                            kb.copy("dve", xk[j], bank[:, 0:64])
                            kb.act(jk, xk[j], AF.Square, accum=sq1[j])
                            kb.act(rm1[j], sq1[j], AF.Sqrt, bias=kb.epsb, scale=1.0 / 64)
                            kb.recip(rs1[j], rm1[j])
                            kb.stt(xb[j], xk[j], rs1[j], gk, ALU.mult, ALU.mult)
                            kb.mm(bk[6 + nt][0:64, 0:128], xb[j], self.anti_bf)
                            kb.copy("act", self.kcmpT[:, g, nt, :], bk[6 + nt][0:64, 0:128])
                        else:
                            kb.copy("dve", xb[j], bank[:, 0:64])
                            kb.mm(bk[6 + nt][:, 0:64], self.anti_bf, xb[j])
                            kb.copy("act", self.vcaug[:, g, nt, 0:64], bk[6 + nt][:, 0:64])
            for g in range(2):
                for nt in range(2):
                    kb.dma(self.vcaug[:, g, nt, 64:128], self.inp["overlap"][nt * 128:(nt + 1) * 128, :], sems[7], pw=True)
            kb.c.barrier()
            kb.c.release(sems)


    def run_attn(self, items, ebuf, tbuf, pbuf):
        kb = self.kb
        bk = self.banks
        n = len(items)
        if n == 0:
            return

        LA = 3
        pending = []

        def score(i):
            it = items[i]
            c0, c1 = it["qa"] * 128, it["qb"] * 128
            kb.mm(bk[i % 4][:, c0:c1], it["kT"], it["q"][:, c0:c1], start=True, stop=False)
            kb.mm(bk[i % 4][:, c0:c1], self.ident_bf, it["strip"][:, c0:c1], start=False, stop=True)
        for i in range(min(LA, n)):
            score(i)
        for i in range(n):
            if i + LA < n:
                score(i + LA)
            it = items[i]
            c0, c1 = it["qa"] * 128, it["qb"] * 128
            p = pbuf[i % len(pbuf)]
            kb.act(p[:, c0:c1], bk[i % 4][:, c0:c1], AF.Exp)
            kb.mm(it["pv"][:, c0:c1], it["vaug"], p[:, c0:c1], start=it["first"], stop=it["last"])
            if it["last"]:
                pending.append((i + 2, it["fin"]))
            while pending and pending[0][0] <= i:
                pending.pop(0)[1]()
        while pending:
            pending.pop(0)[1]()

    def hankel(self, tb, hd, pstep, ncols):
        w = self.scr["wtab"]
        a = w.ap[tb, hd]
        return w.v(AP(a.tensor, a.offset, [[pstep, 128], [1, ncols]]))

    def phase_nsa(self, layer):
        kb = self.kb
        I = self.inp
        bk = self.banks
        with contextlib.ExitStack() as es0:
            gates = kb.sb(es0, [128, NT, 24], F32, "gates")
            selT = kb.sb(es0, [128, 2, S], BF16, "selT")
            sem0 = kb.c.dma_sem()
            kb.dma(gates, self.scr["gates"].v(self.scr["gates"].ap.rearrange("(t p) c -> p t c", p=128)), sem0)
            kb.c.barrier()
            OCv = self.scr["OC"].v(self.scr["OC"].ap.rearrange("(t p) c -> p t c", p=128))
            Ov = self.scr["O"].v(self.scr["O"].ap.rearrange("(t p) c -> p t c", p=128))
            with contextlib.ExitStack() as es:
                fm = kb.sb(es, [128, NT, 64], F32, "fm")
                imp = kb.sb(es, [128, NT, 64], F32, "imp")
                stripc = kb.sbn(es, 2, [128, S], BF16, "stripc")
                qTh = kb.sbn(es, 2, [64, S], BF16, "qTh")
                eb = kb.sbn(es, 3, [128, 512], BF16, "eb")
                pb = kb.sbn(es, 3, [128, 512], BF16, "pb")
                den = kb.sbn(es, 2, [128, 4], F32, "den")
                rd = kb.sbn(es, 2, [128, 4], F32, "rd")
                sc = kb.sbn(es, 2, [128, 4], F32, "sc")
                itmp = kb.sbn(es, 2, [128, 4, 64], F32, "itmp")
                ocs = kb.sbn(es, 2, [128, 4, 64], F32, "ocs")
                impf = kb.sbn(es, 2, [128, 64], F32, "impf")
                imp2 = kb.sbn(es, 2, [128, 64], F32, "imp2")
                m8a = kb.sbn(es, 2, [128, 8], F32, "m8a")
                m8b = kb.sbn(es, 2, [128, 8], F32, "m8b")
                selm = kb.sbn(es, 2, [128, 128], BF16, "selm")
                sems = [kb.c.dma_sem() for _ in range(7)]
                kb.dma(fm, I["fm"].v(I["fm"].ap.rearrange("(t p) c -> p t c", p=128)), sems[0])
                kb.memset("dve", selm[0], 0.0)
                kb.memset("dve", selm[1], 0.0)
                kb.c.barrier()
                it = 0
                cnt = 0
                for g in range(2):
                    for h in range(4):
                        hd = 4 * g + h
                        s = it % 2
                        it += 1
                        kb.dma(stripc[s], self.hankel(3, hd, 16, S), sems[1 + s])
                        kb.dma(qTh[s], self.scr["qaT"][hd], sems[3 + s])
                        for QG in range(8):
                            qs = slice(QG * 512, (QG + 1) * 512)
                            ps = []
                            for nt in range(2 if QG >= 4 else 1):
                                bank = bk[nt]
                                x0 = QG * 512 - nt * 2048
                                kb.mm(bank, self.kcmpT[:, g, nt, :], qTh[s][:, qs], start=True, stop=False)
                                kb.mm(bank, self.ident_bf, stripc[s][:, x0:x0 + 512], start=False, stop=True)
                                p = pb[cnt % 3]
                                cnt += 1
                                kb.act(p, bank, AF.Exp)
                                ps.append(p)
                            u_ = (it * 8 + QG) % 2
                            oc = ocs[u_]
                            ob = bk[2 + u_]
                            for qt in range(4):
                                for nt, p in enumerate(ps):
                                    kb.mm(ob[:, qt * 128:(qt + 1) * 128], p[:, qt * 128:(qt + 1) * 128], self.vcaug[:, g, nt, :], start=(nt == 0), stop=(nt == len(ps) - 1))
                            obv = ob.v(ob.ap.rearrange("p (a b) -> p a b", b=128))
                            kb.reduce(den[u_], obv[:, :, 64:128])
                            kb.ts("dve", den[u_], den[u_], 1e-30, ALU.max)
                            kb.recip(rd[u_], den[u_])
                            kb.tt("dve", sc[u_], rd[u_], gates[:, QG * 4:QG * 4 + 4, hd * 3], ALU.mult)
                            kb.tt("dve", oc, obv[:, :, 0:64], sc[u_].v(bc_last(sc[u_].ap, 64)), ALU.mult)
                            iv = imp[:, QG * 4:QG * 4 + 4, :]
                            if h == 0:
                                kb.tt("dve", iv, obv[:, :, 64:128], rd[u_].v(bc_last(rd[u_].ap, 64)), ALU.mult)
                            else:
                                kb.tt("dve", itmp[u_], obv[:, :, 64:128], rd[u_].v(bc_last(rd[u_].ap, 64)), ALU.mult)
                                kb.tt("pool", iv, iv, itmp[u_], ALU.add)
                            kb.dma(OCv[:, QG * 4:QG * 4 + 4, hd * 64:(hd + 1) * 64], oc, sems[5 + u_], pw=True)
                    b4 = bk[4].v(bk[4].ap.bitcast(BF16))
                    for tile in range(NT):
                        j = tile % 2
                        kb.tt("dve", impf[j], imp[:, tile, :], fm[:, tile, :], ALU.add)
                        a, b, c_ = impf[j].ap, m8a[j].ap, imp2[j].ap
                        kb.op("dve", lambda e, a=a, b=b: e.max(out=b, in_=a), [m8a[j]], [impf[j]])
                        kb.op("dve", lambda e, a=a, b=b, c_=c_: e.match_replace(out=c_, in_to_replace=b, in_values=a, imm_value=-3.0e6), [imp2[j]], [impf[j], m8a[j]])
                        d_ = m8b[j].ap
                        kb.op("dve", lambda e, c_=c_, d_=d_: e.max(out=d_, in_=c_), [m8b[j]], [imp2[j]])
                        kb.ts("dve", selm[j][:, 64:128], impf[j], m8b[j][:, 7:8], ALU.is_ge)
                        kb.tr(b4[:, j * 128:(j + 1) * 128], selm[j], self.ident_bf)
                        kb.ts("dve", selT[64:128, g, tile * 128:(tile + 1) * 128], b4[64:128, j * 128:(j + 1) * 128], -1.0, ALU.add, 30000.0, ALU.mult)
                kb.c.barrier()
                kb.c.release(sems)
            if "selT" in self.debug:
                semd = kb.c.dma_sem()
                kb.dma(self.scr["selT"], selT, semd)
                kb.c.barrier()
            with contextlib.ExitStack() as es:
                if getattr(self, "skip_p3b", False):
                    kb.c.release([sem0])
                    return
                ksT = kb.sbn(es, 2, [128, S], BF16, "ksx")
                kwT = kb.sbn(es, 2, [128, S], BF16, "kwT")
                vsa = kb.sbn(es, 2, [128, NT, 65], BF16, "vsa")
                vwa = kb.sbn(es, 2, [128, NT, 65], BF16, "vwa")
                ssel = kb.sbn(es, 4, [128, 2688], BF16, "ssel")
                swin = kb.sbn(es, 4, [128, 1408], BF16, "swin")
                qT4 = kb.sbn(es, 2, [128, 4, 512], BF16, "qsx")
                oct_ = kb.sbn(es, 2, [128, 4, 256], F32, "oct")
                eb = kb.sbn(es, 5, [128, 512], BF16, "eb")
                tb = kb.sbn(es, 5, [128, 512], BF16, "tb")
                pb = kb.sbn(es, 5, [128, 512], BF16, "pb")
                osb = kb.sbn(es, 2, [65, 512], F32, "osb")
                rd4 = kb.sbn(es, 2, [128, 4], F32, "rd4")
                sc4 = kb.sbn(es, 2, [128, 4], F32, "sc4")
                tmp4 = kb.sbn(es, 2, [128, 4, 64], F32, "tmp4")
                sems = [kb.c.dma_sem() for _ in range(12)]
                kb.memset("pool", kwT[0][64:128, :], 0.0)
                kb.memset("pool", kwT[1][64:128, :], 0.0)
                kb.dma(ksT[0][64:128, :], I["exr"], sems[0], pw=True)
                kb.dma(ksT[1][64:128, :], I["exr"], sems[0], pw=True)
                vswv = self.scr["vsw"].v(self.scr["vsw"].ap.rearrange("(t p) c -> p t c", p=128))
                fcnt = [0]
                for g in range(2):
                    sg = g % 2
                    kb.dma(ksT[sg][0:64, :], self.scr["kswT"][0, g], sems[1 + sg], pw=True)
                    kb.dma(kwT[sg][0:64, :], self.scr["kswT"][1, g], sems[1 + sg], pw=True)
                    kb.dma(vsa[sg][:, :, 0:64], vswv[:, :, g * 64:(g + 1) * 64], sems[1 + sg], pw=True)
                    kb.dma(vwa[sg][:, :, 0:64], vswv[:, :, 128 + g * 64:128 + (g + 1) * 64], sems[1 + sg], pw=True)
                    kb.memset("pool", vsa[sg][:, :, 64:65], 1.0)
                    kb.memset("pool", vwa[sg][:, :, 64:65], 1.0)
                    for h in range(4):
                        kb.dma(ssel[h], self.hankel(0, 4 * g + h, 1, 2688), sems[3], pw=True)
                        kb.dma(swin[h], self.hankel(1, 4 * g + h, 1, 1408), sems[3], pw=True)
                    qsrc = self.scr["qaT"].v(self.scr["qaT"].ap[4 * g:4 * g + 4].rearrange("h d s -> d h s"))
                    for QG in range(8):
                        sq = QG % 2
                        qs = slice(QG * 512, (QG + 1) * 512)
                        nK = 4 * QG + 4
                        kb.dma(qT4[sq][0:64, :, :], qsrc[:, :, qs], sems[4 + sq], pw=True)
                        kb.op("pool", (lambda e, o=qT4[sq].ap[64:128, :, :], i=bc_mid(selT.ap[64:128, g, qs], 4): e.tensor_copy(out=o, in_=i)), [], [selT], pw=[qT4[sq]])
                        acc = oct_[sq]
                        kb.dma(acc, OCv[:, QG * 4:QG * 4 + 4, g * 256:(g + 1) * 256], sems[6 + sq])
                        items = []
                        for h in range(4):
                            hd = 4 * g + h
                            for br in range(2):
                                k0 = 0 if br == 0 else max(0, 4 * QG - 4)
                                pvb = bk[4 + br][0:65, :]

                                def fin(h=h, hd=hd, br=br, pvb=pvb, acc=acc, QG=QG):
                                    f = fcnt[0]
                                    fcnt[0] += 1
                                    o = osb[f % 2]
                                    kb.copy("dve", o, pvb)
                                    tbk = bk[6 + f % 2]
                                    for qt in range(4):
                                        kb.tr(tbk[:, qt * 65:qt * 65 + 65], o[:, qt * 128:(qt + 1) * 128], self.ident_f[0:65, 0:65])
                                    ta = tbk.ap
                                    dens = tbk.v(AP(ta.tensor, ta.offset + 64, [list(ta.ap[0]), [65, 4]]))
                                    vals = tbk.v(AP(ta.tensor, ta.offset, [list(ta.ap[0]), [65, 4], [1, 64]]))
                                    r4, s4, tm = rd4[f % 2], sc4[f % 2], tmp4[f % 2]
                                    kb.recip(r4, dens)
                                    kb.tt("dve", s4, r4, gates[:, QG * 4:QG * 4 + 4, hd * 3 + 1 + br], ALU.mult)
                                    kb.tt("dve", tm, vals, s4.v(bc_last(s4.ap, 64)), ALU.mult)
                                    av = acc[:, :, h * 64:(h + 1) * 64]
                                    kb.tt("pool", av, av, tm, ALU.add)
                                for Kt in range(k0, nK):
                                    if br == 0:
                                        c0 = min(4 * QG - Kt + 3, 16) * 128
                                        items.append(dict(kT=ksT[sg][:, Kt * 128:(Kt + 1) * 128], q=qT4[sq][:, h, :], strip=ssel[h][:, c0:c0 + 512],
                                                          mask=None, vaug=vsa[sg][:, Kt, :], pv=pvb, first=(Kt == k0), last=(Kt == nK - 1), fin=fin,
                                                          qa=max(0, Kt - 4 * QG), qb=4))
                                    else:
                                        c0 = (4 * QG - Kt + 3) * 128
                                        items.append(dict(kT=kwT[sg][:, Kt * 128:(Kt + 1) * 128], q=qT4[sq][:, h, :], strip=swin[h][:, c0:c0 + 512],
                                                          mask=None, vaug=vwa[sg][:, Kt, :], pv=pvb, first=(Kt == k0), last=(Kt == nK - 1), fin=fin,
                                                          qa=max(0, Kt - 4 * QG), qb=min(4, Kt - 4 * QG + 5)))
                        self.run_attn(items, eb, tb, pb)
                        kb.dma(Ov[:, QG * 4:QG * 4 + 4, g * 256:(g + 1) * 256], acc, sems[8 + sq], pw=True)
                kb.c.barrier()
                kb.c.release(sems)
            kb.c.release([sem0])

    def phase_dil(self, layer):
        kb = self.kb
        bk = self.banks
        with contextlib.ExitStack() as es:
            kT = kb.sbn(es, 2, [128, S], BF16, "kbT")
            qT = kb.sbn(es, 2, [128, S], BF16, "qbT")
            for t_ in (kT[0], kT[1], qT[0], qT[1]):
                kb.memset("pool", t_[64:128, :], 0.0)
            va = kb.sbn(es, 2, [128, NT, 65], BF16, "vba")
            sd = kb.sbn(es, 2, [128, 2944], BF16, "sdil")
            eb = kb.sbn(es, 5, [128, 512], BF16, "eb")
            pb = kb.sbn(es, 5, [128, 512], BF16, "pb")
            osb = kb.sbn(es, 2, [65, 512], F32, "osb")
            rd4 = kb.sbn(es, 2, [128, 4], F32, "rd4")
            obs = kb.sbn(es, 2, [128, 4, 64], F32, "obs")
            sems = [kb.c.dma_sem() for _ in range(4)]
            vbv = self.scr["vb"].v(self.scr["vb"].ap.rearrange("(t p) c -> p t c", p=128))
            Ov = self.scr["O"].v(self.scr["O"].ap.rearrange("(t p) c -> p t c", p=128))
            fcnt = [0]

            def load(hd):
                s = hd % 2
                kb.dma(kT[s][0:64, :], self.scr["kbT"][hd], sems[s], pw=True)
                kb.dma(qT[s][0:64, :], self.scr["qbT"][hd], sems[s], pw=True)
                kb.dma(va[s][:, :, 0:64], vbv[:, :, hd * 64:(hd + 1) * 64], sems[s], pw=True)
                kb.memset("pool", va[s][:, :, 64:65], 1.0)
                kb.dma(sd[s], self.hankel(2, 8 + hd, 1, 2944), sems[s])
            load(0)
            for hd in range(8):
                s = hd % 2
                if hd + 1 < 8:
                    load(hd + 1)
                items = []
                for QG in range(8):
                    k0 = max(0, 4 * QG - 16)
                    nK = 4 * QG + 4
                    pvb = bk[4 + QG % 2][0:65, :]

                    def fin(hd=hd, QG=QG, pvb=pvb):
                        f = fcnt[0]
                        fcnt[0] += 1
                        o = osb[f % 2]
                        kb.copy("dve", o, pvb)
                        tbk = bk[6 + f % 2]
                        ob = obs[f % 2]
                        for qt in range(4):
                            kb.tr(tbk[:, qt * 65:qt * 65 + 65], o[:, qt * 128:(qt + 1) * 128], self.ident_f[0:65, 0:65])
                        ta = tbk.ap
                        dens = tbk.v(AP(ta.tensor, ta.offset + 64, [list(ta.ap[0]), [65, 4]]))
                        vals = tbk.v(AP(ta.tensor, ta.offset, [list(ta.ap[0]), [65, 4], [1, 64]]))
                        r4 = rd4[f % 2]
                        kb.recip(r4, dens)
                        kb.tt("dve", ob, vals, r4.v(bc_last(r4.ap, 64)), ALU.mult)
                        kb.dma(Ov[:, QG * 4:QG * 4 + 4, 512 + hd * 64:512 + (hd + 1) * 64], ob, sems[2 + f % 2], pw=True)
                    for Kt in range(k0, nK):
                        c0 = (4 * QG - Kt + 3) * 128
                        items.append(dict(kT=kT[s][:, Kt * 128:(Kt + 1) * 128], q=qT[s][:, QG * 512:(QG + 1) * 512], strip=sd[s][:, c0:c0 + 512],
                                          mask=None, vaug=va[s][:, Kt, :], pv=pvb, first=(Kt == k0), last=(Kt == nK - 1), fin=fin,
                                          qa=max(0, Kt - 4 * QG), qb=min(4, Kt - 4 * QG + 17)))
                self.run_attn(items, eb, None, pb)
            kb.c.barrier()
            kb.c.release(sems)


    def phase_ffn(self, layer, hin_dram, hout_dram):
        kb = self.kb
        I = self.inp
        bk = self.banks
        moe = (layer % 2 == 1)
        li = layer // 2
        dff = DFE if moe else DFF
        nfc = dff // 128
        nfb = dff // 256
        with contextlib.ExitStack() as es:
            Wo = kb.sb(es, [128, 8, D], BF16, "Wo")
            gO = kb.sb(es, [128, D], F32, "gO")
            gF = kb.sb(es, [128, D], F32, "gF")
            hp = kb.sb(es, [128, 4, D], F32, "hp")
            hpt = [Tl(hp.ap[:, i, :]) for i in range(4)]
            vT = kb.sb(es, [128, 8, 512], BF16, "vT")
            hid = kb.sb(es, [128, nfc, 512], BF16, "hid")
            hidc = [Tl(hid.ap[:, i, :]) for i in range(nfc)]
            stg = kb.sbn(es, 2, [128, 8, 256], F32, "stg")
            wgu = kb.sbn(es, 4, [128, 8, 256], BF16, "wgu")
            stgd = kb.sbn(es, 2, [128, 4, 512], F32, "stgd")
            wd = kb.sbn(es, 2, [128, 4, 512], BF16, "wd")
            Ot = kb.sbn(es, 2, [128, D], F32, "Ot")
            ht = kb.sbn(es, 2, [128, D], F32, "ht")
            junk = kb.sb(es, [128, D], F32, "junk")
            on = kb.sbn(es, 2, [128, D], BF16, "on")
            onT = kb.sbn(es, 2, [128, 8, 128], BF16, "onT")
            vb_ = kb.sbn(es, 2, [128, D], BF16, "vb_")
            ss2 = kb.sbn(es, 2, [128, 2], F32, "ss2")
            rm2 = kb.sbn(es, 2, [128, 2], F32, "rm2")
            rs2 = kb.sbn(es, 2, [128, 2], F32, "rs2")
            ss1 = kb.sbn(es, 2, [128, 1], F32, "ss1")
            rm1 = kb.sbn(es, 2, [128, 1], F32, "rm1")
            rs1 = kb.sbn(es, 2, [128, 1], F32, "rs1")
            sg = kb.sbn(es, 2, [128, 512], F32, "sg")
            sems = [kb.c.dma_sem() for _ in range(12)]
            if moe:
                v32 = kb.sbn(es, 2, [128, D], F32, "v32")
                v32T = kb.sb(es, [128, 8, 128], F32, "v32T")
                rw = kb.sb(es, [128, 8, NE], F32, "rw")
                gate = kb.sb(es, [128, 4, NE], F32, "gate")
                lg = kb.sbn(es, 2, [128, NE], F32, "lg")
                m8 = kb.sbn(es, 2, [128, 8], F32, "m8")
                msk = kb.sbn(es, 2, [128, NE], F32, "msk")
                nm1 = kb.sbn(es, 2, [128, 1], F32, "nm1")
                ex = kb.sbn(es, 2, [128, NE], F32, "ex")
                gu = kb.sbn(es, 2, [128, NE], F32, "gu")
                dn = kb.sbn(es, 2, [128, 1], F32, "dn")
                rdn = kb.sbn(es, 2, [128, 1], F32, "rdn")
                kb.dma(rw, I["router_w"].v(I["router_w"].ap[li].rearrange("(kc p) e -> p kc e", p=128)), sems[0])
            wov = I["w_out"].v(I["w_out"].ap[layer].rearrange("(kc p) n -> p kc n", p=128))
            for kc in range(8):
                kb.dma(Wo[:, kc, :], wov[:, kc, :], sems[0], q="pool", pw=True)
            kb.dma(gO, Tl(I["out_norm"].ap[layer].partition_broadcast(128), I["out_norm"].buf), sems[0])
            kb.dma(gF, Tl(I["ffn_norm"].ap[layer].partition_broadcast(128), I["ffn_norm"].buf), sems[0])
            kb.c.barrier()
            Ov = self.scr["O"].v(self.scr["O"].ap.rearrange("(t p) c -> t p c", p=128))
            hv = hin_dram.v(hin_dram.ap.rearrange("(t p) d -> t p d", p=128))
            hov = hout_dram.v(hout_dram.ap.rearrange("(t p) d -> p t d", p=128))
            if moe:
                experts = [(I["exp_w_gate"][li, e], I["exp_w_up"][li, e], I["exp_w_down"][li, e], e) for e in range(NE)]
            else:
                experts = [(I["ffn_w_gate"][li], I["ffn_w_up"][li], I["ffn_w_down"][li], None)]
            cnt = {"ld": 0, "ldd": 0, "fc": 0}
            for blk in range(8):
                def tile_body(ti, blk=blk):
                    t = blk * 4 + ti
                    s = t % 2
                    kb.dma(Ot[s], Ov[t], sems[1 + s])
                    kb.dma(ht[s], hv[t], sems[3 + s])
                    o = Ot[s]
                    kb.act(junk[:, 0:512], o[:, 0:512], AF.Square, accum=ss2[s][:, 0:1])
                    kb.act(junk[:, 512:1024], o[:, 512:1024], AF.Square, accum=ss2[s][:, 1:2])
                    kb.act(rm2[s], ss2[s], AF.Sqrt, bias=kb.epsb, scale=1.0 / 512)
                    kb.recip(rs2[s], rm2[s])
                    for gq in range(2):
                        cs = slice(gq * 512, (gq + 1) * 512)
                        kb.stt(on[s][:, cs], o[:, cs], rs2[s][:, gq:gq + 1], gO[:, cs], ALU.mult, ALU.mult)
                    b0 = bk[s].v(bk[s].ap.bitcast(BF16))
                    for kc in range(8):
                        kb.tr(b0[:, kc * 128:(kc + 1) * 128], on[s][:, kc * 128:(kc + 1) * 128], self.ident_bf)
                    kb.copy("act", onT[s], b0.v(b0.ap.rearrange("p (a b) -> p a b", b=128)))
                    for half in range(2):
                        bank = bk[2 + 2 * s + half]
                        for kc in range(8):
                            kb.mm(bank, onT[s][:, kc, :], Wo[:, kc, half * 512:(half + 1) * 512], start=(kc == 0), stop=(kc == 7))
                        kb.tt("dve", hpt[ti][:, half * 512:(half + 1) * 512], bank, ht[s][:, half * 512:(half + 1) * 512], ALU.add)
                    kb.act(junk, hpt[ti], AF.Square, accum=ss1[s])
                    kb.act(rm1[s], ss1[s], AF.Sqrt, bias=kb.epsb, scale=1.0 / D)
                    kb.recip(rs1[s], rm1[s])
                    if moe:
                        kb.stt(v32[s], hpt[ti], rs1[s], gF, ALU.mult, ALU.mult)
                        kb.copy("pool", vb_[s], v32[s])
                    else:
                        kb.stt(vb_[s], hpt[ti], rs1[s], gF, ALU.mult, ALU.mult)
                    for kc in range(8):
                        kb.tr(b0[:, kc * 128:(kc + 1) * 128], vb_[s][:, kc * 128:(kc + 1) * 128], self.ident_bf)
                    kb.copy("act", vT[:, :, ti * 128:(ti + 1) * 128], b0.v(b0.ap.rearrange("p (a b) -> p a b", b=128)))
                    if moe:
                        for kc in range(8):
                            kb.tr(bk[2 + kc // 4][:, (kc % 4) * 128:(kc % 4 + 1) * 128], v32[s][:, kc * 128:(kc + 1) * 128], self.ident_f)
                        kb.copy("dve", v32T[:, 0:4, :], bk[2].v(bk[2].ap.rearrange("p (a b) -> p a b", b=128)))
                        kb.copy("act", v32T[:, 4:8, :], bk[3].v(bk[3].ap.rearrange("p (a b) -> p a b", b=128)))
                        for kc in range(8):
                            kb.mm(bk[1][:, 0:NE], v32T[:, kc, :], rw[:, kc, :], start=(kc == 0), stop=(kc == 7))
                        kb.copy("dve", lg[s], bk[1][:, 0:NE])
                        a_, b_ = lg[s].ap, m8[s].ap
                        kb.op("dve", lambda e, a_=a_, b_=b_: e.max(out=b_, in_=a_), [m8[s]], [lg[s]])
                        kb.ts("dve", msk[s], lg[s], m8[s][:, 1:2], ALU.is_ge)
                        kb.ts("dve", nm1[s], m8[s][:, 0:1], -1.0, ALU.mult)
                        kb.act(ex[s], lg[s], AF.Exp, bias=nm1[s])
                        kb.tt("dve", gu[s], ex[s], msk[s], ALU.mult)
                        kb.reduce(dn[s], gu[s])
                        kb.recip(rdn[s], dn[s])
                        kb.ts("dve", gate[:, ti, :], gu[s], rdn[s], ALU.mult)
                for pa in (0, 2):
                    la = kb.c.record(lambda: tile_body(pa))
                    lb = kb.c.record(lambda: tile_body(pa + 1))
                    kb.c.play(la, lb)
                for (Wg, Wu, Wd, e) in experts:
                    stages = []
                    wgv = Wg.v(Wg.ap.rearrange("(kc p) f -> p kc f", p=128))
                    wuv = Wu.v(Wu.ap.rearrange("(kc p) f -> p kc f", p=128))
                    wdv = Wd.v(Wd.ap.rearrange("(fc p) n -> p fc n", p=128))
                    for fb in range(nfb):
                        def ld(fb=fb, wgv=wgv, wuv=wuv):
                            res = []
                            for src in (wgv, wuv):
                                k = cnt["ld"]
                                cnt["ld"] += 1
                                st = stg[k % 2]
                                w = wgu[k % 4]
                                kb.dma(st, src[:, :, fb * 256:(fb + 1) * 256], sems[5 + k % 2])
                                kb.copy("act" if k % 2 == 0 else "dve", w, st)
                                res.append(w)
                            return res

                        def comp(ws, fb=fb):
                            wg_, wu_ = ws
                            for j in range(2):
                                fc = fb * 2 + j
                                k = cnt["fc"]
                                cnt["fc"] += 1
                                gb, ub = bk[(k % 2) * 2], bk[(k % 2) * 2 + 1]
                                for kc in range(8):
                                    kb.mm(gb, wg_[:, kc, j * 128:(j + 1) * 128], vT[:, kc, :], start=(kc == 0), stop=(kc == 7))
                                for kc in range(8):
                                    kb.mm(ub, wu_[:, kc, j * 128:(j + 1) * 128], vT[:, kc, :], start=(kc == 0), stop=(kc == 7))
                                kb.act(sg[k % 2], gb, AF.Silu)
                                kb.tt("dve", hidc[fc], sg[k % 2], ub, ALU.mult)
                        stages.append((ld, comp))
                    ngr = (nfc + 3) // 4
                    for half in range(2):
                        for fg in range(ngr):
                            f0 = fg * 4
                            nf = min(4, nfc - f0)

                            def ld(half=half, f0=f0, nf=nf, wdv=wdv):
                                k = cnt["ldd"]
                                cnt["ldd"] += 1
                                st = stgd[k % 2]
                                w = wd[k % 2]
                                kb.dma(st[:, 0:nf, :], wdv[:, f0:f0 + nf, half * 512:(half + 1) * 512], sems[7 + k % 2])
                                kb.copy("act" if k % 2 == 0 else "dve", w[:, 0:nf, :], st[:, 0:nf, :])
                                return w

                            def comp(w, half=half, f0=f0, nf=nf, fg=fg, e=e):
                                for j in range(nf):
                                    fc = f0 + j
                                    for ti in range(4):
                                        kb.mm(bk[4 + ti], hidc[fc][:, ti * 128:(ti + 1) * 128], w[:, j, :], start=(fc == 0), stop=(fc == nfc - 1))
                                if fg == ngr - 1:
                                    for ti in range(4):
                                        dst = hpt[ti][:, half * 512:(half + 1) * 512]
                                        if e is None:
                                            kb.tt("dve", dst, bk[4 + ti], dst, ALU.add)
                                        else:
                                            kb.stt(dst, bk[4 + ti], gate[:, ti, e:e + 1], dst, ALU.mult, ALU.add)
                            stages.append((ld, comp))
                    nxt = stages[0][0]()
                    for i in range(len(stages)):
                        cur = nxt
                        if i + 1 < len(stages):
                            nxt = stages[i + 1][0]()
                        stages[i][1](cur)
                for ti in range(4):
                    kb.dma(hov[:, blk * 4 + ti, :], hpt[ti], sems[9], q="pool", pw=True)
            kb.c.barrier()
            kb.c.release(sems)

    def phase_moe(self, layer, hin_dram, hout_dram):
        kb = self.kb
        c = kb.c
        I = self.inp
        bk = self.banks
        li = layer // 2
        dff = DFE
        nfc = dff // 128
        nfb = dff // 256
        XE = self.scr["XE"]
        YE = self.scr["YE"]
        HP = self.scr["HP"]
        with contextlib.ExitStack() as es0:
            idx_all = kb.sb(es0, [128, NT, 2], I32, "idx_all")
            g_all = kb.sb(es0, [128, NT, 2], F32, "g_all")
            run = kb.sb(es0, [128, NE], F32, "run")
            flags_i = kb.sb(es0, [128, NE * 8], I32, "flags_i")
            HPv = HP.v(HP.ap.rearrange("(t p) d -> t p d", p=128))
            with contextlib.ExitStack() as es:
                Wo = kb.sb(es, [128, 8, D], BF16, "Wo")
                gO = kb.sb(es, [128, D], F32, "gO")
                gF = kb.sb(es, [128, D], F32, "gF")
                ecst = kb.sb(es, [128, NE], F32, "ecst")
                utri = kb.sb(es, [128, 128], BF16, "utri")
                ones_bf = kb.sb(es, [128, 128], BF16, "ones_bf")
                jthr = kb.sb(es, [128, NE * 8], F32, "jthr")
                rw = kb.sb(es, [128, 8, NE], F32, "rw")
                Ot = kb.sbn(es, 2, [128, D], F32, "Ot")
                ht = kb.sbn(es, 2, [128, D], F32, "ht")
                hpt = kb.sbn(es, 2, [128, D], F32, "hpt")
                junk = kb.sb(es, [128, D], F32, "junk")
                on = kb.sbn(es, 2, [128, D], BF16, "on")
                onT = kb.sbn(es, 2, [128, 8, 128], BF16, "onT")
                vb_ = kb.sbn(es, 2, [128, D], BF16, "vb_")
                v32 = kb.sbn(es, 2, [128, D], F32, "v32")
                v32T = kb.sbn(es, 2, [128, 8, 128], F32, "v32T")
                ss2 = kb.sbn(es, 2, [128, 2], F32, "ss2")
                rm2 = kb.sbn(es, 2, [128, 2], F32, "rm2")
                rs2 = kb.sbn(es, 2, [128, 2], F32, "rs2")
                ss1 = kb.sbn(es, 2, [128, 1], F32, "ss1")
                rm1 = kb.sbn(es, 2, [128, 1], F32, "rm1")
                rs1 = kb.sbn(es, 2, [128, 1], F32, "rs1")
                lg = kb.sbn(es, 2, [128, NE], F32, "lg")
                m8 = kb.sbn(es, 2, [128, 8], F32, "m8")
                msk = kb.sbn(es, 2, [128, NE], F32, "msk")
                mskb = kb.sbn(es, 2, [128, NE], BF16, "mskb")
                nm1 = kb.sbn(es, 2, [128, 1], F32, "nm1")
                ex = kb.sbn(es, 2, [128, NE], F32, "ex")
                gu = kb.sbn(es, 2, [128, NE], F32, "gu")
                gate = kb.sbn(es, 2, [128, NE], F32, "gate")
                dn = kb.sbn(es, 2, [128, 1], F32, "dn")
                rdn = kb.sbn(es, 2, [128, 1], F32, "rdn")
                flat = kb.sbn(es, 2, [128, NE], F32, "flat")
                A = kb.sbn(es, 2, [128, NE], F32, "A")
                Bm = kb.sbn(es, 2, [128, NE], F32, "Bm")
                eq = kb.sbn(es, 2, [128, NE], F32, "eq")
                f2 = kb.sbn(es, 2, [128, 2], F32, "f2")
                amax = kb.sbn(es, 2, [128, 1], F32, "amax")
                bmax = kb.sbn(es, 2, [128, 1], F32, "bmax")
                nrep = kb.sb(es, [128, NE * 8], F32, "nrep")
                flags_f = kb.sb(es, [128, NE * 8], F32, "flags_f")
                sems = [kb.c.dma_sem() for _ in range(10)]
                wov = I["w_out"].v(I["w_out"].ap[layer].rearrange("(kc p) n -> p kc n", p=128))
                for kc in range(8):
                    kb.dma(Wo[:, kc, :], wov[:, kc, :], sems[0], q="pool", pw=True)
                kb.dma(gO, Tl(I["out_norm"].ap[layer].partition_broadcast(128), I["out_norm"].buf), sems[0])
                kb.dma(gF, Tl(I["ffn_norm"].ap[layer].partition_broadcast(128), I["ffn_norm"].buf), sems[0])
                kb.dma(rw, I["router_w"].v(I["router_w"].ap[li].rearrange("(kc p) e -> p kc e", p=128)), sems[0])
                kb.dma(ecst, I["ecst"], sems[0])
                kb.dma(utri, I["utri"], sems[0])
                kb.dma(ones_bf, I["ones_bf"], sems[0])
                kb.dma(jthr, I["jthr"], sems[0])
                kb.memset("dve", run, 0.0)
                kb.c.barrier()
                Ov = self.scr["O"].v(self.scr["O"].ap.rearrange("(t p) c -> t p c", p=128))
                hv = hin_dram.v(hin_dram.ap.rearrange("(t p) d -> t p d", p=128))
                def part1(t):
                    s = t % 2
                    kb.dma(Ot[s], Ov[t], sems[1 + s])
                    kb.dma(ht[s], hv[t], sems[3 + s])
                    o = Ot[s]
                    kb.act(junk[:, 0:512], o[:, 0:512], AF.Square, accum=ss2[s][:, 0:1])
                    kb.act(junk[:, 512:1024], o[:, 512:1024], AF.Square, accum=ss2[s][:, 1:2])
                    kb.act(rm2[s], ss2[s], AF.Sqrt, bias=kb.epsb, scale=1.0 / 512)
                    kb.recip(rs2[s], rm2[s])
                    for gq in range(2):
                        cs = slice(gq * 512, (gq + 1) * 512)
                        kb.stt(on[s][:, cs], o[:, cs], rs2[s][:, gq:gq + 1], gO[:, cs], ALU.mult, ALU.mult)
                    b0 = bk[s].v(bk[s].ap.bitcast(BF16))
                    for kc in range(8):
                        kb.tr(b0[:, kc * 128:(kc + 1) * 128], on[s][:, kc * 128:(kc + 1) * 128], self.ident_bf)
                    kb.copy("act", onT[s], b0.v(b0.ap.rearrange("p (a b) -> p a b", b=128)))
                    hp = hpt[s]
                    for half in range(2):
                        bank = bk[2 + s]
                        for kc in range(8):
                            kb.mm(bank, onT[s][:, kc, :], Wo[:, kc, half * 512:(half + 1) * 512], start=(kc == 0), stop=(kc == 7))
                        kb.tt("dve", hp[:, half * 512:(half + 1) * 512], bank, ht[s][:, half * 512:(half + 1) * 512], ALU.add)
                    kb.dma(HPv[t], hp, sems[5 + s], q="pool")
                    kb.act(junk, hp, AF.Square, accum=ss1[s])
                    kb.act(rm1[s], ss1[s], AF.Sqrt, bias=kb.epsb, scale=1.0 / D)
                    kb.recip(rs1[s], rm1[s])
                    kb.stt(v32[s], hp, rs1[s], gF, ALU.mult, ALU.mult)
                    kb.copy("pool", vb_[s], v32[s])
                    for kc in range(8):
                        kb.tr(bk[4 + s][:, (kc % 4) * 128:(kc % 4 + 1) * 128], v32[s][:, kc * 128:(kc + 1) * 128], self.ident_f)
                        if kc % 4 == 3:
                            kb.copy("dve" if kc == 3 else "act", v32T[s][:, kc - 3:kc + 1, :], bk[4 + s].v(bk[4 + s].ap.rearrange("p (a b) -> p a b", b=128)))
                    for kc in range(8):
                        kb.mm(bk[6 + s][:, 0:NE], v32T[s][:, kc, :], rw[:, kc, :], start=(kc == 0), stop=(kc == 7))
                    kb.copy("dve", lg[s], bk[6 + s][:, 0:NE])
                    a_, b_ = lg[s].ap, m8[s].ap
                    kb.op("dve", lambda e, a_=a_, b_=b_: e.max(out=b_, in_=a_), [m8[s]], [lg[s]])
                    kb.ts("dve", msk[s], lg[s], m8[s][:, 1:2], ALU.is_ge)
                    kb.copy("dve", mskb[s], msk[s])
                    kb.ts("dve", nm1[s], m8[s][:, 0:1], -1.0, ALU.mult)
                    kb.act(ex[s], lg[s], AF.Exp, bias=nm1[s])
                    kb.tt("dve", gu[s], ex[s], msk[s], ALU.mult)
                    kb.reduce(dn[s], gu[s])
                    kb.recip(rdn[s], dn[s])
                    kb.ts("dve", gate[s], gu[s], rdn[s], ALU.mult)
                def part2(t):
                    s = t % 2
                    kb.mm(bk[6][:, 0:NE], utri, mskb[s])
                    kb.mm(bk[7][:, 0:NE], ones_bf, mskb[s])
                    kb.tt("dve", flat[s], bk[6][:, 0:NE], run, ALU.add)
                    kb.tt("dve", run, bk[7][:, 0:NE], run, ALU.add)
                    kb.tt("dve", flat[s], flat[s], ecst, ALU.add)
                    kb.stt(A[s], flat[s], 1.0, msk[s], ALU.add, ALU.mult)
                    kb.reduce(amax[s], A[s], op=ALU.max)
                    kb.ts("dve", f2[s][:, 0:1], amax[s], -1.0, ALU.add)
                    kb.ts("dve", Bm[s], flat[s], -1.0, ALU.mult, 40000.0, ALU.add)
                    kb.tt("dve", Bm[s], Bm[s], msk[s], ALU.mult)
                    kb.reduce(bmax[s], Bm[s], op=ALU.max)
                    kb.ts("dve", f2[s][:, 1:2], bmax[s], -1.0, ALU.mult, 40000.0, ALU.add)
                    kb.copy("dve", idx_all[:, t, :], f2[s])
                    kb.ts("dve", eq[s], A[s], amax[s], ALU.is_equal)
                    kb.tt("dve", eq[s], eq[s], gate[s], ALU.mult)
                    kb.reduce(g_all[:, t, 0:1], eq[s])
                    kb.ts("dve", g_all[:, t, 1:2], g_all[:, t, 0:1], -1.0, ALU.mult, 1.0, ALU.add)
                    for k in range(2):
                        xo, ia, va = XE.ap, idx_all.ap[:, t, k:k + 1], vb_[s].ap
                        c.emit("pool", lambda e, xo=xo, ia=ia, va=va: e.indirect_dma_start(
                            out=xo, out_offset=bass.IndirectOffsetOnAxis(ap=ia, axis=0), in_=va, in_offset=None),
                            reads=[idx_all.buf, vb_[s].buf], pwrites=[XE.buf], dsem=sems[7 + s])
                for t in range(0, NT, 2):
                    la = kb.c.record(lambda: part1(t))
                    lb = kb.c.record(lambda: part1(t + 1))
                    kb.c.play(la, lb)
                    part2(t)
                    part2(t + 1)
                kb.copy("dve", nrep.v(nrep.ap.rearrange("p (e j) -> p e j", j=8)), run.v(bc_last(run.ap, 8)))
                kb.tt("dve", flags_f, nrep, jthr, ALU.is_gt)
                kb.copy("dve", flags_i, flags_f)
                kb.c.barrier()
                kb.c.release(sems)
            with contextlib.ExitStack() as es:
                xt_ = kb.sbn(es, 4, [128, D], BF16, "xt_")
                XT = kb.sb(es, [128, 8, 512], BF16, "XT")
                hid = kb.sb(es, [128, nfc, 512], BF16, "hid")
                hidc = [Tl(hid.ap[:, i, :]) for i in range(nfc)]
                stg = kb.sbn(es, 2, [128, 8, 256], F32, "stg")
                wgu = kb.sbn(es, 4, [128, 8, 256], BF16, "wgu")
                stgd = kb.sbn(es, 2, [128, 4, 512], F32, "stgd")
                wd = kb.sbn(es, 2, [128, 4, 512], BF16, "wd")
                sg = kb.sbn(es, 2, [128, 512], F32, "sg")
                yst = kb.sbn(es, 2, [128, 4, 512], F32, "yst")
                sems = [kb.c.dma_sem() for _ in range(8)]
                cnt = {"ld": 0, "ldd": 0, "fc": 0, "x": 0, "y": 0}
                XEv = XE.v(XE.ap.rearrange("(t p) d -> t p d", p=128))
                YEv = YE.v(YE.ap.rearrange("(t p) d -> p t d", p=128))
                for e in range(NE):
                    Wg, Wu, Wd = I["exp_w_gate"][li, e], I["exp_w_up"][li, e], I["exp_w_down"][li, e]
                    wgv = Wg.v(Wg.ap.rearrange("(kc p) f -> p kc f", p=128))
                    wuv = Wu.v(Wu.ap.rearrange("(kc p) f -> p kc f", p=128))
                    wdv = Wd.v(Wd.ap.rearrange("(fc p) n -> p fc n", p=128))
                    for j in range(8):
                        c.cond_begin(flags_i.ap[0:1, e * 8 + j:e * 8 + j + 1], flags_i.buf)
                        t0 = (e * S + j * 512) // 128
                        b0 = bk[0].v(bk[0].ap.bitcast(BF16))
                        for ti in range(4):
                            kb.dma(xt_[ti], XEv[t0 + ti], sems[ti % 2])
                        for ti in range(4):
                            bt = bk[ti % 2].v(bk[ti % 2].ap.bitcast(BF16))
                            for kc in range(8):
                                kb.tr(bt[:, kc * 128:(kc + 1) * 128], xt_[ti][:, kc * 128:(kc + 1) * 128], self.ident_bf)
                            kb.copy("act" if ti % 2 == 0 else "dve", XT[:, :, ti * 128:(ti + 1) * 128], bt.v(bt.ap.rearrange("p (a b) -> p a b", b=128)))
                        stages = []
                        for fb in range(nfb):
                            def ld(fb=fb, wgv=wgv, wuv=wuv):
                                res = []
                                for src in (wgv, wuv):
                                    k = cnt["ld"]
                                    cnt["ld"] += 1
                                    st = stg[k % 2]
                                    w = wgu[k % 4]
                                    kb.dma(st, src[:, :, fb * 256:(fb + 1) * 256], sems[2 + k % 2])
                                    kb.copy("act" if k % 2 == 0 else "dve", w, st)
                                    res.append(w)
                                return res

                            def comp(ws, fb=fb):
                                wg_, wu_ = ws
                                for jj in range(2):
                                    fc = fb * 2 + jj
                                    k = cnt["fc"]
                                    cnt["fc"] += 1
                                    gb, ub = bk[(k % 2) * 2], bk[(k % 2) * 2 + 1]
                                    for kc in range(8):
                                        kb.mm(gb, wg_[:, kc, jj * 128:(jj + 1) * 128], XT[:, kc, :], start=(kc == 0), stop=(kc == 7))
                                    for kc in range(8):
                                        kb.mm(ub, wu_[:, kc, jj * 128:(jj + 1) * 128], XT[:, kc, :], start=(kc == 0), stop=(kc == 7))
                                    kb.act(sg[k % 2], gb, AF.Silu)
                                    kb.tt("dve", hidc[fc], sg[k % 2], ub, ALU.mult)
                            stages.append((ld, comp))
                        ngr = (nfc + 3) // 4
                        for half in range(2):
                            for fg in range(ngr):
                                f0 = fg * 4
                                nf = min(4, nfc - f0)

                                def ld(half=half, f0=f0, nf=nf, wdv=wdv):
                                    k = cnt["ldd"]
                                    cnt["ldd"] += 1
                                    st = stgd[k % 2]
                                    w = wd[k % 2]
                                    kb.dma(st[:, 0:nf, :], wdv[:, f0:f0 + nf, half * 512:(half + 1) * 512], sems[4 + k % 2])
                                    kb.copy("act" if k % 2 == 0 else "dve", w[:, 0:nf, :], st[:, 0:nf, :])
                                    return w

                                def comp(w, half=half, f0=f0, nf=nf, fg=fg, t0=t0):
                                    for jj in range(nf):
                                        fc = f0 + jj
                                        for ti in range(4):
                                            kb.mm(bk[4 + ti], hidc[fc][:, ti * 128:(ti + 1) * 128], w[:, jj, :], start=(fc == 0), stop=(fc == nfc - 1))
                                    if fg == ngr - 1:
                                        k = cnt["y"]
                                        cnt["y"] += 1
                                        y = yst[k % 2]
                                        for ti in range(4):
                                            kb.copy("act" if ti % 2 == 0 else "dve", y[:, ti, :], bk[4 + ti])
                                        kb.dma(YEv[:, t0:t0 + 4, half * 512:(half + 1) * 512], y, sems[6 + k % 2], q="pool", pw=True)
                                stages.append((ld, comp))
                        nxt = stages[0][0]()
                        for i in range(len(stages)):
                            cur = nxt
                            if i + 1 < len(stages):
                                nxt = stages[i + 1][0]()
                            stages[i][1](cur)
                        c.cond_end()
                kb.c.barrier()
                kb.c.release(sems)
            with contextlib.ExitStack() as es:
                hpb = kb.sbn(es, 2, [128, D], F32, "hpb")
                yh = kb.sbn(es, 2, [128, D], F32, "yh")
                yl = kb.sbn(es, 2, [128, D], F32, "yl")
                ob = kb.sbn(es, 2, [128, D], F32, "ob")
                sems = [kb.c.dma_sem() for _ in range(8)]
                hov = hout_dram.v(hout_dram.ap.rearrange("(t p) d -> t p d", p=128))
                for t in range(NT):
                    s = t % 2
                    kb.dma(hpb[s], HPv[t], sems[s])
                    for k, dst in enumerate((yh[s], yl[s])):
                        yi, ia, da = YE.ap, idx_all.ap[:, t, k:k + 1], dst.ap
                        c.emit("pool", lambda e, yi=yi, ia=ia, da=da: e.indirect_dma_start(
                            out=da, out_offset=None, in_=yi, in_offset=bass.IndirectOffsetOnAxis(ap=ia, axis=0)),
                            reads=[idx_all.buf, YE.buf], writes=[dst.buf], dsem=sems[2 + 2 * k + s])
                    kb.stt(ob[s], yh[s], g_all[:, t, 0:1], hpb[s], ALU.mult, ALU.add)
                    kb.stt(ob[s], yl[s], g_all[:, t, 1:2], ob[s], ALU.mult, ALU.add)
                    kb.dma(hov[t], ob[s], sems[6 + s], q="act", pw=True)
                kb.c.barrier()
                kb.c.release(sems)

    def build_all(self):
        self.declare()
        self.setup_globals()
        self.phase_tables()
        hin = self.inp["x"]
        for layer in range(DEPTH):
            hout = self.scr["H1"] if layer == 0 else self.out
            self.phase_inproj(layer, hin)
            self.phase_compress(layer)
            self.phase_nsa(layer)
            self.phase_dil(layer)
            if layer % 2 == 1:
                self.phase_moe(layer, hin, hout)
            else:
                self.phase_ffn(layer, hin, hout)
            hin = hout
        self.kb.c.barrier()
        self.kb.c.flush()
        return self.nc


INPUT_NAMES = ["x", "rel_bias", "attn_norm", "w_in", "nsa_q_norm", "nsa_k_norm", "cmp_pos", "cmp_w1", "cmp_b1", "cmp_w2",
               "dil_q_norm", "dil_k_norm", "out_norm", "w_out", "ffn_norm", "ffn_w_gate", "ffn_w_up", "ffn_w_down",
               "router_w", "exp_w_gate", "exp_w_up", "exp_w_down"]


def make_in_maps(inputs, cores):
    cst = host_consts()
    maps = []
    shared = {k: np.ascontiguousarray(np.asarray(inputs[k], dtype=np.float32)) for k in INPUT_NAMES if k != "x"}
    x = np.asarray(inputs["x"], dtype=np.float32)
    for b in cores:
        m = dict(shared)
        m["x"] = np.ascontiguousarray(x[b])
        m.update(cst)
        maps.append(m)
    return maps


_CACHE = {}


def kernel(**inputs):
    cores = list(range(8))
    if "nc" not in _CACHE:
        _CACHE["nc"] = Prog().build_all()
    nc = _CACHE["nc"]
    maps = make_in_maps(inputs, cores)
    res = run_bass_kernel_spmd(nc, maps, core_ids=cores)
    out = np.stack([np.asarray(r["out"], dtype=np.float32) for r in res.results], axis=0)
    return out
```

```python
import contextlib
import numpy as np
import ml_dtypes
import concourse.bass as bass
import concourse.mybir as mybir
from concourse.ap import AP
from concourse.bass_utils import run_bass_kernel_spmd

F32 = mybir.dt.float32
BF16 = mybir.dt.bfloat16
I32 = mybir.dt.int32
AF = mybir.ActivationFunctionType
ALU = mybir.AluOpType
AX = mybir.AxisListType

S = 4096
D = 1024
NT = 32
DEPTH = 2
INW = 2840
DFF = 2816
DFE = 3584
NE = 8
EPS = 1e-6
XA = 3072
XC = 6144
C_QA, C_KC, C_VC, C_KS, C_VS, C_KW, C_VW, C_GA, C_QB, C_KB, C_VB = 0, 512, 640, 768, 896, 1024, 1152, 1280, 1304, 1816, 2328


class Buf:
    __slots__ = ("name", "writers", "readers")

    def __init__(self, name=""):
        self.name = name
        self.writers = []
        self.readers = []


class Tl:
    __slots__ = ("ap", "buf", "psum")

    def __init__(self, ap, buf=None, psum=False):
        self.ap = ap
        self.buf = buf if buf is not None else Buf()
        self.psum = psum

    def __getitem__(self, k):
        return Tl(self.ap[k], self.buf, self.psum)

    def v(self, ap):
        return Tl(ap, self.buf, self.psum)


class Ctx:
    COMPUTE = ("pe", "act", "dve", "pool")

    def __init__(self, nc):
        self.nc = nc
        self.ops = {e: [] for e in ("pe", "act", "dve", "pool", "sp")}
        self.sems = {}
        self.count = {}
        self.known = {e: {} for e in self.ops}
        for e in self.COMPUTE:
            self.sems[e] = nc.alloc_semaphore(name=f"sem_{e}")
            self.count[e] = 0
        self.free_dsems = []
        self.nsem = 0
        self.region = None
        self.rec = None

    def dma_sem(self):
        if self.free_dsems:
            return self.free_dsems.pop()
        self.nsem += 1
        k = f"dma{self.nsem}"
        self.sems[k] = self.nc.alloc_semaphore(name=k)
        self.count[k] = 0
        return k

    def release(self, ks):
        self.free_dsems.extend(ks)

    def record(self, body):
        assert self.rec is None
        self.rec = []
        body()
        r, self.rec = self.rec, None
        return r

    def play(self, *lists):
        n = max(len(l) for l in lists)
        for i in range(n):
            for l in lists:
                if i < len(l):
                    self.emit(*l[i])

    def emit(self, eng, fn, reads=(), writes=(), pwrites=(), dsem=None):
        if self.rec is not None:
            self.rec.append((eng, fn, list(reads), list(writes), list(pwrites), dsem))
            return None
        deps = {}

        def add(ev, kind):
            sk, v = ev
            if sk == eng:
                if eng == "pe" or kind != "raw":
                    return
            if deps.get(sk, 0) < v:
                deps[sk] = v

        for b in reads:
            for ev in b.writers:
                add(ev, "raw")
        for b in writes:
            for ev in b.writers:
                add(ev, "waw")
            for ev in b.readers:
                add(ev, "war")
        for b in pwrites:
            for ev in b.readers:
                add(ev, "war")
        waits = []
        kn = self.known[eng]
        for sk, v in deps.items():
            if sk not in self.COMPUTE:
                v = self.count[sk]
            if kn.get(sk, 0) < v:
                kn[sk] = v
                waits.append((sk, v))
        if dsem is not None:
            self.count[dsem] += 16
            ev = (dsem, self.count[dsem])
            inc = (dsem, 16)
        else:
            self.count[eng] += 1
            ev = (eng, self.count[eng])
            inc = (eng, 1)
        self.ops[eng].append((waits, fn, inc))
        if self.region is not None and dsem is not None:
            self.region["dq"].setdefault((eng, dsem), 0)
            self.region["dq"][(eng, dsem)] += 16
        for b in reads:
            b.readers.append(ev)
            if len(b.readers) > 48:
                b.readers = self._compact(b.readers)
        for b in writes:
            b.writers = [ev]
            b.readers = []
        for b in pwrites:
            b.writers.append(ev)
            if len(b.writers) > 48:
                b.writers = self._compact(b.writers)
        return ev

    @staticmethod
    def _compact(evs):
        d = {}
        for sk, v in evs:
            if d.get(sk, 0) < v:
                d[sk] = v
        return list(d.items())

    def barrier(self):
        for eng in self.ops:
            waits = []
            kn = self.known[eng]
            for sk, v in self.count.items():
                if v > 0 and sk != eng and kn.get(sk, 0) < v:
                    kn[sk] = v
                    waits.append((sk, v))
            if waits:
                self.ops[eng].append((waits, None, None))

    def cond_begin(self, flag_ap, flag_buf):
        assert self.region is None
        self.region = {"start": dict(self.count), "known": {e: dict(k) for e, k in self.known.items()}, "dq": {}}
        for eng in self.ops:
            waits = []
            kn = self.known[eng]
            for sk, v in flag_buf.writers:
                if sk not in self.COMPUTE:
                    v = self.count[sk]
                if kn.get(sk, 0) < v:
                    kn[sk] = v
                    waits.append((sk, v))
            self.ops[eng].append(("begin", waits, flag_ap))

    def cond_end(self):
        r = self.region
        self.region = None
        for eng in self.ops:
            fix = []
            if eng in self.COMPUTE:
                n = self.count[eng] - r["start"][eng]
                if n > 0:
                    fix.append((eng, r["start"][eng], n))
            for (q, dsem), n in r["dq"].items():
                if q == eng:
                    fix.append((dsem, r["start"].get(dsem, 0), n))
            self.ops[eng].append(("end", fix, None))
            self.known[eng] = r["known"][eng]

    def flush(self):
        nc = self.nc
        sems = self.sems
        with nc.Block() as block:
            def mk(ename):
                def run(engine):
                    ops = self.ops[ename]
                    stack = []
                    i = 0
                    while i < len(ops):
                        a, b, c3 = ops[i]
                        if isinstance(a, str) and a == "begin":
                            for sk, v in b:
                                engine.wait_ge(sems[sk], v)
                            if isinstance(ops[i + 1][0], str) and ops[i + 1][0] == "end" and not ops[i + 1][1]:
                                i += 2
                                continue
                            val = engine.value_load(c3)
                            guard = engine.If(val)
                            guard.__enter__()
                            stack.append((guard, val))
                        elif isinstance(a, str) and a == "end":
                            guard, val = stack.pop()
                            guard.__exit__(None, None, None)
                            with engine.Else():
                                for sk, prior, n in b:
                                    if prior > 0:
                                        engine.wait_ge(sems[sk], prior)
                                    engine.sem_inc(sems[sk], n)
                            engine.free_register(val.val)
                        else:
                            for sk, v in a:
                                engine.wait_ge(sems[sk], v)
                            if b is not None:
                                b(engine).then_inc(sems[c3[0]], c3[1])
                        i += 1
                return run
            block.tensor(mk("pe"))
            block.scalar(mk("act"))
            block.vector(mk("dve"))
            block.gpsimd(mk("pool"))
            block.sync(mk("sp"))


def bc_last(ap, n):
    return AP(ap.tensor, ap.offset, [list(x) for x in ap.ap] + [[0, n]])


def bc_mid(ap, nh):
    a = [list(x) for x in ap.ap]
    return AP(ap.tensor, ap.offset, [a[0], [0, nh]] + a[1:])


class KB:
    def __init__(self, nc):
        self.nc = nc
        self.c = Ctx(nc)
        self.uid = 0

    def name(self, p):
        self.uid += 1
        return f"{p}_{self.uid}"

    def sb(self, es, shape, dt, name="t"):
        h = es.enter_context(self.nc.sbuf_tensor(self.name(name), list(shape), dt))
        return Tl(h.ap())

    def sbn(self, es, n, shape, dt, name="t"):
        return [self.sb(es, shape, dt, name) for _ in range(n)]

    def _rw(self, outs, ins):
        reads, writes = [], []
        for t in ins:
            if t is None:
                continue
            (writes if t.psum else reads).append(t.buf)
        for t in outs:
            writes.append(t.buf)
        return reads, writes

    def op(self, eng, fn, outs, ins, pw=()):
        reads, writes = self._rw(outs, ins)
        pwb = [t.buf for t in pw]
        return self.c.emit(eng, fn, reads=reads, writes=writes, pwrites=pwb)

    def dma(self, out, in_, sem, q="sp", pw=False):
        o, i = out.ap, in_.ap
        reads = [in_.buf]
        if pw:
            return self.c.emit(q, lambda e: e.dma_start(out=o, in_=i), reads=reads, pwrites=[out.buf], dsem=sem)
        return self.c.emit(q, lambda e: e.dma_start(out=o, in_=i), reads=reads, writes=[out.buf], dsem=sem)

    def mm(self, out, lhsT, rhs, start=True, stop=True):
        o, l, r = out.ap, lhsT.ap, rhs.ap
        return self.op("pe", lambda e: e.matmul(o, lhsT=l, rhs=r, start=start, stop=stop, skip_group_check=True), [out], [lhsT, rhs])

    def tr(self, out, in_, ident):
        o, i, d = out.ap, in_.ap, ident.ap
        return self.op("pe", lambda e: e.transpose(out=o, in_=i, identity=d), [out], [in_, ident])

    def act(self, out, in_, func, bias=None, scale=None, accum=None, eng="act"):
        o, i = out.ap, in_.ap
        kw = {}
        ins = [in_]
        if bias is not None:
            if isinstance(bias, Tl):
                kw["bias"] = bias.ap
                ins.append(bias)
            else:
                kw["bias"] = bias
        if scale is not None:
            if isinstance(scale, Tl):
                kw["scale"] = scale.ap
                ins.append(scale)
            else:
                kw["scale"] = scale
        outs = [out]
        if accum is not None:
            kw["accum_out"] = accum.ap
            outs.append(accum)
        return self.op("act", lambda e: e.activation(out=o, in_=i, func=func, **kw), outs, ins)

    def tt(self, eng, out, in0, in1, op):
        o, a, b = out.ap, in0.ap, in1.ap
        return self.op(eng, lambda e: e.tensor_tensor(out=o, in0=a, in1=b, op=op), [out], [in0, in1])

    def ts(self, eng, out, in0, s1, op0, s2=None, op1=None):
        o, a = out.ap, in0.ap
        ins = [in0]
        v1 = s1
        if isinstance(s1, Tl):
            ins.append(s1)
            v1 = s1.ap
        v2 = s2
        if isinstance(s2, Tl):
            ins.append(s2)
            v2 = s2.ap
        if op1 is None:
            return self.op(eng, lambda e: e.tensor_scalar(out=o, in0=a, scalar1=v1, scalar2=None, op0=op0), [out], ins)
        return self.op(eng, lambda e: e.tensor_scalar(out=o, in0=a, scalar1=v1, scalar2=v2, op0=op0, op1=op1), [out], ins)

    def stt(self, out, in0, scalar, in1, op0, op1):
        o, a, b = out.ap, in0.ap, in1.ap
        ins = [in0, in1]
        sv = scalar
        if isinstance(scalar, Tl):
            ins.append(scalar)
            sv = scalar.ap
        return self.op("dve", lambda e: e.scalar_tensor_tensor(out=o, in0=a, scalar=sv, in1=b, op0=op0, op1=op1), [out], ins)

    def copy(self, eng, out, in_):
        o, i = out.ap, in_.ap
        if eng == "act":
            return self.op("act", lambda e: e.copy(out=o, in_=i), [out], [in_])
        return self.op(eng, lambda e: e.tensor_copy(out=o, in_=i), [out], [in_])

    def memset(self, eng, out, val):
        o = out.ap
        return self.op(eng, lambda e: e.memset(o, val), [out], [])

    def recip(self, out, in_):
        o, i = out.ap, in_.ap
        return self.op("dve", lambda e: e.reciprocal(out=o, in_=i), [out], [in_])

    def reduce(self, out, in_, op=ALU.add):
        o, i = out.ap, in_.ap
        return self.op("dve", lambda e: e.tensor_reduce(out=o, in_=i, axis=AX.X, op=op), [out], [in_])

    def rstd(self, es_tmp, out, ssq, n, tmp):
        self.act(tmp, ssq, AF.Sqrt, bias=self.epsb[0:ssq.ap.shape[0], :], scale=1.0 / n)
        self.recip(out, tmp)


def t5_bucket_np(dist):
    dist = np.maximum(dist, 0)
    max_exact = 16
    scaled = np.log(np.maximum(dist, 1).astype(np.float32) / np.float32(max_exact)) / np.float32(np.log(2048 / 16))
    large = np.minimum(max_exact + (scaled.astype(np.float32) * np.float32(16)).astype(np.int32), 31)
    return np.where(dist < max_exact, dist, large)


def host_consts():
    bf = ml_dtypes.bfloat16
    cst = {}
    cst["ident_bf"] = np.eye(128, dtype=np.float32).astype(bf)
    cst["anti_bf"] = np.eye(128, dtype=np.float32)[::-1].copy().astype(bf)
    cst["ident_f"] = np.eye(128, dtype=np.float32)
    da = np.arange(XA) - 511
    oh = np.zeros((32, XA + XC), np.float32)
    ba = t5_bucket_np(da)
    oh[ba, np.arange(XA)] = 1.0
    dc = np.arange(XC) - 2063
    bc = t5_bucket_np(dc)
    oh[bc, XA + np.arange(XC)] = 1.0
    cst["onehot"] = oh
    mult = np.zeros((4, XC), np.float32)
    mult[0, :XA] = (da >= 0)
    mult[1, :XA] = (da >= 0) & (da < 512)
    mult[2, :XA] = ((da >= 0) & (da <= 128)).astype(np.float32) + ((da >= 0) & (da % 4 == 0) & (da <= 512)) + ((da >= 0) & (da % 16 == 0) & (da <= 2048))
    mult[3, :] = (dc >= 0)
    with np.errstate(divide="ignore"):
        mult[:] = np.where(mult > 0, np.log(np.maximum(mult, 1e-30)), -30000.0)
    mult[0:3, XA:] = 0
    cst["mult"] = np.ascontiguousarray(np.broadcast_to(mult[:, None, :], (4, 16, XC))).astype(np.float32)
    n = 128 * (np.arange(256) // 128) + 127 - (np.arange(256) % 128)
    c_start = n[:, None] * 16
    s_start = np.arange(64)[None, :] * 64
    ov = np.clip(np.minimum(c_start + 32, s_start + 64) - np.maximum(c_start, s_start), 0, None).astype(np.float32) / 32
    ov[n == 255] = 0
    cst["overlap"] = ov.astype(bf)
    key = 128 * (np.arange(S) // 128) + 127 - (np.arange(S) % 128)
    ex = np.zeros((64, S), np.float32)
    ex[key // 64, np.arange(S)] = 1
    cst["exr"] = ex.astype(bf)
    t = np.arange(S)[:, None]
    j = np.arange(64)[None, :]
    cur = t // 64
    fm = np.where((j == 0) | (j == cur) | (j == cur - 1), 1e6, np.where(j * 64 > t, -1e6, 0.0)).astype(np.float32)
    cst["fm"] = fm
    cst["ecst"] = np.ascontiguousarray(np.broadcast_to((np.arange(NE) * S).astype(np.float32)[None, :], (128, NE)))
    cst["utri"] = np.triu(np.ones((128, 128), np.float32), k=1).astype(bf)
    cst["ones_bf"] = np.ones((128, 128), np.float32).astype(bf)
    thr = np.broadcast_to((np.arange(8) * 512).astype(np.float32)[None, None, :], (128, NE, 8))
    cst["jthr"] = np.ascontiguousarray(thr).reshape(128, NE * 8)
    return cst


class Prog:
    def __init__(self, debug=()):
        self.debug = set(debug)
        nc = self.nc = bass.Bass("TRN2", target_bir_lowering=False)
        self.kb = KB(nc)
        self.es = contextlib.ExitStack()
        self.inp = {}
        self.scr = {}

    def din(self, name, shape, dt=F32):
        t = self.nc.dram_tensor(name, list(shape), dt, kind="ExternalInput").ap()
        self.inp[name] = Tl(t)
        return self.inp[name]

    def dscr(self, name, shape, dt):
        kind = "ExternalOutput" if name in self.debug else "Internal"
        t = self.nc.dram_tensor(name, list(shape), dt, kind=kind).ap()
        self.scr[name] = Tl(t)
        return self.scr[name]

    def declare(self):
        d = self.din
        d("x", [S, D]); d("rel_bias", [32, 16]); d("attn_norm", [DEPTH, D]); d("w_in", [DEPTH, D, INW])
        d("nsa_q_norm", [DEPTH, 64]); d("nsa_k_norm", [DEPTH, 3, 64]); d("cmp_pos", [DEPTH, 2, 32, 64])
        d("cmp_w1", [DEPTH, 2, 2048, 256]); d("cmp_b1", [DEPTH, 2, 256]); d("cmp_w2", [DEPTH, 2, 256, 64])
        d("dil_q_norm", [DEPTH, 64]); d("dil_k_norm", [DEPTH, 64]); d("out_norm", [DEPTH, D]); d("w_out", [DEPTH, D, D])
        d("ffn_norm", [DEPTH, D]); d("ffn_w_gate", [1, D, DFF]); d("ffn_w_up", [1, D, DFF]); d("ffn_w_down", [1, DFF, D])
        d("router_w", [1, D, NE]); d("exp_w_gate", [1, NE, D, DFE]); d("exp_w_up", [1, NE, D, DFE]); d("exp_w_down", [1, NE, DFE, D])
        d("ident_bf", [128, 128], BF16); d("anti_bf", [128, 128], BF16); d("ident_f", [128, 128])
        d("onehot", [32, XA + XC]); d("mult", [4, 16, XC]); d("overlap", [256, 64], BF16); d("exr", [64, S], BF16); d("fm", [S, 64])
        d("ecst", [128, NE]); d("utri", [128, 128], BF16); d("ones_bf", [128, 128], BF16); d("jthr", [128, NE * 8])
        self.out = Tl(self.nc.dram_tensor("out", [S, D], F32, kind="ExternalOutput").ap())
        s = self.dscr
        s("wtab", [4, 16, XC], BF16)
        s("qaT", [8, 64, S], BF16); s("kcvT", [2, 2, 64, S], BF16); s("kswT", [2, 2, 64, S], BF16)
        s("vsw", [S, 256], BF16); s("gates", [S, 24], F32)
        s("qbT", [8, 64, S], BF16); s("kbT", [8, 64, S], BF16); s("vb", [S, 512], BF16)
        s("OC", [S, 512], F32); s("O", [S, D], F32); s("H1", [S, D], F32)
        s("HP", [S, D], F32); s("XE", [NE * S, D], BF16); s("YE", [NE * S, D], F32)
        if "dbg" in self.debug:
            s("dbg", [128, 4096], F32)

    def setup_globals(self):
        kb, es = self.kb, self.es
        nc = self.nc
        self.banks = []
        for i in range(8):
            h = es.enter_context(nc.psum_tensor(f"bank{i}", [128, 512], F32))
            self.banks.append(Tl(h.ap(), psum=True))
        self.ident_bf = kb.sb(es, [128, 128], BF16, "identbf")
        self.anti_bf = kb.sb(es, [128, 128], BF16, "antibf")
        self.ident_f = kb.sb(es, [128, 128], F32, "identf")
        kb.epsb = kb.sb(es, [128, 1], F32, "epsb")
        self.one11 = kb.sb(es, [1, 1], F32, "one11")
        self.kcmpT = kb.sb(es, [64, 2, 2, 128], BF16, "kcmpT")
        self.vcaug = kb.sb(es, [128, 2, 2, 128], BF16, "vcaug")
        sem = kb.c.dma_sem()
        kb.dma(self.ident_bf, self.inp["ident_bf"], sem)
        kb.dma(self.anti_bf, self.inp["anti_bf"], sem)
        kb.dma(self.ident_f, self.inp["ident_f"], sem)
        kb.memset("dve", kb.epsb, EPS)
        kb.memset("dve", self.one11, 1.0)
        kb.c.barrier()

    def phase_tables(self):
        kb = self.kb
        with contextlib.ExitStack() as es:
            tbl = kb.sb(es, [32, 16], F32, "tbl")
            oh = kb.sbn(es, 2, [32, 512], F32, "oh")
            e32 = kb.sbn(es, 2, [16, 512], F32, "e32")
            mt = kb.sbn(es, 2, [16, 512], F32, "mt")
            wb = kb.sbn(es, 2, [16, 512], BF16, "wb")
            sems = [kb.c.dma_sem() for _ in range(7)]
            kb.dma(tbl, self.inp["rel_bias"], sems[0])
            wtab = self.scr["wtab"]
            k = 0
            for ci in range((XA + XC) // 512):
                x0 = ci * 512
                o = oh[ci % 2]
                kb.dma(o, self.inp["onehot"][:, x0:x0 + 512], sems[1 + ci % 2])
                bank = self.banks[ci % 2]
                kb.mm(bank[0:16, :], tbl, o)
                e = e32[ci % 2]
                kb.copy("act", e, bank[0:16, :])
                tabs = [(0, x0), (1, x0), (2, x0)] if x0 < XA else [(3, x0 - XA)]
                for tb, xx in tabs:
                    m = mt[k % 2]
                    w = wb[k % 2]
                    kb.dma(m, self.inp["mult"][tb, :, xx:xx + 512], sems[3 + k % 2])
                    kb.tt("dve", w, e, m, ALU.add)
                    kb.dma(wtab[tb, :, xx:xx + 512], w, sems[5 + k % 2], pw=True)
                    k += 1
            kb.c.barrier()
            kb.c.release(sems)

    def phase_inproj(self, layer, hin_dram):
        kb = self.kb
        I = self.inp
        with contextlib.ExitStack() as es:
            W = kb.sb(es, [128, 8, INW], BF16, "win")
            gA = kb.sb(es, [128, D], F32, "gA")
            g6 = kb.sb(es, [128, 6, 64], F32, "g6")
            hin = kb.sbn(es, 2, [128, D], F32, "hin")
            junk = kb.sb(es, [128, INW], F32, "junk")
            junkA = kb.sb(es, [128, D], F32, "junkA")
            ssq = kb.sbn(es, 2, [128, 1], F32, "ssq")
            rms = kb.sbn(es, 2, [128, 1], F32, "rms")
            rstd = kb.sbn(es, 2, [128, 1], F32, "rstd")
            u = kb.sbn(es, 2, [128, D], BF16, "u")
            uT = kb.sbn(es, 2, [128, 8, 128], BF16, "uT")
            pj = kb.sbn(es, 2, [128, INW], F32, "pj")
            ssh = kb.sbn(es, 2, [128, 28], F32, "ssh")
            rmh = kb.sbn(es, 2, [128, 28], F32, "rmh")
            rsh = kb.sbn(es, 2, [128, 28], F32, "rsh")
            t1 = kb.sbn(es, 2, [128, 1024], F32, "t1")
            nb = kb.sbn(es, 2, [128, 2328], BF16, "nb")
            vall = kb.sbn(es, 2, [128, 768], BF16, "vall")
            gt = kb.sbn(es, 2, [128, 24], F32, "gt")
            stg_q = kb.sbn(es, 2, [128, 4, 128], BF16, "stgq")
            stg_c = kb.sbn(es, 2, [128, 2, 128], BF16, "stgc")
            stg_k = kb.sbn(es, 2, [128, 2, 128], BF16, "stgk")
            stg_qb = kb.sbn(es, 2, [128, 4, 128], BF16, "stgqb")
            stg_kb = kb.sbn(es, 2, [128, 4, 128], BF16, "stgkb")
            stg_v = kb.sbn(es, 2, [128, 768], BF16, "stgv")
            sems = [kb.c.dma_sem() for _ in range(20)]
            wv = I["w_in"][layer].v(I["w_in"].ap[layer].rearrange("(kc p) n -> p kc n", p=128))
            for kc in range(8):
                kb.dma(W[:, kc, :], wv[:, kc, :], sems[0], q="pool", pw=True)
            kb.dma(gA, I["attn_norm"].v(I["attn_norm"].ap[layer].partition_broadcast(128)), sems[1])
            gsrc = [I["nsa_q_norm"].ap[layer], I["nsa_k_norm"].ap[layer, 1], I["nsa_k_norm"].ap[layer, 2],
                    I["dil_q_norm"].ap[layer], I["dil_k_norm"].ap[layer], I["nsa_k_norm"].ap[layer, 0]]
            for i, a in enumerate(gsrc):
                kb.dma(g6[:, i, :], Tl(a.partition_broadcast(128), I["nsa_q_norm"].buf), sems[1], pw=True)
            kb.ts("dve", g6[:, 0, :], g6[:, 0, :], 0.125, ALU.mult)
            kb.ts("dve", g6[:, 3, :], g6[:, 3, :], 0.125, ALU.mult)
            self.g6_k0 = None
            bk = self.banks
            hv = hin_dram.v(hin_dram.ap.rearrange("(t p) d -> t p d", p=128))
            qaT2 = self.scr["qaT"].v(self.scr["qaT"].ap.rearrange("h d s -> (h d) s").rearrange("(a p) s -> p a s", p=128))
            kcvT2 = self.scr["kcvT"].v(self.scr["kcvT"].ap.rearrange("k g d s -> (k g d) s").rearrange("(a p) s -> p a s", p=128))
            kswT2 = self.scr["kswT"].v(self.scr["kswT"].ap.rearrange("k g d s -> (k g d) s").rearrange("(a p) s -> p a s", p=128))
            qbT2 = self.scr["qbT"].v(self.scr["qbT"].ap.rearrange("h d s -> (h d) s").rearrange("(a p) s -> p a s", p=128))
            kbT2 = self.scr["kbT"].v(self.scr["kbT"].ap.rearrange("h d s -> (h d) s").rearrange("(a p) s -> p a s", p=128))
            def pre(t):
                s = t % 2
                ts_ = slice(t * 128, (t + 1) * 128)
                kb.dma(hin[s], hv[t], sems[2 + s], q="pool")
                h = hin[s]
                kb.act(junkA, h, AF.Square, accum=ssq[s])
                kb.act(rms[s], ssq[s], AF.Sqrt, bias=kb.epsb, scale=1.0 / D)
                kb.recip(rstd[s], rms[s])
                kb.stt(u[s], h, rstd[s], gA, ALU.mult, ALU.mult)
                b2 = bk[2].v(bk[2].ap.bitcast(BF16))
                for kc in range(8):
                    kb.tr(b2[:, kc * 128:(kc + 1) * 128], u[s][:, kc * 128:(kc + 1) * 128], self.ident_bf)
                kb.copy("act", uT[s], b2.v(b2.ap.rearrange("p (a b) -> p a b", b=128)))
            def mmf(t):
                s = t % 2
                for cg in range(6):
                    c0 = cg * 512
                    cw = min(512, INW - c0)
                    bank = bk[cg % 2]
                    for kc in range(8):
                        kb.mm(bank[:, 0:cw], uT[s][:, kc, :], W[:, kc, c0:c0 + cw], start=(kc == 0), stop=(kc == 7))
                    kb.copy("act" if cg % 2 == 0 else "dve", pj[s][:, c0:c0 + cw], bank[:, 0:cw])
            def back(t):
                s = t % 2
                ts_ = slice(t * 128, (t + 1) * 128)
                p = pj[s]
                kb.act(junk, p, AF.Square)
                for (c0, nh, r0) in ((C_QA, 8, 0), (C_KS, 2, 8), (C_KW, 2, 10), (C_QB, 16, 12)):
                    kb.reduce(ssh[s][:, r0:r0 + nh], junk.v(junk.ap[:, c0:c0 + nh * 64].rearrange("p (h d) -> p h d", d=64)))
                kb.act(rmh[s], ssh[s], AF.Sqrt, bias=kb.epsb, scale=1.0 / 64)
                kb.recip(rsh[s], rmh[s])
                n_ = nb[s]
                for (c0, nh, r0, gi) in ((C_QA, 8, 0, 0), (C_KS, 2, 8, 1), (C_KW, 2, 10, 2), (C_QB, 8, 12, 3), (C_KB, 8, 20, 4)):
                    tv = t1[s].v(t1[s].ap[:, 0:nh * 64].rearrange("p (h d) -> p h d", d=64))
                    pv = p.v(p.ap[:, c0:c0 + nh * 64].rearrange("p (h d) -> p h d", d=64))
                    kb.tt("dve", tv, pv, rsh[s].v(bc_last(rsh[s].ap[:, r0:r0 + nh], 64)), ALU.mult)
                    nv = n_.v(n_.ap[:, c0:c0 + nh * 64].rearrange("p (h d) -> p h d", d=64))
                    kb.tt("pool", nv, tv, g6.v(bc_mid(g6.ap[:, gi, :], nh)), ALU.mult)
                kb.copy("pool", n_[:, C_KC:C_KC + 256], p[:, C_KC:C_KC + 256])
                kb.copy("act", vall[s][:, 0:128], p[:, C_VS:C_VS + 128])
                kb.copy("act", vall[s][:, 128:256], p[:, C_VW:C_VW + 128])
                kb.copy("act", vall[s][:, 256:768], p[:, C_VB:C_VB + 512])
                kb.act(gt[s], p[:, C_GA:C_GA + 24], AF.Sigmoid)
                kb.dma(self.scr["gates"][ts_, :], gt[s], sems[4 + s], pw=True)
                b3 = bk[3].v(bk[3].ap.bitcast(BF16))
                for a in range(4):
                    kb.tr(b3[:, a * 128:(a + 1) * 128], n_[:, C_QA + a * 128:C_QA + (a + 1) * 128], self.ident_bf)
                for a in range(4):
                    kb.tr(b3[:, 512 + a * 128:512 + (a + 1) * 128], n_[:, C_QB + a * 128:C_QB + (a + 1) * 128], self.ident_bf)
                kb.copy("dve", stg_q[s], b3.v(b3.ap[:, 0:512].rearrange("p (a b) -> p a b", b=128)))
                kb.copy("act", stg_qb[s], b3.v(b3.ap[:, 512:1024].rearrange("p (a b) -> p a b", b=128)))
                kb.dma(qaT2[:, :, ts_], stg_q[s], sems[6 + s], pw=True)
                kb.dma(qbT2[:, :, ts_], stg_qb[s], sems[8 + s], pw=True)
                b4 = bk[4].v(bk[4].ap.bitcast(BF16))
                for a in range(2):
                    kb.tr(b4[:, a * 128:(a + 1) * 128], n_[:, C_KC + a * 128:C_KC + (a + 1) * 128], self.ident_bf)
                kb.copy("dve", stg_c[s], b4.v(b4.ap[:, 0:256].rearrange("p (a b) -> p a b", b=128)))
                kb.dma(kcvT2[:, :, ts_], stg_c[s], sems[10 + s], pw=True)
                for a, c0 in enumerate((C_KS, C_KW)):
                    kb.mm(bk[5][:, a * 128:(a + 1) * 128], n_[:, c0:c0 + 128], self.anti_bf)
                kb.copy("act", stg_k[s], bk[5].v(bk[5].ap[:, 0:256].rearrange("p (a b) -> p a b", b=128)))
                kb.dma(kswT2[:, :, ts_], stg_k[s], sems[12 + s], pw=True)
                for a in range(4):
                    kb.mm(bk[6][:, a * 128:(a + 1) * 128], n_[:, C_KB + a * 128:C_KB + (a + 1) * 128], self.anti_bf)
                kb.copy("dve", stg_kb[s], bk[6].v(bk[6].ap.rearrange("p (a b) -> p a b", b=128)))
                kb.dma(kbT2[:, :, ts_], stg_kb[s], sems[14 + s], pw=True)
                kb.mm(bk[7], self.anti_bf, vall[s][:, 256:768])
                kb.copy("act", stg_v[s][:, 256:768], bk[7])
                kb.mm(bk[5][:, 256:512], self.anti_bf, vall[s][:, 0:256])
                kb.copy("dve", stg_v[s][:, 0:256], bk[5][:, 256:512])
                kb.dma(self.scr["vsw"][ts_, :], stg_v[s][:, 0:256], sems[16 + s], pw=True)
                kb.dma(self.scr["vb"][ts_, :], stg_v[s][:, 256:768], sems[18 + s], pw=True)

            kb.c.play(kb.c.record(lambda: pre(0)))
            kb.c.play(kb.c.record(lambda: pre(1)), kb.c.record(lambda: mmf(0)))
            for t in range(NT):
                lb = kb.c.record(lambda: back(t))
                k = next(i for i, o in enumerate(lb) if o[0] == "pe")
                lists = [lb[:k]]
                if t + 1 < NT:
                    lists.insert(0, kb.c.record(lambda: mmf(t + 1)))
                if t + 2 < NT:
                    lists.insert(0, kb.c.record(lambda: pre(t + 2)))
                kb.c.play(*lists)
                kb.c.play(lb[k:])
            kb.c.barrier()
            kb.c.release(sems)

    def phase_compress(self, layer):
        kb = self.kb
        I = self.inp
        bk = self.banks
        with contextlib.ExitStack() as es:
            kvT = kb.sbn(es, 2, [64, S], BF16, "kvT")
            w1 = kb.sbn(es, 2, [64, 32, 256], BF16, "w1")
            w2 = kb.sbn(es, 2, [128, 2, 64], BF16, "w2")
            pos = kb.sbn(es, 2, [32, 64], F32, "pos")
            posT = kb.sbn(es, 2, [64, 32], BF16, "posT")
            b1 = kb.sbn(es, 2, [1, 256], F32, "b1")
            bias = kb.sbn(es, 2, [128, 2], F32, "bias")
            hid = kb.sbn(es, 2, [128, 2, 256], BF16, "hid")
            gk = kb.sb(es, [128, 64], F32, "gk")
            xk = kb.sbn(es, 2, [128, 64], F32, "xk")
            jk = kb.sb(es, [128, 64], F32, "jk")
            sq1 = kb.sbn(es, 2, [128, 1], F32, "sq1")
            rm1 = kb.sbn(es, 2, [128, 1], F32, "rm1")
            rs1 = kb.sbn(es, 2, [128, 1], F32, "rs1")
            xb = kb.sbn(es, 2, [128, 64], BF16, "xb")
            sems = [kb.c.dma_sem() for _ in range(8)]
            kb.dma(gk, Tl(I["nsa_k_norm"].ap[layer, 0].partition_broadcast(128), I["nsa_k_norm"].buf), sems[0])
            def cload(i_):
                g_, kv_ = i_ // 2, i_ % 2
                s_ = i_ % 2
                kb.dma(kvT[s_], self.scr["kcvT"][kv_, g_], sems[1 + s_])
                w1src = I["cmp_w1"].v(I["cmp_w1"].ap[layer, kv_].rearrange("(l d) c -> d l c", d=64))
                for lq in range(4):
                    kb.dma(w1[s_][:, lq * 8:(lq + 1) * 8, :], w1src[:, lq * 8:(lq + 1) * 8, :], sems[3 + s_], q="pool", pw=True)
                kb.dma(w2[s_], I["cmp_w2"].v(I["cmp_w2"].ap[layer, kv_].rearrange("(hh p) c -> p hh c", p=128)), sems[3 + s_], q="pool", pw=True)
                kb.dma(pos[s_], I["cmp_pos"][layer, kv_], sems[5 + s_], pw=True)
                kb.dma(b1[s_], I["cmp_b1"][layer, kv_:kv_ + 1, :], sems[5 + s_], pw=True)
            it = 0
            cload(0)
            for g in range(2):
                for kv in range(2):
                    s = it % 2
                    it += 1
                    if it < 4:
                        cload(it)
                    kb.tr(bk[2][0:64, 0:32], pos[s], self.ident_f[0:32, 0:32])
                    kb.copy("dve", posT[s], bk[2][0:64, 0:32])
                    for hh in range(2):
                        hs = slice(hh * 128, (hh + 1) * 128)
                        for l in range(32):
                            kb.mm(bk[3][:, hh:hh + 1], w1[s][:, l, hs], posT[s][:, l:l + 1], start=(l == 0), stop=False)
                        kb.mm(bk[3][:, hh:hh + 1], b1[s][0:1, hs], self.one11, start=False, stop=True)
                    kb.copy("dve", bias[s], bk[3][:, 0:2])
                    kb.memset("pool", hid[s][:, :, 255:256], 0.0)
                    for hh in range(2):
                        hs = slice(hh * 128, (hh + 1) * 128)
                        bank = bk[hh]
                        ka = kvT[s].ap
                        for l in range(32):
                            rhs = kvT[s].v(AP(ka.tensor, ka.offset + l, [list(ka.ap[0]), [16, 255]]))
                            kb.mm(bank[:, 0:255], w1[s][:, l, hs], rhs, start=(l == 0), stop=(l == 31))
                        kb.act(hid[s][:, hh, 0:255], bank[:, 0:255], AF.Gelu_apprx_tanh, bias=bias[s][:, hh:hh + 1])
                    for nt in range(2):
                        ns = slice(nt * 128, (nt + 1) * 128)
                        bank = bk[4 + nt]
                        for hh in range(2):
                            kb.mm(bank[:, 0:64], hid[s][:, hh, ns], w2[s][:, hh, :], start=(hh == 0), stop=(hh == 1))
                        j = (it + nt) % 2
                        if kv == 0:
                            kb.copy("dve", xk[j], bank[:, 0:64])
                            kb.act(jk, xk[j], AF.Square, accum=sq1[j])
                            kb.act(rm1[j], sq1[j], AF.Sqrt, bias=kb.epsb, scale=1.0 / 64)
                            kb.recip(rs1[j], rm1[j])
                            kb.stt(xb[j], xk[j], rs1[j], gk, ALU.mult, ALU.mult)
                            kb.mm(bk[6 + nt][0:64, 0:128], xb[j], self.anti_bf)
                            kb.copy("act", self.kcmpT[:, g, nt, :], bk[6 + nt][0:64, 0:128])
                        else:
                            kb.copy("dve", xb[j], bank[:, 0:64])
                            kb.mm(bk[6 + nt][:, 0:64], self.anti_bf, xb[j])
                            kb.copy("act", self.vcaug[:, g, nt, 0:64], bk[6 + nt][:, 0:64])
            for g in range(2):
                for nt in range(2):
                    kb.dma(self.vcaug[:, g, nt, 64:128], self.inp["overlap"][nt * 128:(nt + 1) * 128, :], sems[7], pw=True)
            kb.c.barrier()
            kb.c.release(sems)


    def run_attn(self, items, ebuf, tbuf, pbuf):
        kb = self.kb
        bk = self.banks
        n = len(items)
        if n == 0:
            return

        LA = 3
        pending = []

        def score(i):
            it = items[i]
            c0, c1 = it["qa"] * 128, it["qb"] * 128
            kb.mm(bk[i % 4][:, c0:c1], it["kT"], it["q"][:, c0:c1], start=True, stop=False)
            kb.mm(bk[i % 4][:, c0:c1], self.ident_bf, it["strip"][:, c0:c1], start=False, stop=True)
        for i in range(min(LA, n)):
            score(i)
        for i in range(n):
            if i + LA < n:
                score(i + LA)
            it = items[i]
            c0, c1 = it["qa"] * 128, it["qb"] * 128
            p = pbuf[i % len(pbuf)]
            kb.act(p[:, c0:c1], bk[i % 4][:, c0:c1], AF.Exp)
            kb.mm(it["pv"][:, c0:c1], it["vaug"], p[:, c0:c1], start=it["first"], stop=it["last"])
            if it["last"]:
                pending.append((i + 2, it["fin"]))
            while pending and pending[0][0] <= i:
                pending.pop(0)[1]()
        while pending:
            pending.pop(0)[1]()

    def hankel(self, tb, hd, pstep, ncols):
        w = self.scr["wtab"]
        a = w.ap[tb, hd]
        return w.v(AP(a.tensor, a.offset, [[pstep, 128], [1, ncols]]))

    def phase_nsa(self, layer):
        kb = self.kb
        I = self.inp
        bk = self.banks
        with contextlib.ExitStack() as es0:
            gates = kb.sb(es0, [128, NT, 24], F32, "gates")
            selT = kb.sb(es0, [128, 2, S], BF16, "selT")
            sem0 = kb.c.dma_sem()
            kb.dma(gates, self.scr["gates"].v(self.scr["gates"].ap.rearrange("(t p) c -> p t c", p=128)), sem0)
            kb.c.barrier()
            OCv = self.scr["OC"].v(self.scr["OC"].ap.rearrange("(t p) c -> p t c", p=128))
            Ov = self.scr["O"].v(self.scr["O"].ap.rearrange("(t p) c -> p t c", p=128))
            with contextlib.ExitStack() as es:
                fm = kb.sb(es, [128, NT, 64], F32, "fm")
                imp = kb.sb(es, [128, NT, 64], F32, "imp")
                stripc = kb.sbn(es, 2, [128, S], BF16, "stripc")
                qTh = kb.sbn(es, 2, [64, S], BF16, "qTh")
                eb = kb.sbn(es, 3, [128, 512], BF16, "eb")
                pb = kb.sbn(es, 3, [128, 512], BF16, "pb")
                den = kb.sbn(es, 2, [128, 4], F32, "den")
                rd = kb.sbn(es, 2, [128, 4], F32, "rd")
                sc = kb.sbn(es, 2, [128, 4], F32, "sc")
                itmp = kb.sbn(es, 2, [128, 4, 64], F32, "itmp")
                ocs = kb.sbn(es, 2, [128, 4, 64], F32, "ocs")
                impf = kb.sbn(es, 2, [128, 64], F32, "impf")
                imp2 = kb.sbn(es, 2, [128, 64], F32, "imp2")
                m8a = kb.sbn(es, 2, [128, 8], F32, "m8a")
                m8b = kb.sbn(es, 2, [128, 8], F32, "m8b")
                selm = kb.sbn(es, 2, [128, 128], BF16, "selm")
                sems = [kb.c.dma_sem() for _ in range(7)]
                kb.dma(fm, I["fm"].v(I["fm"].ap.rearrange("(t p) c -> p t c", p=128)), sems[0])
                kb.memset("dve", selm[0], 0.0)
                kb.memset("dve", selm[1], 0.0)
                kb.c.barrier()
                it = 0
                cnt = 0
                def hload(hd_):
                    s_ = hd_ % 2
                    kb.dma(stripc[s_], self.hankel(3, hd_, 16, S), sems[1 + s_])
                    kb.dma(qTh[s_], self.scr["qaT"][hd_], sems[3 + s_])
                hload(0)
                for g in range(2):
                    for h in range(4):
                        hd = 4 * g + h
                        s = it % 2
                        it += 1
                        if hd + 1 < 8:
                            hload(hd + 1)
                        for QG in range(8):
                            qs = slice(QG * 512, (QG + 1) * 512)
                            ps = []
                            for nt in range(2 if QG >= 4 else 1):
                                bank = bk[nt]
                                x0 = QG * 512 - nt * 2048
                                kb.mm(bank, self.kcmpT[:, g, nt, :], qTh[s][:, qs], start=True, stop=False)
                                kb.mm(bank, self.ident_bf, stripc[s][:, x0:x0 + 512], start=False, stop=True)
                                p = pb[cnt % 3]
                                cnt += 1
                                kb.act(p, bank, AF.Exp)
                                ps.append(p)
                            u_ = (it * 8 + QG) % 2
                            oc = ocs[u_]
                            ob = bk[2 + u_]
                            for qt in range(4):
                                for nt, p in enumerate(ps):
                                    kb.mm(ob[:, qt * 128:(qt + 1) * 128], p[:, qt * 128:(qt + 1) * 128], self.vcaug[:, g, nt, :], start=(nt == 0), stop=(nt == len(ps) - 1))
                            obv = ob.v(ob.ap.rearrange("p (a b) -> p a b", b=128))
                            kb.reduce(den[u_], obv[:, :, 64:128])
                            kb.ts("dve", den[u_], den[u_], 1e-30, ALU.max)
                            kb.recip(rd[u_], den[u_])
                            kb.tt("dve", sc[u_], rd[u_], gates[:, QG * 4:QG * 4 + 4, hd * 3], ALU.mult)
                            kb.tt("dve", oc, obv[:, :, 0:64], sc[u_].v(bc_last(sc[u_].ap, 64)), ALU.mult)
                            iv = imp[:, QG * 4:QG * 4 + 4, :]
                            if h == 0:
                                kb.tt("dve", iv, obv[:, :, 64:128], rd[u_].v(bc_last(rd[u_].ap, 64)), ALU.mult)
                            else:
                                kb.tt("dve", itmp[u_], obv[:, :, 64:128], rd[u_].v(bc_last(rd[u_].ap, 64)), ALU.mult)
                                kb.tt("pool", iv, iv, itmp[u_], ALU.add)
                            kb.dma(OCv[:, QG * 4:QG * 4 + 4, hd * 64:(hd + 1) * 64], oc, sems[5 + u_], pw=True)
                    b4 = bk[4].v(bk[4].ap.bitcast(BF16))
                    for tile in range(NT):
                        j = tile % 2
                        kb.tt("dve", impf[j], imp[:, tile, :], fm[:, tile, :], ALU.add)
                        a, b, c_ = impf[j].ap, m8a[j].ap, imp2[j].ap
                        kb.op("dve", lambda e, a=a, b=b: e.max(out=b, in_=a), [m8a[j]], [impf[j]])
                        kb.op("dve", lambda e, a=a, b=b, c_=c_: e.match_replace(out=c_, in_to_replace=b, in_values=a, imm_value=-3.0e6), [imp2[j]], [impf[j], m8a[j]])
                        d_ = m8b[j].ap
                        kb.op("dve", lambda e, c_=c_, d_=d_: e.max(out=d_, in_=c_), [m8b[j]], [imp2[j]])
                        kb.ts("dve", selm[j][:, 64:128], impf[j], m8b[j][:, 7:8], ALU.is_ge)
                        kb.tr(b4[:, j * 128:(j + 1) * 128], selm[j], self.ident_bf)
                        kb.ts("dve", selT[64:128, g, tile * 128:(tile + 1) * 128], b4[64:128, j * 128:(j + 1) * 128], -1.0, ALU.add, 30000.0, ALU.mult)
                kb.c.barrier()
                kb.c.release(sems)
            if "selT" in self.debug:
                semd = kb.c.dma_sem()
                kb.dma(self.scr["selT"], selT, semd)
                kb.c.barrier()
            with contextlib.ExitStack() as es:
                if getattr(self, "skip_p3b", False):
                    kb.c.release([sem0])
                    return
                ksT = kb.sbn(es, 2, [128, S], BF16, "ksx")
                kwT = kb.sbn(es, 2, [128, S], BF16, "kwT")
                vsa = kb.sbn(es, 2, [128, NT, 65], BF16, "vsa")
                vwa = kb.sbn(es, 2, [128, NT, 65], BF16, "vwa")
                ssel = kb.sbn(es, 4, [128, 2688], BF16, "ssel")
                swin = kb.sbn(es, 4, [128, 1408], BF16, "swin")
                qT4 = kb.sbn(es, 2, [128, 4, 512], BF16, "qsx")
                oct_ = kb.sbn(es, 2, [128, 4, 256], F32, "oct")
                eb = kb.sbn(es, 5, [128, 512], BF16, "eb")
                tb = kb.sbn(es, 5, [128, 512], BF16, "tb")
                pb = kb.sbn(es, 5, [128, 512], BF16, "pb")
                osb = kb.sbn(es, 2, [65, 512], F32, "osb")
                rd4 = kb.sbn(es, 2, [128, 4], F32, "rd4")
                sc4 = kb.sbn(es, 2, [128, 4], F32, "sc4")
                tmp4 = kb.sbn(es, 2, [128, 4, 64], F32, "tmp4")
                sems = [kb.c.dma_sem() for _ in range(12)]
                kb.memset("pool", kwT[0][64:128, :], 0.0)
                kb.memset("pool", kwT[1][64:128, :], 0.0)
                kb.dma(ksT[0][64:128, :], I["exr"], sems[0], pw=True)
                kb.dma(ksT[1][64:128, :], I["exr"], sems[0], pw=True)
                vswv = self.scr["vsw"].v(self.scr["vsw"].ap.rearrange("(t p) c -> p t c", p=128))
                fcnt = [0]
                for g in range(2):
                    sg = g % 2
                    kb.dma(ksT[sg][0:64, :], self.scr["kswT"][0, g], sems[1 + sg], pw=True)
                    kb.dma(kwT[sg][0:64, :], self.scr["kswT"][1, g], sems[1 + sg], pw=True)
                    kb.dma(vsa[sg][:, :, 0:64], vswv[:, :, g * 64:(g + 1) * 64], sems[1 + sg], pw=True)
                    kb.dma(vwa[sg][:, :, 0:64], vswv[:, :, 128 + g * 64:128 + (g + 1) * 64], sems[1 + sg], pw=True)
                    kb.memset("pool", vsa[sg][:, :, 64:65], 1.0)
                    kb.memset("pool", vwa[sg][:, :, 64:65], 1.0)
                    for h in range(4):
                        kb.dma(ssel[h], self.hankel(0, 4 * g + h, 1, 2688), sems[3], pw=True)
                        kb.dma(swin[h], self.hankel(1, 4 * g + h, 1, 1408), sems[3], pw=True)
                    qsrc = self.scr["qaT"].v(self.scr["qaT"].ap[4 * g:4 * g + 4].rearrange("h d s -> d h s"))
                    def qload(QG, g=g, qsrc=qsrc):
                        sq = QG % 2
                        qs = slice(QG * 512, (QG + 1) * 512)
                        kb.dma(qT4[sq][0:64, :, :], qsrc[:, :, qs], sems[4 + sq], pw=True)
                        kb.op("pool", (lambda e, o=qT4[sq].ap[64:128, :, :], i=bc_mid(selT.ap[64:128, g, qs], 4): e.tensor_copy(out=o, in_=i)), [], [selT], pw=[qT4[sq]])
                        kb.dma(oct_[sq], OCv[:, QG * 4:QG * 4 + 4, g * 256:(g + 1) * 256], sems[6 + sq])
                    qload(0)
                    for QG in range(8):
                        sq = QG % 2
                        qs = slice(QG * 512, (QG + 1) * 512)
                        nK = 4 * QG + 4
                        if QG + 1 < 8:
                            qload(QG + 1)
                        acc = oct_[sq]
                        items = []
                        for h in range(4):
                            hd = 4 * g + h
                            for br in range(2):
                                k0 = 0 if br == 0 else max(0, 4 * QG - 4)
                                pvb = bk[4 + br][0:65, :]

                                def fin(h=h, hd=hd, br=br, pvb=pvb, acc=acc, QG=QG):
                                    f = fcnt[0]
                                    fcnt[0] += 1
                                    o = osb[f % 2]
                                    kb.copy("dve", o, pvb)
                                    tbk = bk[6 + f % 2]
                                    for qt in range(4):
                                        kb.tr(tbk[:, qt * 65:qt * 65 + 65], o[:, qt * 128:(qt + 1) * 128], self.ident_f[0:65, 0:65])
                                    ta = tbk.ap
                                    dens = tbk.v(AP(ta.tensor, ta.offset + 64, [list(ta.ap[0]), [65, 4]]))
                                    vals = tbk.v(AP(ta.tensor, ta.offset, [list(ta.ap[0]), [65, 4], [1, 64]]))
                                    r4, s4, tm = rd4[f % 2], sc4[f % 2], tmp4[f % 2]
                                    kb.recip(r4, dens)
                                    kb.tt("dve", s4, r4, gates[:, QG * 4:QG * 4 + 4, hd * 3 + 1 + br], ALU.mult)
                                    kb.tt("dve", tm, vals, s4.v(bc_last(s4.ap, 64)), ALU.mult)
                                    av = acc[:, :, h * 64:(h + 1) * 64]
                                    kb.tt("pool", av, av, tm, ALU.add)
                                for Kt in range(k0, nK):
                                    if br == 0:
                                        c0 = min(4 * QG - Kt + 3, 16) * 128
                                        items.append(dict(kT=ksT[sg][:, Kt * 128:(Kt + 1) * 128], q=qT4[sq][:, h, :], strip=ssel[h][:, c0:c0 + 512],
                                                          mask=None, vaug=vsa[sg][:, Kt, :], pv=pvb, first=(Kt == k0), last=(Kt == nK - 1), fin=fin,
                                                          qa=max(0, Kt - 4 * QG), qb=4))
                                    else:
                                        c0 = (4 * QG - Kt + 3) * 128
                                        items.append(dict(kT=kwT[sg][:, Kt * 128:(Kt + 1) * 128], q=qT4[sq][:, h, :], strip=swin[h][:, c0:c0 + 512],
                                                          mask=None, vaug=vwa[sg][:, Kt, :], pv=pvb, first=(Kt == k0), last=(Kt == nK - 1), fin=fin,
                                                          qa=max(0, Kt - 4 * QG), qb=min(4, Kt - 4 * QG + 5)))
                        self.run_attn(items, eb, tb, pb)
                        kb.dma(Ov[:, QG * 4:QG * 4 + 4, g * 256:(g + 1) * 256], acc, sems[8 + sq], pw=True)
                kb.c.barrier()
                kb.c.release(sems)
            kb.c.release([sem0])

    def phase_dil(self, layer):
        kb = self.kb
        bk = self.banks
        with contextlib.ExitStack() as es:
            kT = kb.sbn(es, 2, [128, S], BF16, "kbT")
            qT = kb.sbn(es, 2, [128, S], BF16, "qbT")
            for t_ in (kT[0], kT[1], qT[0], qT[1]):
                kb.memset("pool", t_[64:128, :], 0.0)
            va = kb.sbn(es, 2, [128, NT, 65], BF16, "vba")
            sd = kb.sbn(es, 2, [128, 2944], BF16, "sdil")
            eb = kb.sbn(es, 5, [128, 512], BF16, "eb")
            pb = kb.sbn(es, 5, [128, 512], BF16, "pb")
            osb = kb.sbn(es, 2, [65, 512], F32, "osb")
            rd4 = kb.sbn(es, 2, [128, 4], F32, "rd4")
            obs = kb.sbn(es, 2, [128, 4, 64], F32, "obs")
            sems = [kb.c.dma_sem() for _ in range(4)]
            vbv = self.scr["vb"].v(self.scr["vb"].ap.rearrange("(t p) c -> p t c", p=128))
            Ov = self.scr["O"].v(self.scr["O"].ap.rearrange("(t p) c -> p t c", p=128))
            fcnt = [0]

            def load(hd):
                s = hd % 2
                kb.dma(kT[s][0:64, :], self.scr["kbT"][hd], sems[s], pw=True)
                kb.dma(qT[s][0:64, :], self.scr["qbT"][hd], sems[s], pw=True)
                kb.dma(va[s][:, :, 0:64], vbv[:, :, hd * 64:(hd + 1) * 64], sems[s], pw=True)
                kb.memset("pool", va[s][:, :, 64:65], 1.0)
                kb.dma(sd[s], self.hankel(2, 8 + hd, 1, 2944), sems[s])
            load(0)
            for hd in range(8):
                s = hd % 2
                if hd + 1 < 8:
                    load(hd + 1)
                items = []
                for QG in range(8):
                    k0 = max(0, 4 * QG - 16)
                    nK = 4 * QG + 4
                    pvb = bk[4 + QG % 2][0:65, :]

                    def fin(hd=hd, QG=QG, pvb=pvb):
                        f = fcnt[0]
                        fcnt[0] += 1
                        o = osb[f % 2]
                        kb.copy("dve", o, pvb)
                        tbk = bk[6 + f % 2]
                        ob = obs[f % 2]
                        for qt in range(4):
                            kb.tr(tbk[:, qt * 65:qt * 65 + 65], o[:, qt * 128:(qt + 1) * 128], self.ident_f[0:65, 0:65])
                        ta = tbk.ap
                        dens = tbk.v(AP(ta.tensor, ta.offset + 64, [list(ta.ap[0]), [65, 4]]))
                        vals = tbk.v(AP(ta.tensor, ta.offset, [list(ta.ap[0]), [65, 4], [1, 64]]))
                        r4 = rd4[f % 2]
                        kb.recip(r4, dens)
                        kb.tt("dve", ob, vals, r4.v(bc_last(r4.ap, 64)), ALU.mult)
                        kb.dma(Ov[:, QG * 4:QG * 4 + 4, 512 + hd * 64:512 + (hd + 1) * 64], ob, sems[2 + f % 2], pw=True)
                    for Kt in range(k0, nK):
                        c0 = (4 * QG - Kt + 3) * 128
                        items.append(dict(kT=kT[s][:, Kt * 128:(Kt + 1) * 128], q=qT[s][:, QG * 512:(QG + 1) * 512], strip=sd[s][:, c0:c0 + 512],
                                          mask=None, vaug=va[s][:, Kt, :], pv=pvb, first=(Kt == k0), last=(Kt == nK - 1), fin=fin,
                                          qa=max(0, Kt - 4 * QG), qb=min(4, Kt - 4 * QG + 17)))
                self.run_attn(items, eb, None, pb)
            kb.c.barrier()
            kb.c.release(sems)


    def phase_ffn(self, layer, hin_dram, hout_dram):
        kb = self.kb
        I = self.inp
        bk = self.banks
        moe = (layer % 2 == 1)
        li = layer // 2
        dff = DFE if moe else DFF
        nfc = dff // 128
        nfb = dff // 256
        with contextlib.ExitStack() as es:
            Wo = kb.sb(es, [128, 8, D], BF16, "Wo")
            gO = kb.sb(es, [128, D], F32, "gO")
            gF = kb.sb(es, [128, D], F32, "gF")
            hp = kb.sb(es, [128, 4, D], F32, "hp")
            hpt = [Tl(hp.ap[:, i, :]) for i in range(4)]
            vT = kb.sb(es, [128, 8, 512], BF16, "vT")
            hid = kb.sb(es, [128, nfc, 512], BF16, "hid")
            hidc = [Tl(hid.ap[:, i, :]) for i in range(nfc)]
            stg = kb.sbn(es, 2, [128, 8, 256], F32, "stg")
            wgu = kb.sbn(es, 4, [128, 8, 256], BF16, "wgu")
            stgd = kb.sbn(es, 2, [128, 4, 512], F32, "stgd")
            wd = kb.sbn(es, 2, [128, 4, 512], BF16, "wd")
            Ot = kb.sbn(es, 2, [128, D], F32, "Ot")
            ht = kb.sbn(es, 2, [128, D], F32, "ht")
            junk = kb.sb(es, [128, D], F32, "junk")
            on = kb.sbn(es, 2, [128, D], BF16, "on")
            onT = kb.sbn(es, 2, [128, 8, 128], BF16, "onT")
            vb_ = kb.sbn(es, 2, [128, D], BF16, "vb_")
            ss2 = kb.sbn(es, 2, [128, 2], F32, "ss2")
            rm2 = kb.sbn(es, 2, [128, 2], F32, "rm2")
            rs2 = kb.sbn(es, 2, [128, 2], F32, "rs2")
            ss1 = kb.sbn(es, 2, [128, 1], F32, "ss1")
            rm1 = kb.sbn(es, 2, [128, 1], F32, "rm1")
            rs1 = kb.sbn(es, 2, [128, 1], F32, "rs1")
            sg = kb.sbn(es, 2, [128, 512], F32, "sg")
            sems = [kb.c.dma_sem() for _ in range(12)]
            if moe:
                v32 = kb.sbn(es, 2, [128, D], F32, "v32")
                v32T = kb.sb(es, [128, 8, 128], F32, "v32T")
                rw = kb.sb(es, [128, 8, NE], F32, "rw")
                gate = kb.sb(es, [128, 4, NE], F32, "gate")
                lg = kb.sbn(es, 2, [128, NE], F32, "lg")
                m8 = kb.sbn(es, 2, [128, 8], F32, "m8")
                msk = kb.sbn(es, 2, [128, NE], F32, "msk")
                nm1 = kb.sbn(es, 2, [128, 1], F32, "nm1")
                ex = kb.sbn(es, 2, [128, NE], F32, "ex")
                gu = kb.sbn(es, 2, [128, NE], F32, "gu")
                dn = kb.sbn(es, 2, [128, 1], F32, "dn")
                rdn = kb.sbn(es, 2, [128, 1], F32, "rdn")
                kb.dma(rw, I["router_w"].v(I["router_w"].ap[li].rearrange("(kc p) e -> p kc e", p=128)), sems[0])
            wov = I["w_out"].v(I["w_out"].ap[layer].rearrange("(kc p) n -> p kc n", p=128))
            for kc in range(8):
                kb.dma(Wo[:, kc, :], wov[:, kc, :], sems[0], q="pool", pw=True)
            kb.dma(gO, Tl(I["out_norm"].ap[layer].partition_broadcast(128), I["out_norm"].buf), sems[0])
            kb.dma(gF, Tl(I["ffn_norm"].ap[layer].partition_broadcast(128), I["ffn_norm"].buf), sems[0])
            kb.c.barrier()
            Ov = self.scr["O"].v(self.scr["O"].ap.rearrange("(t p) c -> t p c", p=128))
            hv = hin_dram.v(hin_dram.ap.rearrange("(t p) d -> t p d", p=128))
            hov = hout_dram.v(hout_dram.ap.rearrange("(t p) d -> p t d", p=128))
            if moe:
                experts = [(I["exp_w_gate"][li, e], I["exp_w_up"][li, e], I["exp_w_down"][li, e], e) for e in range(NE)]
            else:
                experts = [(I["ffn_w_gate"][li], I["ffn_w_up"][li], I["ffn_w_down"][li], None)]
            cnt = {"ld": 0, "ldd": 0, "fc": 0}
            for blk in range(8):
                def tile_body(ti, blk=blk):
                    t = blk * 4 + ti
                    s = t % 2
                    kb.dma(Ot[s], Ov[t], sems[1 + s])
                    kb.dma(ht[s], hv[t], sems[3 + s])
                    o = Ot[s]
                    kb.act(junk[:, 0:512], o[:, 0:512], AF.Square, accum=ss2[s][:, 0:1])
                    kb.act(junk[:, 512:1024], o[:, 512:1024], AF.Square, accum=ss2[s][:, 1:2])
                    kb.act(rm2[s], ss2[s], AF.Sqrt, bias=kb.epsb, scale=1.0 / 512)
                    kb.recip(rs2[s], rm2[s])
                    for gq in range(2):
                        cs = slice(gq * 512, (gq + 1) * 512)
                        kb.stt(on[s][:, cs], o[:, cs], rs2[s][:, gq:gq + 1], gO[:, cs], ALU.mult, ALU.mult)
                    b0 = bk[s].v(bk[s].ap.bitcast(BF16))
                    for kc in range(8):
                        kb.tr(b0[:, kc * 128:(kc + 1) * 128], on[s][:, kc * 128:(kc + 1) * 128], self.ident_bf)
                    kb.copy("act", onT[s], b0.v(b0.ap.rearrange("p (a b) -> p a b", b=128)))
                    for half in range(2):
                        bank = bk[2 + 2 * s + half]
                        for kc in range(8):
                            kb.mm(bank, onT[s][:, kc, :], Wo[:, kc, half * 512:(half + 1) * 512], start=(kc == 0), stop=(kc == 7))
                        kb.tt("dve", hpt[ti][:, half * 512:(half + 1) * 512], bank, ht[s][:, half * 512:(half + 1) * 512], ALU.add)
                    kb.act(junk, hpt[ti], AF.Square, accum=ss1[s])
                    kb.act(rm1[s], ss1[s], AF.Sqrt, bias=kb.epsb, scale=1.0 / D)
                    kb.recip(rs1[s], rm1[s])
                    if moe:
                        kb.stt(v32[s], hpt[ti], rs1[s], gF, ALU.mult, ALU.mult)
                        kb.copy("pool", vb_[s], v32[s])
                    else:
                        kb.stt(vb_[s], hpt[ti], rs1[s], gF, ALU.mult, ALU.mult)
                    for kc in range(8):
                        kb.tr(b0[:, kc * 128:(kc + 1) * 128], vb_[s][:, kc * 128:(kc + 1) * 128], self.ident_bf)
                    kb.copy("act", vT[:, :, ti * 128:(ti + 1) * 128], b0.v(b0.ap.rearrange("p (a b) -> p a b", b=128)))
                    if moe:
                        for kc in range(8):
                            kb.tr(bk[2 + kc // 4][:, (kc % 4) * 128:(kc % 4 + 1) * 128], v32[s][:, kc * 128:(kc + 1) * 128], self.ident_f)
                        kb.copy("dve", v32T[:, 0:4, :], bk[2].v(bk[2].ap.rearrange("p (a b) -> p a b", b=128)))
                        kb.copy("act", v32T[:, 4:8, :], bk[3].v(bk[3].ap.rearrange("p (a b) -> p a b", b=128)))
                        for kc in range(8):
                            kb.mm(bk[1][:, 0:NE], v32T[:, kc, :], rw[:, kc, :], start=(kc == 0), stop=(kc == 7))
                        kb.copy("dve", lg[s], bk[1][:, 0:NE])
                        a_, b_ = lg[s].ap, m8[s].ap
                        kb.op("dve", lambda e, a_=a_, b_=b_: e.max(out=b_, in_=a_), [m8[s]], [lg[s]])
                        kb.ts("dve", msk[s], lg[s], m8[s][:, 1:2], ALU.is_ge)
                        kb.ts("dve", nm1[s], m8[s][:, 0:1], -1.0, ALU.mult)
                        kb.act(ex[s], lg[s], AF.Exp, bias=nm1[s])
                        kb.tt("dve", gu[s], ex[s], msk[s], ALU.mult)
                        kb.reduce(dn[s], gu[s])
                        kb.recip(rdn[s], dn[s])
                        kb.ts("dve", gate[:, ti, :], gu[s], rdn[s], ALU.mult)
                for pa in (0, 2):
                    la = kb.c.record(lambda: tile_body(pa))
                    lb = kb.c.record(lambda: tile_body(pa + 1))
                    kb.c.play(la, lb)
                for (Wg, Wu, Wd, e) in experts:
                    stages = []
                    wgv = Wg.v(Wg.ap.rearrange("(kc p) f -> p kc f", p=128))
                    wuv = Wu.v(Wu.ap.rearrange("(kc p) f -> p kc f", p=128))
                    wdv = Wd.v(Wd.ap.rearrange("(fc p) n -> p fc n", p=128))
                    for fb in range(nfb):
                        def ld(fb=fb, wgv=wgv, wuv=wuv):
                            res = []
                            for src in (wgv, wuv):
                                k = cnt["ld"]
                                cnt["ld"] += 1
                                st = stg[k % 2]
                                w = wgu[k % 4]
                                kb.dma(st, src[:, :, fb * 256:(fb + 1) * 256], sems[5 + k % 2])
                                kb.copy("act" if k % 2 == 0 else "dve", w, st)
                                res.append(w)
                            return res

                        def comp(ws, fb=fb):
                            wg_, wu_ = ws
                            for j in range(2):
                                fc = fb * 2 + j
                                k = cnt["fc"]
                                cnt["fc"] += 1
                                gb, ub = bk[(k % 2) * 2], bk[(k % 2) * 2 + 1]
                                for kc in range(8):
                                    kb.mm(gb, wg_[:, kc, j * 128:(j + 1) * 128], vT[:, kc, :], start=(kc == 0), stop=(kc == 7))
                                for kc in range(8):
                                    kb.mm(ub, wu_[:, kc, j * 128:(j + 1) * 128], vT[:, kc, :], start=(kc == 0), stop=(kc == 7))
                                kb.act(sg[k % 2], gb, AF.Silu)
                                kb.tt("dve", hidc[fc], sg[k % 2], ub, ALU.mult)
                        stages.append((ld, comp))
                    ngr = (nfc + 3) // 4
                    for half in range(2):
                        for fg in range(ngr):
                            f0 = fg * 4
                            nf = min(4, nfc - f0)

                            def ld(half=half, f0=f0, nf=nf, wdv=wdv):
                                k = cnt["ldd"]
                                cnt["ldd"] += 1
                                st = stgd[k % 2]
                                w = wd[k % 2]
                                kb.dma(st[:, 0:nf, :], wdv[:, f0:f0 + nf, half * 512:(half + 1) * 512], sems[7 + k % 2])
                                kb.copy("act" if k % 2 == 0 else "dve", w[:, 0:nf, :], st[:, 0:nf, :])
                                return w

                            def comp(w, half=half, f0=f0, nf=nf, fg=fg, e=e):
                                for j in range(nf):
                                    fc = f0 + j
                                    for ti in range(4):
                                        kb.mm(bk[4 + ti], hidc[fc][:, ti * 128:(ti + 1) * 128], w[:, j, :], start=(fc == 0), stop=(fc == nfc - 1))
                                if fg == ngr - 1:
                                    for ti in range(4):
                                        dst = hpt[ti][:, half * 512:(half + 1) * 512]
                                        if e is None:
                                            kb.tt("dve", dst, bk[4 + ti], dst, ALU.add)
                                        else:
                                            kb.stt(dst, bk[4 + ti], gate[:, ti, e:e + 1], dst, ALU.mult, ALU.add)
                            stages.append((ld, comp))
                    nxt = stages[0][0]()
                    for i in range(len(stages)):
                        cur = nxt
                        if i + 1 < len(stages):
                            nxt = stages[i + 1][0]()
                        stages[i][1](cur)
                for ti in range(4):
                    kb.dma(hov[:, blk * 4 + ti, :], hpt[ti], sems[9], q="pool", pw=True)
            kb.c.barrier()
            kb.c.release(sems)

    def phase_moe(self, layer, hin_dram, hout_dram):
        kb = self.kb
        c = kb.c
        I = self.inp
        bk = self.banks
        li = layer // 2
        dff = DFE
        nfc = dff // 128
        nfb = dff // 256
        XE = self.scr["XE"]
        YE = self.scr["YE"]
        HP = self.scr["HP"]
        with contextlib.ExitStack() as es0:
            idx_all = kb.sb(es0, [128, NT, 2], I32, "idx_all")
            g_all = kb.sb(es0, [128, NT, 2], F32, "g_all")
            run = kb.sb(es0, [128, NE], F32, "run")
            flags_i = kb.sb(es0, [128, NE * 8], I32, "flags_i")
            HPv = HP.v(HP.ap.rearrange("(t p) d -> t p d", p=128))
            with contextlib.ExitStack() as es:
                Wo = kb.sb(es, [128, 8, D], BF16, "Wo")
                gO = kb.sb(es, [128, D], F32, "gO")
                gF = kb.sb(es, [128, D], F32, "gF")
                ecst = kb.sb(es, [128, NE], F32, "ecst")
                utri = kb.sb(es, [128, 128], BF16, "utri")
                ones_bf = kb.sb(es, [128, 128], BF16, "ones_bf")
                jthr = kb.sb(es, [128, NE * 8], F32, "jthr")
                rw = kb.sb(es, [128, 8, NE], F32, "rw")
                Ot = kb.sbn(es, 2, [128, D], F32, "Ot")
                ht = kb.sbn(es, 2, [128, D], F32, "ht")
                hpt = kb.sbn(es, 2, [128, D], F32, "hpt")
                junk = kb.sb(es, [128, D], F32, "junk")
                on = kb.sbn(es, 2, [128, D], BF16, "on")
                onT = kb.sbn(es, 2, [128, 8, 128], BF16, "onT")
                vb_ = kb.sbn(es, 2, [128, D], BF16, "vb_")
                v32 = kb.sbn(es, 2, [128, D], F32, "v32")
                v32T = kb.sbn(es, 2, [128, 8, 128], F32, "v32T")
                ss2 = kb.sbn(es, 2, [128, 2], F32, "ss2")
                rm2 = kb.sbn(es, 2, [128, 2], F32, "rm2")
                rs2 = kb.sbn(es, 2, [128, 2], F32, "rs2")
                ss1 = kb.sbn(es, 2, [128, 1], F32, "ss1")
                rm1 = kb.sbn(es, 2, [128, 1], F32, "rm1")
                rs1 = kb.sbn(es, 2, [128, 1], F32, "rs1")
                lg = kb.sbn(es, 2, [128, NE], F32, "lg")
                m8 = kb.sbn(es, 2, [128, 8], F32, "m8")
                msk = kb.sbn(es, 2, [128, NE], F32, "msk")
                mskb = kb.sbn(es, 2, [128, NE], BF16, "mskb")
                nm1 = kb.sbn(es, 2, [128, 1], F32, "nm1")
                ex = kb.sbn(es, 2, [128, NE], F32, "ex")
                gu = kb.sbn(es, 2, [128, NE], F32, "gu")
                gate = kb.sbn(es, 2, [128, NE], F32, "gate")
                dn = kb.sbn(es, 2, [128, 1], F32, "dn")
                rdn = kb.sbn(es, 2, [128, 1], F32, "rdn")
                flat = kb.sbn(es, 2, [128, NE], F32, "flat")
                A = kb.sbn(es, 2, [128, NE], F32, "A")
                Bm = kb.sbn(es, 2, [128, NE], F32, "Bm")
                eq = kb.sbn(es, 2, [128, NE], F32, "eq")
                f2 = kb.sbn(es, 2, [128, 2], F32, "f2")
                amax = kb.sbn(es, 2, [128, 1], F32, "amax")
                bmax = kb.sbn(es, 2, [128, 1], F32, "bmax")
                nrep = kb.sb(es, [128, NE * 8], F32, "nrep")
                flags_f = kb.sb(es, [128, NE * 8], F32, "flags_f")
                sems = [kb.c.dma_sem() for _ in range(10)]
                wov = I["w_out"].v(I["w_out"].ap[layer].rearrange("(kc p) n -> p kc n", p=128))
                for kc in range(8):
                    kb.dma(Wo[:, kc, :], wov[:, kc, :], sems[0], q="pool", pw=True)
                kb.dma(gO, Tl(I["out_norm"].ap[layer].partition_broadcast(128), I["out_norm"].buf), sems[0])
                kb.dma(gF, Tl(I["ffn_norm"].ap[layer].partition_broadcast(128), I["ffn_norm"].buf), sems[0])
                kb.dma(rw, I["router_w"].v(I["router_w"].ap[li].rearrange("(kc p) e -> p kc e", p=128)), sems[0])
                kb.dma(ecst, I["ecst"], sems[0])
                kb.dma(utri, I["utri"], sems[0])
                kb.dma(ones_bf, I["ones_bf"], sems[0])
                kb.dma(jthr, I["jthr"], sems[0])
                kb.memset("dve", run, 0.0)
                kb.c.barrier()
                Ov = self.scr["O"].v(self.scr["O"].ap.rearrange("(t p) c -> t p c", p=128))
                hv = hin_dram.v(hin_dram.ap.rearrange("(t p) d -> t p d", p=128))
                def part1(t):
                    s = t % 2
                    kb.dma(Ot[s], Ov[t], sems[1 + s])
                    kb.dma(ht[s], hv[t], sems[3 + s])
                    o = Ot[s]
                    kb.act(junk[:, 0:512], o[:, 0:512], AF.Square, accum=ss2[s][:, 0:1])
                    kb.act(junk[:, 512:1024], o[:, 512:1024], AF.Square, accum=ss2[s][:, 1:2])
                    kb.act(rm2[s], ss2[s], AF.Sqrt, bias=kb.epsb, scale=1.0 / 512)
                    kb.recip(rs2[s], rm2[s])
                    for gq in range(2):
                        cs = slice(gq * 512, (gq + 1) * 512)
                        kb.stt(on[s][:, cs], o[:, cs], rs2[s][:, gq:gq + 1], gO[:, cs], ALU.mult, ALU.mult)
                    b0 = bk[s].v(bk[s].ap.bitcast(BF16))
                    for kc in range(8):
                        kb.tr(b0[:, kc * 128:(kc + 1) * 128], on[s][:, kc * 128:(kc + 1) * 128], self.ident_bf)
                    kb.copy("act", onT[s], b0.v(b0.ap.rearrange("p (a b) -> p a b", b=128)))
                    hp = hpt[s]
                    for half in range(2):
                        bank = bk[2 + s]
                        for kc in range(8):
                            kb.mm(bank, onT[s][:, kc, :], Wo[:, kc, half * 512:(half + 1) * 512], start=(kc == 0), stop=(kc == 7))
                        kb.tt("dve", hp[:, half * 512:(half + 1) * 512], bank, ht[s][:, half * 512:(half + 1) * 512], ALU.add)
                    kb.dma(HPv[t], hp, sems[5 + s], q="pool")
                    kb.act(junk, hp, AF.Square, accum=ss1[s])
                    kb.act(rm1[s], ss1[s], AF.Sqrt, bias=kb.epsb, scale=1.0 / D)
                    kb.recip(rs1[s], rm1[s])
                    kb.stt(v32[s], hp, rs1[s], gF, ALU.mult, ALU.mult)
                    kb.copy("pool", vb_[s], v32[s])
                    for kc in range(8):
                        kb.tr(bk[4 + s][:, (kc % 4) * 128:(kc % 4 + 1) * 128], v32[s][:, kc * 128:(kc + 1) * 128], self.ident_f)
                        if kc % 4 == 3:
                            kb.copy("dve" if kc == 3 else "act", v32T[s][:, kc - 3:kc + 1, :], bk[4 + s].v(bk[4 + s].ap.rearrange("p (a b) -> p a b", b=128)))
                    for kc in range(8):
                        kb.mm(bk[6 + s][:, 0:NE], v32T[s][:, kc, :], rw[:, kc, :], start=(kc == 0), stop=(kc == 7))
                    kb.copy("dve", lg[s], bk[6 + s][:, 0:NE])
                    a_, b_ = lg[s].ap, m8[s].ap
                    kb.op("dve", lambda e, a_=a_, b_=b_: e.max(out=b_, in_=a_), [m8[s]], [lg[s]])
                    kb.ts("dve", msk[s], lg[s], m8[s][:, 1:2], ALU.is_ge)
                    kb.copy("dve", mskb[s], msk[s])
                    kb.ts("dve", nm1[s], m8[s][:, 0:1], -1.0, ALU.mult)
                    kb.act(ex[s], lg[s], AF.Exp, bias=nm1[s])
                    kb.tt("dve", gu[s], ex[s], msk[s], ALU.mult)
                    kb.reduce(dn[s], gu[s])
                    kb.recip(rdn[s], dn[s])
                    kb.ts("dve", gate[s], gu[s], rdn[s], ALU.mult)
                def part2(t):
                    s = t % 2
                    kb.mm(bk[6][:, 0:NE], utri, mskb[s])
                    kb.mm(bk[7][:, 0:NE], ones_bf, mskb[s])
                    kb.tt("dve", flat[s], bk[6][:, 0:NE], run, ALU.add)
                    kb.tt("dve", run, bk[7][:, 0:NE], run, ALU.add)
                    kb.tt("dve", flat[s], flat[s], ecst, ALU.add)
                    kb.stt(A[s], flat[s], 1.0, msk[s], ALU.add, ALU.mult)
                    kb.reduce(amax[s], A[s], op=ALU.max)
                    kb.ts("dve", f2[s][:, 0:1], amax[s], -1.0, ALU.add)
                    kb.ts("dve", Bm[s], flat[s], -1.0, ALU.mult, 40000.0, ALU.add)
                    kb.tt("dve", Bm[s], Bm[s], msk[s], ALU.mult)
                    kb.reduce(bmax[s], Bm[s], op=ALU.max)
                    kb.ts("dve", f2[s][:, 1:2], bmax[s], -1.0, ALU.mult, 40000.0, ALU.add)
                    kb.copy("dve", idx_all[:, t, :], f2[s])
                    kb.ts("dve", eq[s], A[s], amax[s], ALU.is_equal)
                    kb.tt("dve", eq[s], eq[s], gate[s], ALU.mult)
                    kb.reduce(g_all[:, t, 0:1], eq[s])
                    kb.ts("dve", g_all[:, t, 1:2], g_all[:, t, 0:1], -1.0, ALU.mult, 1.0, ALU.add)
                    for k in range(2):
                        xo, ia, va = XE.ap, idx_all.ap[:, t, k:k + 1], vb_[s].ap
                        c.emit("pool", lambda e, xo=xo, ia=ia, va=va: e.indirect_dma_start(
                            out=xo, out_offset=bass.IndirectOffsetOnAxis(ap=ia, axis=0), in_=va, in_offset=None),
                            reads=[idx_all.buf, vb_[s].buf], pwrites=[XE.buf], dsem=sems[7 + s])
                for t in range(0, NT, 2):
                    la = kb.c.record(lambda: part1(t))
                    lb = kb.c.record(lambda: part1(t + 1))
                    kb.c.play(la, lb)
                    part2(t)
                    part2(t + 1)
                kb.copy("dve", nrep.v(nrep.ap.rearrange("p (e j) -> p e j", j=8)), run.v(bc_last(run.ap, 8)))
                kb.tt("dve", flags_f, nrep, jthr, ALU.is_gt)
                kb.copy("dve", flags_i, flags_f)
                kb.c.barrier()
                kb.c.release(sems)
            with contextlib.ExitStack() as es:
                xt_ = kb.sbn(es, 4, [128, D], BF16, "xt_")
                XT = kb.sb(es, [128, 8, 512], BF16, "XT")
                hid = kb.sb(es, [128, nfc, 512], BF16, "hid")
                hidc = [Tl(hid.ap[:, i, :]) for i in range(nfc)]
                stg = kb.sbn(es, 2, [128, 8, 256], F32, "stg")
                wgu = kb.sbn(es, 4, [128, 8, 256], BF16, "wgu")
                stgd = kb.sbn(es, 2, [128, 4, 512], F32, "stgd")
                wd = kb.sbn(es, 2, [128, 4, 512], BF16, "wd")
                sg = kb.sbn(es, 2, [128, 512], F32, "sg")
                yst = kb.sbn(es, 2, [128, 4, 512], F32, "yst")
                sems = [kb.c.dma_sem() for _ in range(8)]
                cnt = {"ld": 0, "ldd": 0, "fc": 0, "x": 0, "y": 0}
                XEv = XE.v(XE.ap.rearrange("(t p) d -> t p d", p=128))
                YEv = YE.v(YE.ap.rearrange("(t p) d -> p t d", p=128))
                for e in range(NE):
                    Wg, Wu, Wd = I["exp_w_gate"][li, e], I["exp_w_up"][li, e], I["exp_w_down"][li, e]
                    wgv = Wg.v(Wg.ap.rearrange("(kc p) f -> p kc f", p=128))
                    wuv = Wu.v(Wu.ap.rearrange("(kc p) f -> p kc f", p=128))
                    wdv = Wd.v(Wd.ap.rearrange("(fc p) n -> p fc n", p=128))
                    for j in range(8):
                        c.cond_begin(flags_i.ap[0:1, e * 8 + j:e * 8 + j + 1], flags_i.buf)
                        t0 = (e * S + j * 512) // 128
                        b0 = bk[0].v(bk[0].ap.bitcast(BF16))
                        for ti in range(4):
                            kb.dma(xt_[ti], XEv[t0 + ti], sems[ti % 2])
                        for ti in range(4):
                            bt = bk[ti % 2].v(bk[ti % 2].ap.bitcast(BF16))
                            for kc in range(8):
                                kb.tr(bt[:, kc * 128:(kc + 1) * 128], xt_[ti][:, kc * 128:(kc + 1) * 128], self.ident_bf)
                            kb.copy("act" if ti % 2 == 0 else "dve", XT[:, :, ti * 128:(ti + 1) * 128], bt.v(bt.ap.rearrange("p (a b) -> p a b", b=128)))
                        stages = []
                        for fb in range(nfb):
                            def ld(fb=fb, wgv=wgv, wuv=wuv):
                                res = []
                                for src in (wgv, wuv):
                                    k = cnt["ld"]
                                    cnt["ld"] += 1
                                    st = stg[k % 2]
                                    w = wgu[k % 4]
                                    kb.dma(st, src[:, :, fb * 256:(fb + 1) * 256], sems[2 + k % 2])
                                    kb.copy("act" if k % 2 == 0 else "dve", w, st)
                                    res.append(w)
                                return res

                            def comp(ws, fb=fb):
                                wg_, wu_ = ws
                                for jj in range(2):
                                    fc = fb * 2 + jj
                                    k = cnt["fc"]
                                    cnt["fc"] += 1
                                    gb, ub = bk[(k % 2) * 2], bk[(k % 2) * 2 + 1]
                                    for kc in range(8):
                                        kb.mm(gb, wg_[:, kc, jj * 128:(jj + 1) * 128], XT[:, kc, :], start=(kc == 0), stop=(kc == 7))
                                    for kc in range(8):
                                        kb.mm(ub, wu_[:, kc, jj * 128:(jj + 1) * 128], XT[:, kc, :], start=(kc == 0), stop=(kc == 7))
                                    kb.act(sg[k % 2], gb, AF.Silu)
                                    kb.tt("dve", hidc[fc], sg[k % 2], ub, ALU.mult)
                            stages.append((ld, comp))
                        ngr = (nfc + 3) // 4
                        for half in range(2):
                            for fg in range(ngr):
                                f0 = fg * 4
                                nf = min(4, nfc - f0)

                                def ld(half=half, f0=f0, nf=nf, wdv=wdv):
                                    k = cnt["ldd"]
                                    cnt["ldd"] += 1
                                    st = stgd[k % 2]
                                    w = wd[k % 2]
                                    kb.dma(st[:, 0:nf, :], wdv[:, f0:f0 + nf, half * 512:(half + 1) * 512], sems[4 + k % 2])
                                    kb.copy("act" if k % 2 == 0 else "dve", w[:, 0:nf, :], st[:, 0:nf, :])
                                    return w

                                def comp(w, half=half, f0=f0, nf=nf, fg=fg, t0=t0):
                                    for jj in range(nf):
                                        fc = f0 + jj
                                        for ti in range(4):
                                            kb.mm(bk[4 + ti], hidc[fc][:, ti * 128:(ti + 1) * 128], w[:, jj, :], start=(fc == 0), stop=(fc == nfc - 1))
                                    if fg == ngr - 1:
                                        k = cnt["y"]
                                        cnt["y"] += 1
                                        y = yst[k % 2]
                                        for ti in range(4):
                                            kb.copy("act" if ti % 2 == 0 else "dve", y[:, ti, :], bk[4 + ti])
                                        kb.dma(YEv[:, t0:t0 + 4, half * 512:(half + 1) * 512], y, sems[6 + k % 2], q="pool", pw=True)
                                stages.append((ld, comp))
                        nxt = stages[0][0]()
                        for i in range(len(stages)):
                            cur = nxt
                            if i + 1 < len(stages):
                                nxt = stages[i + 1][0]()
                            stages[i][1](cur)
                        c.cond_end()
                kb.c.barrier()
                kb.c.release(sems)
            with contextlib.ExitStack() as es:
                hpb = kb.sbn(es, 2, [128, D], F32, "hpb")
                yh = kb.sbn(es, 2, [128, D], F32, "yh")
                yl = kb.sbn(es, 2, [128, D], F32, "yl")
                ob = kb.sbn(es, 2, [128, D], F32, "ob")
                sems = [kb.c.dma_sem() for _ in range(8)]
                hov = hout_dram.v(hout_dram.ap.rearrange("(t p) d -> t p d", p=128))
                for t in range(NT):
                    s = t % 2
                    kb.dma(hpb[s], HPv[t], sems[s])
                    for k, dst in enumerate((yh[s], yl[s])):
                        yi, ia, da = YE.ap, idx_all.ap[:, t, k:k + 1], dst.ap
                        c.emit("pool", lambda e, yi=yi, ia=ia, da=da: e.indirect_dma_start(
                            out=da, out_offset=None, in_=yi, in_offset=bass.IndirectOffsetOnAxis(ap=ia, axis=0)),
                            reads=[idx_all.buf, YE.buf], writes=[dst.buf], dsem=sems[2 + 2 * k + s])
                    kb.stt(ob[s], yh[s], g_all[:, t, 0:1], hpb[s], ALU.mult, ALU.add)
                    kb.stt(ob[s], yl[s], g_all[:, t, 1:2], ob[s], ALU.mult, ALU.add)
                    kb.dma(hov[t], ob[s], sems[6 + s], q="act", pw=True)
                kb.c.barrier()
                kb.c.release(sems)

    def build_all(self):
        self.declare()
        self.setup_globals()
        self.phase_tables()
        hin = self.inp["x"]
        for layer in range(DEPTH):
            hout = self.scr["H1"] if layer == 0 else self.out
            self.phase_inproj(layer, hin)
            self.phase_compress(layer)
            self.phase_nsa(layer)
            self.phase_dil(layer)
            if layer % 2 == 1:
                self.phase_moe(layer, hin, hout)
            else:
                self.phase_ffn(layer, hin, hout)
            hin = hout
        self.kb.c.barrier()
        self.kb.c.flush()
        return self.nc


INPUT_NAMES = ["x", "rel_bias", "attn_norm", "w_in", "nsa_q_norm", "nsa_k_norm", "cmp_pos", "cmp_w1", "cmp_b1", "cmp_w2",
               "dil_q_norm", "dil_k_norm", "out_norm", "w_out", "ffn_norm", "ffn_w_gate", "ffn_w_up", "ffn_w_down",
               "router_w", "exp_w_gate", "exp_w_up", "exp_w_down"]


def make_in_maps(inputs, cores):
    cst = host_consts()
    maps = []
    shared = {k: np.ascontiguousarray(np.asarray(inputs[k], dtype=np.float32)) for k in INPUT_NAMES if k != "x"}
    x = np.asarray(inputs["x"], dtype=np.float32)
    for b in cores:
        m = dict(shared)
        m["x"] = np.ascontiguousarray(x[b])
        m.update(cst)
        maps.append(m)
    return maps


_CACHE = {}


def kernel(**inputs):
    cores = list(range(8))
    if "nc" not in _CACHE:
        _CACHE["nc"] = Prog().build_all()
    nc = _CACHE["nc"]
    maps = make_in_maps(inputs, cores)
    res = run_bass_kernel_spmd(nc, maps, core_ids=cores)
    out = np.stack([np.asarray(r["out"], dtype=np.float32) for r in res.results], axis=0)
    return out
```

```python
import contextlib
import numpy as np
import ml_dtypes
import concourse.bass as bass
import concourse.mybir as mybir
from concourse.ap import AP
from concourse.bass_utils import run_bass_kernel_spmd

F32 = mybir.dt.float32
BF16 = mybir.dt.bfloat16
I32 = mybir.dt.int32
AF = mybir.ActivationFunctionType
ALU = mybir.AluOpType
AX = mybir.AxisListType

S = 4096
D = 1024
NT = 32
DEPTH = 2
INW = 2840
DFF = 2816
DFE = 3584
NE = 8
EPS = 1e-6
XA = 3072
XC = 6144
C_QA, C_KC, C_VC, C_KS, C_VS, C_KW, C_VW, C_GA, C_QB, C_KB, C_VB = 0, 512, 640, 768, 896, 1024, 1152, 1280, 1304, 1816, 2328


class Buf:
    __slots__ = ("name", "writers", "readers")

    def __init__(self, name=""):
        self.name = name
        self.writers = []
        self.readers = []


class Tl:
    __slots__ = ("ap", "buf", "psum")

    def __init__(self, ap, buf=None, psum=False):
        self.ap = ap
        self.buf = buf if buf is not None else Buf()
        self.psum = psum

    def __getitem__(self, k):
        return Tl(self.ap[k], self.buf, self.psum)

    def v(self, ap):
        return Tl(ap, self.buf, self.psum)


class Ctx:
    COMPUTE = ("pe", "act", "dve", "pool")

    def __init__(self, nc):
        self.nc = nc
        self.ops = {e: [] for e in ("pe", "act", "dve", "pool", "sp")}
        self.sems = {}
        self.count = {}
        self.known = {e: {} for e in self.ops}
        for e in self.COMPUTE:
            self.sems[e] = nc.alloc_semaphore(name=f"sem_{e}")
            self.count[e] = 0
        self.free_dsems = []
        self.nsem = 0
        self.region = None
        self.rec = None

    def dma_sem(self):
        if self.free_dsems:
            return self.free_dsems.pop()
        self.nsem += 1
        k = f"dma{self.nsem}"
        self.sems[k] = self.nc.alloc_semaphore(name=k)
        self.count[k] = 0
        return k

    def release(self, ks):
        self.free_dsems.extend(ks)

    def record(self, body):
        assert self.rec is None
        self.rec = []
        body()
        r, self.rec = self.rec, None
        return r

    def play(self, *lists):
        n = max(len(l) for l in lists)
        for i in range(n):
            for l in lists:
                if i < len(l):
                    self.emit(*l[i])

    def emit(self, eng, fn, reads=(), writes=(), pwrites=(), dsem=None):
        if self.rec is not None:
            self.rec.append((eng, fn, list(reads), list(writes), list(pwrites), dsem))
            return None
        deps = {}

        def add(ev, kind):
            sk, v = ev
            if sk == eng:
                if eng == "pe" or kind != "raw":
                    return
            if deps.get(sk, 0) < v:
                deps[sk] = v

        for b in reads:
            for ev in b.writers:
                add(ev, "raw")
        for b in writes:
            for ev in b.writers:
                add(ev, "waw")
            for ev in b.readers:
                add(ev, "war")
        for b in pwrites:
            for ev in b.readers:
                add(ev, "war")
        waits = []
        kn = self.known[eng]
        for sk, v in deps.items():
            if sk not in self.COMPUTE:
                v = self.count[sk]
            if kn.get(sk, 0) < v:
                kn[sk] = v
                waits.append((sk, v))
        if dsem is not None:
            self.count[dsem] += 16
            ev = (dsem, self.count[dsem])
            inc = (dsem, 16)
        else:
            self.count[eng] += 1
            ev = (eng, self.count[eng])
            inc = (eng, 1)
        self.ops[eng].append((waits, fn, inc))
        if self.region is not None and dsem is not None:
            self.region["dq"].setdefault((eng, dsem), 0)
            self.region["dq"][(eng, dsem)] += 16
        for b in reads:
            b.readers.append(ev)
            if len(b.readers) > 48:
                b.readers = self._compact(b.readers)
        for b in writes:
            b.writers = [ev]
            b.readers = []
        for b in pwrites:
            b.writers.append(ev)
            if len(b.writers) > 48:
                b.writers = self._compact(b.writers)
        return ev

    @staticmethod
    def _compact(evs):
        d = {}
        for sk, v in evs:
            if d.get(sk, 0) < v:
                d[sk] = v
        return list(d.items())

    def barrier(self):
        for eng in self.ops:
            waits = []
            kn = self.known[eng]
            for sk, v in self.count.items():
                if v > 0 and sk != eng and kn.get(sk, 0) < v:
                    kn[sk] = v
                    waits.append((sk, v))
            if waits:
                self.ops[eng].append((waits, None, None))

    def cond_begin(self, flag_ap, flag_buf):
        assert self.region is None
        self.region = {"start": dict(self.count), "known": {e: dict(k) for e, k in self.known.items()}, "dq": {}}
        for eng in self.ops:
            waits = []
            kn = self.known[eng]
            for sk, v in flag_buf.writers:
                if sk not in self.COMPUTE:
                    v = self.count[sk]
                if kn.get(sk, 0) < v:
                    kn[sk] = v
                    waits.append((sk, v))
            self.ops[eng].append(("begin", waits, flag_ap))

    def cond_end(self):
        r = self.region
        self.region = None
        for eng in self.ops:
            fix = []
            if eng in self.COMPUTE:
                n = self.count[eng] - r["start"][eng]
                if n > 0:
                    fix.append((eng, r["start"][eng], n))
            for (q, dsem), n in r["dq"].items():
                if q == eng:
                    fix.append((dsem, r["start"].get(dsem, 0), n))
            self.ops[eng].append(("end", fix, None))
            self.known[eng] = r["known"][eng]

    def flush(self):
        nc = self.nc
        sems = self.sems
        with nc.Block() as block:
            def mk(ename):
                def run(engine):
                    ops = self.ops[ename]
                    stack = []
                    i = 0
                    while i < len(ops):
                        a, b, c3 = ops[i]
                        if isinstance(a, str) and a == "begin":
                            for sk, v in b:
                                engine.wait_ge(sems[sk], v)
                            if isinstance(ops[i + 1][0], str) and ops[i + 1][0] == "end" and not ops[i + 1][1]:
                                i += 2
                                continue
                            val = engine.value_load(c3)
                            guard = engine.If(val)
                            guard.__enter__()
                            stack.append((guard, val))
                        elif isinstance(a, str) and a == "end":
                            guard, val = stack.pop()
                            guard.__exit__(None, None, None)
                            with engine.Else():
                                for sk, prior, n in b:
                                    if prior > 0:
                                        engine.wait_ge(sems[sk], prior)
                                    engine.sem_inc(sems[sk], n)
                            engine.free_register(val.val)
                        else:
                            for sk, v in a:
                                engine.wait_ge(sems[sk], v)
                            if b is not None:
                                b(engine).then_inc(sems[c3[0]], c3[1])
                        i += 1
                return run
            block.tensor(mk("pe"))
            block.scalar(mk("act"))
            block.vector(mk("dve"))
            block.gpsimd(mk("pool"))
            block.sync(mk("sp"))


def bc_last(ap, n):
    return AP(ap.tensor, ap.offset, [list(x) for x in ap.ap] + [[0, n]])


def bc_mid(ap, nh):
    a = [list(x) for x in ap.ap]
    return AP(ap.tensor, ap.offset, [a[0], [0, nh]] + a[1:])


class KB:
    def __init__(self, nc):
        self.nc = nc
        self.c = Ctx(nc)
        self.uid = 0

    def name(self, p):
        self.uid += 1
        return f"{p}_{self.uid}"

    def sb(self, es, shape, dt, name="t"):
        h = es.enter_context(self.nc.sbuf_tensor(self.name(name), list(shape), dt))
        return Tl(h.ap())

    def sbn(self, es, n, shape, dt, name="t"):
        return [self.sb(es, shape, dt, name) for _ in range(n)]

    def _rw(self, outs, ins):
        reads, writes = [], []
        for t in ins:
            if t is None:
                continue
            (writes if t.psum else reads).append(t.buf)
        for t in outs:
            writes.append(t.buf)
        return reads, writes

    def op(self, eng, fn, outs, ins, pw=()):
        reads, writes = self._rw(outs, ins)
        pwb = [t.buf for t in pw]
        return self.c.emit(eng, fn, reads=reads, writes=writes, pwrites=pwb)

    def dma(self, out, in_, sem, q="sp", pw=False):
        o, i = out.ap, in_.ap
        reads = [in_.buf]
        if pw:
            return self.c.emit(q, lambda e: e.dma_start(out=o, in_=i), reads=reads, pwrites=[out.buf], dsem=sem)
        return self.c.emit(q, lambda e: e.dma_start(out=o, in_=i), reads=reads, writes=[out.buf], dsem=sem)

    def mm(self, out, lhsT, rhs, start=True, stop=True):
        o, l, r = out.ap, lhsT.ap, rhs.ap
        return self.op("pe", lambda e: e.matmul(o, lhsT=l, rhs=r, start=start, stop=stop, skip_group_check=True), [out], [lhsT, rhs])

    def tr(self, out, in_, ident):
        o, i, d = out.ap, in_.ap, ident.ap
        return self.op("pe", lambda e: e.transpose(out=o, in_=i, identity=d), [out], [in_, ident])

    def act(self, out, in_, func, bias=None, scale=None, accum=None, eng="act"):
        o, i = out.ap, in_.ap
        kw = {}
        ins = [in_]
        if bias is not None:
            if isinstance(bias, Tl):
                kw["bias"] = bias.ap
                ins.append(bias)
            else:
                kw["bias"] = bias
        if scale is not None:
            if isinstance(scale, Tl):
                kw["scale"] = scale.ap
                ins.append(scale)
            else:
                kw["scale"] = scale
        outs = [out]
        if accum is not None:
            kw["accum_out"] = accum.ap
            outs.append(accum)
        return self.op("act", lambda e: e.activation(out=o, in_=i, func=func, **kw), outs, ins)

    def tt(self, eng, out, in0, in1, op):
        o, a, b = out.ap, in0.ap, in1.ap
        return self.op(eng, lambda e: e.tensor_tensor(out=o, in0=a, in1=b, op=op), [out], [in0, in1])

    def ts(self, eng, out, in0, s1, op0, s2=None, op1=None):
        o, a = out.ap, in0.ap
        ins = [in0]
        v1 = s1
        if isinstance(s1, Tl):
            ins.append(s1)
            v1 = s1.ap
        v2 = s2
        if isinstance(s2, Tl):
            ins.append(s2)
            v2 = s2.ap
        if op1 is None:
            return self.op(eng, lambda e: e.tensor_scalar(out=o, in0=a, scalar1=v1, scalar2=None, op0=op0), [out], ins)
        return self.op(eng, lambda e: e.tensor_scalar(out=o, in0=a, scalar1=v1, scalar2=v2, op0=op0, op1=op1), [out], ins)

    def stt(self, out, in0, scalar, in1, op0, op1):
        o, a, b = out.ap, in0.ap, in1.ap
        ins = [in0, in1]
        sv = scalar
        if isinstance(scalar, Tl):
            ins.append(scalar)
            sv = scalar.ap
        return self.op("dve", lambda e: e.scalar_tensor_tensor(out=o, in0=a, scalar=sv, in1=b, op0=op0, op1=op1), [out], ins)

    def copy(self, eng, out, in_):
        o, i = out.ap, in_.ap
        if eng == "act":
            return self.op("act", lambda e: e.copy(out=o, in_=i), [out], [in_])
        return self.op(eng, lambda e: e.tensor_copy(out=o, in_=i), [out], [in_])

    def memset(self, eng, out, val):
        o = out.ap
        return self.op(eng, lambda e: e.memset(o, val), [out], [])

    def recip(self, out, in_):
        o, i = out.ap, in_.ap
        return self.op("dve", lambda e: e.reciprocal(out=o, in_=i), [out], [in_])

    def reduce(self, out, in_, op=ALU.add):
        o, i = out.ap, in_.ap
        return self.op("dve", lambda e: e.tensor_reduce(out=o, in_=i, axis=AX.X, op=op), [out], [in_])

    def rstd(self, es_tmp, out, ssq, n, tmp):
        self.act(tmp, ssq, AF.Sqrt, bias=self.epsb[0:ssq.ap.shape[0], :], scale=1.0 / n)
        self.recip(out, tmp)


def t5_bucket_np(dist):
    dist = np.maximum(dist, 0)
    max_exact = 16
    scaled = np.log(np.maximum(dist, 1).astype(np.float32) / np.float32(max_exact)) / np.float32(np.log(2048 / 16))
    large = np.minimum(max_exact + (scaled.astype(np.float32) * np.float32(16)).astype(np.int32), 31)
    return np.where(dist < max_exact, dist, large)


def host_consts():
    bf = ml_dtypes.bfloat16
    cst = {}
    cst["ident_bf"] = np.eye(128, dtype=np.float32).astype(bf)
    cst["anti_bf"] = np.eye(128, dtype=np.float32)[::-1].copy().astype(bf)
    cst["ident_f"] = np.eye(128, dtype=np.float32)
    da = np.arange(XA) - 511
    oh = np.zeros((32, XA + XC), np.float32)
    ba = t5_bucket_np(da)
    oh[ba, np.arange(XA)] = 1.0
    dc = np.arange(XC) - 2063
    bc = t5_bucket_np(dc)
    oh[bc, XA + np.arange(XC)] = 1.0
    cst["onehot"] = oh
    mult = np.zeros((4, XC), np.float32)
    mult[0, :XA] = (da >= 0)
    mult[1, :XA] = (da >= 0) & (da < 512)
    mult[2, :XA] = ((da >= 0) & (da <= 128)).astype(np.float32) + ((da >= 0) & (da % 4 == 0) & (da <= 512)) + ((da >= 0) & (da % 16 == 0) & (da <= 2048))
    mult[3, :] = (dc >= 0)
    with np.errstate(divide="ignore"):
        mult[:] = np.where(mult > 0, np.log(np.maximum(mult, 1e-30)), -30000.0)
    mult[0:3, XA:] = 0
    cst["mult"] = np.ascontiguousarray(np.broadcast_to(mult[:, None, :], (4, 16, XC))).astype(np.float32)
    n = 128 * (np.arange(256) // 128) + 127 - (np.arange(256) % 128)
    c_start = n[:, None] * 16
    s_start = np.arange(64)[None, :] * 64
    ov = np.clip(np.minimum(c_start + 32, s_start + 64) - np.maximum(c_start, s_start), 0, None).astype(np.float32) / 32
    ov[n == 255] = 0
    cst["overlap"] = ov.astype(bf)
    key = 128 * (np.arange(S) // 128) + 127 - (np.arange(S) % 128)
    ex = np.zeros((64, S), np.float32)
    ex[key // 64, np.arange(S)] = 1
    cst["exr"] = ex.astype(bf)
    t = np.arange(S)[:, None]
    j = np.arange(64)[None, :]
    cur = t // 64
    fm = np.where((j == 0) | (j == cur) | (j == cur - 1), 1e6, np.where(j * 64 > t, -1e6, 0.0)).astype(np.float32)
    cst["fm"] = fm
    cst["ecst"] = np.ascontiguousarray(np.broadcast_to((np.arange(NE) * S).astype(np.float32)[None, :], (128, NE)))
    cst["utri"] = np.triu(np.ones((128, 128), np.float32), k=1).astype(bf)
    cst["ones_bf"] = np.ones((128, 128), np.float32).astype(bf)
    thr = np.broadcast_to((np.arange(8) * 512).astype(np.float32)[None, None, :], (128, NE, 8))
    cst["jthr"] = np.ascontiguousarray(thr).reshape(128, NE * 8)
    return cst


class Prog:
    def __init__(self, debug=()):
        self.debug = set(debug)
        nc = self.nc = bass.Bass("TRN2", target_bir_lowering=False)
        self.kb = KB(nc)
        self.es = contextlib.ExitStack()
        self.inp = {}
        self.scr = {}

    def din(self, name, shape, dt=F32):
        t = self.nc.dram_tensor(name, list(shape), dt, kind="ExternalInput").ap()
        self.inp[name] = Tl(t)
        return self.inp[name]

    def dscr(self, name, shape, dt):
        kind = "ExternalOutput" if name in self.debug else "Internal"
        t = self.nc.dram_tensor(name, list(shape), dt, kind=kind).ap()
        self.scr[name] = Tl(t)
        return self.scr[name]

    def declare(self):
        d = self.din
        d("x", [S, D]); d("rel_bias", [32, 16]); d("attn_norm", [DEPTH, D]); d("w_in", [DEPTH, D, INW])
        d("nsa_q_norm", [DEPTH, 64]); d("nsa_k_norm", [DEPTH, 3, 64]); d("cmp_pos", [DEPTH, 2, 32, 64])
        d("cmp_w1", [DEPTH, 2, 2048, 256]); d("cmp_b1", [DEPTH, 2, 256]); d("cmp_w2", [DEPTH, 2, 256, 64])
        d("dil_q_norm", [DEPTH, 64]); d("dil_k_norm", [DEPTH, 64]); d("out_norm", [DEPTH, D]); d("w_out", [DEPTH, D, D])
        d("ffn_norm", [DEPTH, D]); d("ffn_w_gate", [1, D, DFF]); d("ffn_w_up", [1, D, DFF]); d("ffn_w_down", [1, DFF, D])
        d("router_w", [1, D, NE]); d("exp_w_gate", [1, NE, D, DFE]); d("exp_w_up", [1, NE, D, DFE]); d("exp_w_down", [1, NE, DFE, D])
        d("ident_bf", [128, 128], BF16); d("anti_bf", [128, 128], BF16); d("ident_f", [128, 128])
        d("onehot", [32, XA + XC]); d("mult", [4, 16, XC]); d("overlap", [256, 64], BF16); d("exr", [64, S], BF16); d("fm", [S, 64])
        d("ecst", [128, NE]); d("utri", [128, 128], BF16); d("ones_bf", [128, 128], BF16); d("jthr", [128, NE * 8])
        self.out = Tl(self.nc.dram_tensor("out", [S, D], F32, kind="ExternalOutput").ap())
        s = self.dscr
        s("wtab", [4, 16, XC], BF16)
        s("qaT", [8, 64, S], BF16); s("kcvT", [2, 2, 64, S], BF16); s("kswT", [2, 2, 64, S], BF16)
        s("vsw", [S, 256], BF16); s("gates", [S, 24], F32)
        s("qbT", [8, 64, S], BF16); s("kbT", [8, 64, S], BF16); s("vb", [S, 512], BF16)
        s("OC", [S, 512], F32); s("O", [S, D], F32); s("H1", [S, D], F32)
        s("HP", [S, D], F32); s("XE", [NE * S, D], BF16); s("YE", [NE * S, D], F32)
        if "dbg" in self.debug:
            s("dbg", [128, 4096], F32)

    def setup_globals(self):
        kb, es = self.kb, self.es
        nc = self.nc
        self.banks = []
        for i in range(8):
            h = es.enter_context(nc.psum_tensor(f"bank{i}", [128, 512], F32))
            self.banks.append(Tl(h.ap(), psum=True))
        self.ident_bf = kb.sb(es, [128, 128], BF16, "identbf")
        self.anti_bf = kb.sb(es, [128, 128], BF16, "antibf")
        self.ident_f = kb.sb(es, [128, 128], F32, "identf")
        kb.epsb = kb.sb(es, [128, 1], F32, "epsb")
        self.one11 = kb.sb(es, [1, 1], F32, "one11")
        self.kcmpT = kb.sb(es, [64, 2, 2, 128], BF16, "kcmpT")
        self.vcaug = kb.sb(es, [128, 2, 2, 128], BF16, "vcaug")
        sem = kb.c.dma_sem()
        kb.dma(self.ident_bf, self.inp["ident_bf"], sem)
        kb.dma(self.anti_bf, self.inp["anti_bf"], sem)
        kb.dma(self.ident_f, self.inp["ident_f"], sem)
        kb.memset("dve", kb.epsb, EPS)
        kb.memset("dve", self.one11, 1.0)
        kb.c.barrier()

    def phase_tables(self):
        kb = self.kb
        with contextlib.ExitStack() as es:
            tbl = kb.sb(es, [32, 16], F32, "tbl")
            oh = kb.sbn(es, 2, [32, 512], F32, "oh")
            e32 = kb.sbn(es, 2, [16, 512], F32, "e32")
            mt = kb.sbn(es, 2, [16, 512], F32, "mt")
            wb = kb.sbn(es, 2, [16, 512], BF16, "wb")
            sems = [kb.c.dma_sem() for _ in range(7)]
            kb.dma(tbl, self.inp["rel_bias"], sems[0])
            wtab = self.scr["wtab"]
            k = 0
            for ci in range((XA + XC) // 512):
                x0 = ci * 512
                o = oh[ci % 2]
                kb.dma(o, self.inp["onehot"][:, x0:x0 + 512], sems[1 + ci % 2])
                bank = self.banks[ci % 2]
                kb.mm(bank[0:16, :], tbl, o)
                e = e32[ci % 2]
                kb.copy("act", e, bank[0:16, :])
                tabs = [(0, x0), (1, x0), (2, x0)] if x0 < XA else [(3, x0 - XA)]
                for tb, xx in tabs:
                    m = mt[k % 2]
                    w = wb[k % 2]
                    kb.dma(m, self.inp["mult"][tb, :, xx:xx + 512], sems[3 + k % 2])
                    kb.tt("dve", w, e, m, ALU.add)
                    kb.dma(wtab[tb, :, xx:xx + 512], w, sems[5 + k % 2], pw=True)
                    k += 1
            kb.c.barrier()
            kb.c.release(sems)

    def phase_inproj(self, layer, hin_dram):
        kb = self.kb
        I = self.inp
        with contextlib.ExitStack() as es:
            W = kb.sb(es, [128, 8, INW], BF16, "win")
            gA = kb.sb(es, [128, D], F32, "gA")
            g6 = kb.sb(es, [128, 6, 64], F32, "g6")
            hin = kb.sbn(es, 2, [128, D], F32, "hin")
            junk = kb.sb(es, [128, INW], F32, "junk")
            junkA = kb.sb(es, [128, D], F32, "junkA")
            ssq = kb.sbn(es, 2, [128, 1], F32, "ssq")
            rms = kb.sbn(es, 2, [128, 1], F32, "rms")
            rstd = kb.sbn(es, 2, [128, 1], F32, "rstd")
            u = kb.sbn(es, 2, [128, D], BF16, "u")
            uT = kb.sbn(es, 2, [128, 8, 128], BF16, "uT")
            pj = kb.sbn(es, 2, [128, INW], F32, "pj")
            ssh = kb.sbn(es, 2, [128, 28], F32, "ssh")
            rmh = kb.sbn(es, 2, [128, 28], F32, "rmh")
            rsh = kb.sbn(es, 2, [128, 28], F32, "rsh")
            t1 = kb.sbn(es, 2, [128, 1024], F32, "t1")
            nb = kb.sbn(es, 2, [128, 2328], BF16, "nb")
            vall = kb.sbn(es, 2, [128, 768], BF16, "vall")
            gt = kb.sbn(es, 2, [128, 24], F32, "gt")
            stg_q = kb.sbn(es, 2, [128, 4, 128], BF16, "stgq")
            stg_c = kb.sbn(es, 2, [128, 2, 128], BF16, "stgc")
            stg_k = kb.sbn(es, 2, [128, 2, 128], BF16, "stgk")
            stg_qb = kb.sbn(es, 2, [128, 4, 128], BF16, "stgqb")
            stg_kb = kb.sbn(es, 2, [128, 4, 128], BF16, "stgkb")
            stg_v = kb.sbn(es, 2, [128, 768], BF16, "stgv")
            sems = [kb.c.dma_sem() for _ in range(20)]
            wv = I["w_in"][layer].v(I["w_in"].ap[layer].rearrange("(kc p) n -> p kc n", p=128))
            for kc in range(8):
                kb.dma(W[:, kc, :], wv[:, kc, :], sems[0], q="pool", pw=True)
            kb.dma(gA, I["attn_norm"].v(I["attn_norm"].ap[layer].partition_broadcast(128)), sems[1])
            gsrc = [I["nsa_q_norm"].ap[layer], I["nsa_k_norm"].ap[layer, 1], I["nsa_k_norm"].ap[layer, 2],
                    I["dil_q_norm"].ap[layer], I["dil_k_norm"].ap[layer], I["nsa_k_norm"].ap[layer, 0]]
            for i, a in enumerate(gsrc):
                kb.dma(g6[:, i, :], Tl(a.partition_broadcast(128), I["nsa_q_norm"].buf), sems[1], pw=True)
            kb.ts("dve", g6[:, 0, :], g6[:, 0, :], 0.125, ALU.mult)
            kb.ts("dve", g6[:, 3, :], g6[:, 3, :], 0.125, ALU.mult)
            self.g6_k0 = None
            bk = self.banks
            hv = hin_dram.v(hin_dram.ap.rearrange("(t p) d -> t p d", p=128))
            qaT2 = self.scr["qaT"].v(self.scr["qaT"].ap.rearrange("h d s -> (h d) s").rearrange("(a p) s -> p a s", p=128))
            kcvT2 = self.scr["kcvT"].v(self.scr["kcvT"].ap.rearrange("k g d s -> (k g d) s").rearrange("(a p) s -> p a s", p=128))
            kswT2 = self.scr["kswT"].v(self.scr["kswT"].ap.rearrange("k g d s -> (k g d) s").rearrange("(a p) s -> p a s", p=128))
            qbT2 = self.scr["qbT"].v(self.scr["qbT"].ap.rearrange("h d s -> (h d) s").rearrange("(a p) s -> p a s", p=128))
            kbT2 = self.scr["kbT"].v(self.scr["kbT"].ap.rearrange("h d s -> (h d) s").rearrange("(a p) s -> p a s", p=128))
            def pre(t):
                s = t % 2
                ts_ = slice(t * 128, (t + 1) * 128)
                kb.dma(hin[s], hv[t], sems[2 + s], q="pool")
                h = hin[s]
                kb.act(junkA, h, AF.Square, accum=ssq[s])
                kb.act(rms[s], ssq[s], AF.Sqrt, bias=kb.epsb, scale=1.0 / D)
                kb.recip(rstd[s], rms[s])
                kb.stt(u[s], h, rstd[s], gA, ALU.mult, ALU.mult)
                b2 = bk[2].v(bk[2].ap.bitcast(BF16))
                for kc in range(8):
                    kb.tr(b2[:, kc * 128:(kc + 1) * 128], u[s][:, kc * 128:(kc + 1) * 128], self.ident_bf)
                kb.copy("act", uT[s], b2.v(b2.ap.rearrange("p (a b) -> p a b", b=128)))
            def mmf(t):
                s = t % 2
                for cg in range(6):
                    c0 = cg * 512
                    cw = min(512, INW - c0)
                    bank = bk[cg % 2]
                    for kc in range(8):
                        kb.mm(bank[:, 0:cw], uT[s][:, kc, :], W[:, kc, c0:c0 + cw], start=(kc == 0), stop=(kc == 7))
                    kb.copy("act" if cg % 2 == 0 else "dve", pj[s][:, c0:c0 + cw], bank[:, 0:cw])
            def back(t):
                s = t % 2
                ts_ = slice(t * 128, (t + 1) * 128)
                p = pj[s]
                kb.act(junk, p, AF.Square)
                for (c0, nh, r0) in ((C_QA, 8, 0), (C_KS, 2, 8), (C_KW, 2, 10), (C_QB, 16, 12)):
                    kb.reduce(ssh[s][:, r0:r0 + nh], junk.v(junk.ap[:, c0:c0 + nh * 64].rearrange("p (h d) -> p h d", d=64)))
                kb.act(rmh[s], ssh[s], AF.Sqrt, bias=kb.epsb, scale=1.0 / 64)
                kb.recip(rsh[s], rmh[s])
                n_ = nb[s]
                for (c0, nh, r0, gi) in ((C_QA, 8, 0, 0), (C_KS, 2, 8, 1), (C_KW, 2, 10, 2), (C_QB, 8, 12, 3), (C_KB, 8, 20, 4)):
                    tv = t1[s].v(t1[s].ap[:, 0:nh * 64].rearrange("p (h d) -> p h d", d=64))
                    pv = p.v(p.ap[:, c0:c0 + nh * 64].rearrange("p (h d) -> p h d", d=64))
                    kb.tt("dve", tv, pv, rsh[s].v(bc_last(rsh[s].ap[:, r0:r0 + nh], 64)), ALU.mult)
                    nv = n_.v(n_.ap[:, c0:c0 + nh * 64].rearrange("p (h d) -> p h d", d=64))
                    kb.tt("pool", nv, tv, g6.v(bc_mid(g6.ap[:, gi, :], nh)), ALU.mult)
                kb.copy("pool", n_[:, C_KC:C_KC + 256], p[:, C_KC:C_KC + 256])
                kb.copy("act", vall[s][:, 0:128], p[:, C_VS:C_VS + 128])
                kb.copy("act", vall[s][:, 128:256], p[:, C_VW:C_VW + 128])
                kb.copy("act", vall[s][:, 256:768], p[:, C_VB:C_VB + 512])
                kb.act(gt[s], p[:, C_GA:C_GA + 24], AF.Sigmoid)
                kb.dma(self.scr["gates"][ts_, :], gt[s], sems[4 + s], pw=True)
                b3 = bk[3].v(bk[3].ap.bitcast(BF16))
                for a in range(4):
                    kb.tr(b3[:, a * 128:(a + 1) * 128], n_[:, C_QA + a * 128:C_QA + (a + 1) * 128], self.ident_bf)
                for a in range(4):
                    kb.tr(b3[:, 512 + a * 128:512 + (a + 1) * 128], n_[:, C_QB + a * 128:C_QB + (a + 1) * 128], self.ident_bf)
                kb.copy("dve", stg_q[s], b3.v(b3.ap[:, 0:512].rearrange("p (a b) -> p a b", b=128)))
                kb.copy("act", stg_qb[s], b3.v(b3.ap[:, 512:1024].rearrange("p (a b) -> p a b", b=128)))
                kb.dma(qaT2[:, :, ts_], stg_q[s], sems[6 + s], pw=True)
                kb.dma(qbT2[:, :, ts_], stg_qb[s], sems[8 + s], pw=True)
                b4 = bk[4].v(bk[4].ap.bitcast(BF16))
                for a in range(2):
                    kb.tr(b4[:, a * 128:(a + 1) * 128], n_[:, C_KC + a * 128:C_KC + (a + 1) * 128], self.ident_bf)
                kb.copy("dve", stg_c[s], b4.v(b4.ap[:, 0:256].rearrange("p (a b) -> p a b", b=128)))
                kb.dma(kcvT2[:, :, ts_], stg_c[s], sems[10 + s], pw=True)
                for a, c0 in enumerate((C_KS, C_KW)):
                    kb.mm(bk[5][:, a * 128:(a + 1) * 128], n_[:, c0:c0 + 128], self.anti_bf)
                kb.copy("act", stg_k[s], bk[5].v(bk[5].ap[:, 0:256].rearrange("p (a b) -> p a b", b=128)))
                kb.dma(kswT2[:, :, ts_], stg_k[s], sems[12 + s], pw=True)
                for a in range(4):
                    kb.mm(bk[6][:, a * 128:(a + 1) * 128], n_[:, C_KB + a * 128:C_KB + (a + 1) * 128], self.anti_bf)
                kb.copy("dve", stg_kb[s], bk[6].v(bk[6].ap.rearrange("p (a b) -> p a b", b=128)))
                kb.dma(kbT2[:, :, ts_], stg_kb[s], sems[14 + s], pw=True)
                kb.mm(bk[7], self.anti_bf, vall[s][:, 256:768])
                kb.copy("act", stg_v[s][:, 256:768], bk[7])
                kb.mm(bk[5][:, 256:512], self.anti_bf, vall[s][:, 0:256])
                kb.copy("dve", stg_v[s][:, 0:256], bk[5][:, 256:512])
                kb.dma(self.scr["vsw"][ts_, :], stg_v[s][:, 0:256], sems[16 + s], pw=True)
                kb.dma(self.scr["vb"][ts_, :], stg_v[s][:, 256:768], sems[18 + s], pw=True)

            kb.c.play(kb.c.record(lambda: pre(0)))
            kb.c.play(kb.c.record(lambda: pre(1)), kb.c.record(lambda: mmf(0)))
            for t in range(NT):
                lb = kb.c.record(lambda: back(t))
                k = next(i for i, o in enumerate(lb) if o[0] == "pe")
                lists = [lb[:k]]
                if t + 1 < NT:
                    lists.insert(0, kb.c.record(lambda: mmf(t + 1)))
                if t + 2 < NT:
                    lists.insert(0, kb.c.record(lambda: pre(t + 2)))
                kb.c.play(*lists)
                kb.c.play(lb[k:])
            kb.c.barrier()
            kb.c.release(sems)

    def phase_compress(self, layer):
        kb = self.kb
        I = self.inp
        bk = self.banks
        with contextlib.ExitStack() as es:
            kvT = kb.sbn(es, 2, [64, S], BF16, "kvT")
            w1 = kb.sbn(es, 2, [64, 32, 256], BF16, "w1")
            w2 = kb.sbn(es, 2, [128, 2, 64], BF16, "w2")
            pos = kb.sbn(es, 2, [32, 64], F32, "pos")
            posT = kb.sbn(es, 2, [64, 32], BF16, "posT")
            b1 = kb.sbn(es, 2, [1, 256], F32, "b1")
            bias = kb.sbn(es, 2, [128, 2], F32, "bias")
            hid = kb.sbn(es, 2, [128, 2, 256], BF16, "hid")
            gk = kb.sb(es, [128, 64], F32, "gk")
            xk = kb.sbn(es, 2, [128, 64], F32, "xk")
            jk = kb.sb(es, [128, 64], F32, "jk")
            sq1 = kb.sbn(es, 2, [128, 1], F32, "sq1")
            rm1 = kb.sbn(es, 2, [128, 1], F32, "rm1")
            rs1 = kb.sbn(es, 2, [128, 1], F32, "rs1")
            xb = kb.sbn(es, 2, [128, 64], BF16, "xb")
            sems = [kb.c.dma_sem() for _ in range(8)]
            kb.dma(gk, Tl(I["nsa_k_norm"].ap[layer, 0].partition_broadcast(128), I["nsa_k_norm"].buf), sems[0])
            it = 0
            for g in range(2):
                for kv in range(2):
                    s = it % 2
                    it += 1
                    kb.dma(kvT[s], self.scr["kcvT"][kv, g], sems[1 + s])
                    w1src = I["cmp_w1"].v(I["cmp_w1"].ap[layer, kv].rearrange("(l d) c -> d l c", d=64))
                    for lq in range(4):
                        kb.dma(w1[s][:, lq * 8:(lq + 1) * 8, :], w1src[:, lq * 8:(lq + 1) * 8, :], sems[3 + s], q="pool", pw=True)
                    kb.dma(w2[s], I["cmp_w2"].v(I["cmp_w2"].ap[layer, kv].rearrange("(hh p) c -> p hh c", p=128)), sems[3 + s], q="pool", pw=True)
                    kb.dma(pos[s], I["cmp_pos"][layer, kv], sems[5 + s], pw=True)
                    kb.dma(b1[s], I["cmp_b1"][layer, kv:kv + 1, :], sems[5 + s], pw=True)
                    kb.tr(bk[2][0:64, 0:32], pos[s], self.ident_f[0:32, 0:32])
                    kb.copy("dve", posT[s], bk[2][0:64, 0:32])
                    for hh in range(2):
                        hs = slice(hh * 128, (hh + 1) * 128)
                        for l in range(32):
                            kb.mm(bk[3][:, hh:hh + 1], w1[s][:, l, hs], posT[s][:, l:l + 1], start=(l == 0), stop=False)
                        kb.mm(bk[3][:, hh:hh + 1], b1[s][0:1, hs], self.one11, start=False, stop=True)
                    kb.copy("dve", bias[s], bk[3][:, 0:2])
                    kb.memset("pool", hid[s][:, :, 255:256], 0.0)
                    for hh in range(2):
                        hs = slice(hh * 128, (hh + 1) * 128)
                        bank = bk[hh]
                        ka = kvT[s].ap
                        for l in range(32):
                            rhs = kvT[s].v(AP(ka.tensor, ka.offset + l, [list(ka.ap[0]), [16, 255]]))
                            kb.mm(bank[:, 0:255], w1[s][:, l, hs], rhs, start=(l == 0), stop=(l == 31))
                        kb.act(hid[s][:, hh, 0:255], bank[:, 0:255], AF.Gelu_apprx_tanh, bias=bias[s][:, hh:hh + 1])
                    for nt in range(2):
                        ns = slice(nt * 128, (nt + 1) * 128)
                        bank = bk[4 + nt]
                        for hh in range(2):
                            kb.mm(bank[:, 0:64], hid[s][:, hh, ns], w2[s][:, hh, :], start=(hh == 0), stop=(hh == 1))
                        j = (it + nt) % 2
                        if kv == 0:
                            kb.copy("dve", xk[j], bank[:, 0:64])
                            kb.act(jk, xk[j], AF.Square, accum=sq1[j])
                            kb.act(rm1[j], sq1[j], AF.Sqrt, bias=kb.epsb, scale=1.0 / 64)
                            kb.recip(rs1[j], rm1[j])
                            kb.stt(xb[j], xk[j], rs1[j], gk, ALU.mult, ALU.mult)
                            kb.mm(bk[6 + nt][0:64, 0:128], xb[j], self.anti_bf)
                            kb.copy("act", self.kcmpT[:, g, nt, :], bk[6 + nt][0:64, 0:128])
                        else:
                            kb.copy("dve", xb[j], bank[:, 0:64])
                            kb.mm(bk[6 + nt][:, 0:64], self.anti_bf, xb[j])
                            kb.copy("act", self.vcaug[:, g, nt, 0:64], bk[6 + nt][:, 0:64])
            for g in range(2):
                for nt in range(2):
                    kb.dma(self.vcaug[:, g, nt, 64:128], self.inp["overlap"][nt * 128:(nt + 1) * 128, :], sems[7], pw=True)
            kb.c.barrier()
            kb.c.release(sems)


    def run_attn(self, items, ebuf, tbuf, pbuf):
        kb = self.kb
        bk = self.banks
        n = len(items)
        if n == 0:
            return

        LA = 3
        pending = []

        def score(i):
            it = items[i]
            c0, c1 = it["qa"] * 128, it["qb"] * 128
            kb.mm(bk[i % 4][:, c0:c1], it["kT"], it["q"][:, c0:c1], start=True, stop=False)
            kb.mm(bk[i % 4][:, c0:c1], self.ident_bf, it["strip"][:, c0:c1], start=False, stop=True)
        for i in range(min(LA, n)):
            score(i)
        for i in range(n):
            if i + LA < n:
                score(i + LA)
            it = items[i]
            c0, c1 = it["qa"] * 128, it["qb"] * 128
            p = pbuf[i % len(pbuf)]
            kb.act(p[:, c0:c1], bk[i % 4][:, c0:c1], AF.Exp)
            kb.mm(it["pv"][:, c0:c1], it["vaug"], p[:, c0:c1], start=it["first"], stop=it["last"])
            if it["last"]:
                pending.append((i + 2, it["fin"]))
            while pending and pending[0][0] <= i:
                pending.pop(0)[1]()
        while pending:
            pending.pop(0)[1]()

    def hankel(self, tb, hd, pstep, ncols):
        w = self.scr["wtab"]
        a = w.ap[tb, hd]
        return w.v(AP(a.tensor, a.offset, [[pstep, 128], [1, ncols]]))

    def phase_nsa(self, layer):
        kb = self.kb
        I = self.inp
        bk = self.banks
        with contextlib.ExitStack() as es0:
            gates = kb.sb(es0, [128, NT, 24], F32, "gates")
            selT = kb.sb(es0, [128, 2, S], BF16, "selT")
            sem0 = kb.c.dma_sem()
            kb.dma(gates, self.scr["gates"].v(self.scr["gates"].ap.rearrange("(t p) c -> p t c", p=128)), sem0)
            kb.c.barrier()
            OCv = self.scr["OC"].v(self.scr["OC"].ap.rearrange("(t p) c -> p t c", p=128))
            Ov = self.scr["O"].v(self.scr["O"].ap.rearrange("(t p) c -> p t c", p=128))
            with contextlib.ExitStack() as es:
                fm = kb.sb(es, [128, NT, 64], F32, "fm")
                imp = kb.sb(es, [128, NT, 64], F32, "imp")
                stripc = kb.sbn(es, 2, [128, S], BF16, "stripc")
                qTh = kb.sbn(es, 2, [64, S], BF16, "qTh")
                eb = kb.sbn(es, 3, [128, 512], BF16, "eb")
                pb = kb.sbn(es, 3, [128, 512], BF16, "pb")
                den = kb.sbn(es, 2, [128, 4], F32, "den")
                rd = kb.sbn(es, 2, [128, 4], F32, "rd")
                sc = kb.sbn(es, 2, [128, 4], F32, "sc")
                itmp = kb.sbn(es, 2, [128, 4, 64], F32, "itmp")
                ocs = kb.sbn(es, 2, [128, 4, 64], F32, "ocs")
                impf = kb.sbn(es, 2, [128, 64], F32, "impf")
                imp2 = kb.sbn(es, 2, [128, 64], F32, "imp2")
                m8a = kb.sbn(es, 2, [128, 8], F32, "m8a")
                m8b = kb.sbn(es, 2, [128, 8], F32, "m8b")
                selm = kb.sbn(es, 2, [128, 128], BF16, "selm")
                sems = [kb.c.dma_sem() for _ in range(7)]
                kb.dma(fm, I["fm"].v(I["fm"].ap.rearrange("(t p) c -> p t c", p=128)), sems[0])
                kb.memset("dve", selm[0], 0.0)
                kb.memset("dve", selm[1], 0.0)
                kb.c.barrier()
                it = 0
                cnt = 0
                def hload(hd_):
                    s_ = hd_ % 2
                    kb.dma(stripc[s_], self.hankel(3, hd_, 16, S), sems[1 + s_])
                    kb.dma(qTh[s_], self.scr["qaT"][hd_], sems[3 + s_])
                hload(0)
                for g in range(2):
                    for h in range(4):
                        hd = 4 * g + h
                        s = it % 2
                        it += 1
                        if hd + 1 < 8:
                            hload(hd + 1)
                        for QG in range(8):
                            qs = slice(QG * 512, (QG + 1) * 512)
                            ps = []
                            for nt in range(2 if QG >= 4 else 1):
                                bank = bk[nt]
                                x0 = QG * 512 - nt * 2048
                                kb.mm(bank, self.kcmpT[:, g, nt, :], qTh[s][:, qs], start=True, stop=False)
                                kb.mm(bank, self.ident_bf, stripc[s][:, x0:x0 + 512], start=False, stop=True)
                                p = pb[cnt % 3]
                                cnt += 1
                                kb.act(p, bank, AF.Exp)
                                ps.append(p)
                            u_ = (it * 8 + QG) % 2
                            oc = ocs[u_]
                            ob = bk[2 + u_]
                            for qt in range(4):
                                for nt, p in enumerate(ps):
                                    kb.mm(ob[:, qt * 128:(qt + 1) * 128], p[:, qt * 128:(qt + 1) * 128], self.vcaug[:, g, nt, :], start=(nt == 0), stop=(nt == len(ps) - 1))
                            obv = ob.v(ob.ap.rearrange("p (a b) -> p a b", b=128))
                            kb.reduce(den[u_], obv[:, :, 64:128])
                            kb.ts("dve", den[u_], den[u_], 1e-30, ALU.max)
                            kb.recip(rd[u_], den[u_])
                            kb.tt("dve", sc[u_], rd[u_], gates[:, QG * 4:QG * 4 + 4, hd * 3], ALU.mult)
                            kb.tt("dve", oc, obv[:, :, 0:64], sc[u_].v(bc_last(sc[u_].ap, 64)), ALU.mult)
                            iv = imp[:, QG * 4:QG * 4 + 4, :]
                            if h == 0:
                                kb.tt("dve", iv, obv[:, :, 64:128], rd[u_].v(bc_last(rd[u_].ap, 64)), ALU.mult)
                            else:
                                kb.tt("dve", itmp[u_], obv[:, :, 64:128], rd[u_].v(bc_last(rd[u_].ap, 64)), ALU.mult)
                                kb.tt("pool", iv, iv, itmp[u_], ALU.add)
                            kb.dma(OCv[:, QG * 4:QG * 4 + 4, hd * 64:(hd + 1) * 64], oc, sems[5 + u_], pw=True)
                    b4 = bk[4].v(bk[4].ap.bitcast(BF16))
                    for tile in range(NT):
                        j = tile % 2
                        kb.tt("dve", impf[j], imp[:, tile, :], fm[:, tile, :], ALU.add)
                        a, b, c_ = impf[j].ap, m8a[j].ap, imp2[j].ap
                        kb.op("dve", lambda e, a=a, b=b: e.max(out=b, in_=a), [m8a[j]], [impf[j]])
                        kb.op("dve", lambda e, a=a, b=b, c_=c_: e.match_replace(out=c_, in_to_replace=b, in_values=a, imm_value=-3.0e6), [imp2[j]], [impf[j], m8a[j]])
                        d_ = m8b[j].ap
                        kb.op("dve", lambda e, c_=c_, d_=d_: e.max(out=d_, in_=c_), [m8b[j]], [imp2[j]])
                        kb.ts("dve", selm[j][:, 64:128], impf[j], m8b[j][:, 7:8], ALU.is_ge)
                        kb.tr(b4[:, j * 128:(j + 1) * 128], selm[j], self.ident_bf)
                        kb.ts("dve", selT[64:128, g, tile * 128:(tile + 1) * 128], b4[64:128, j * 128:(j + 1) * 128], -1.0, ALU.add, 30000.0, ALU.mult)
                kb.c.barrier()
                kb.c.release(sems)
            if "selT" in self.debug:
                semd = kb.c.dma_sem()
                kb.dma(self.scr["selT"], selT, semd)
                kb.c.barrier()
            with contextlib.ExitStack() as es:
                if getattr(self, "skip_p3b", False):
                    kb.c.release([sem0])
                    return
                ksT = kb.sbn(es, 2, [128, S], BF16, "ksx")
                kwT = kb.sbn(es, 2, [128, S], BF16, "kwT")
                vsa = kb.sbn(es, 2, [128, NT, 65], BF16, "vsa")
                vwa = kb.sbn(es, 2, [128, NT, 65], BF16, "vwa")
                ssel = kb.sbn(es, 4, [128, 2688], BF16, "ssel")
                swin = kb.sbn(es, 4, [128, 1408], BF16, "swin")
                qT4 = kb.sbn(es, 2, [128, 4, 512], BF16, "qsx")
                oct_ = kb.sbn(es, 2, [128, 4, 256], F32, "oct")
                eb = kb.sbn(es, 5, [128, 512], BF16, "eb")
                tb = kb.sbn(es, 5, [128, 512], BF16, "tb")
                pb = kb.sbn(es, 5, [128, 512], BF16, "pb")
                osb = kb.sbn(es, 2, [65, 512], F32, "osb")
                rd4 = kb.sbn(es, 2, [128, 4], F32, "rd4")
                sc4 = kb.sbn(es, 2, [128, 4], F32, "sc4")
                tmp4 = kb.sbn(es, 2, [128, 4, 64], F32, "tmp4")
                sems = [kb.c.dma_sem() for _ in range(12)]
                kb.memset("pool", kwT[0][64:128, :], 0.0)
                kb.memset("pool", kwT[1][64:128, :], 0.0)
                kb.dma(ksT[0][64:128, :], I["exr"], sems[0], pw=True)
                kb.dma(ksT[1][64:128, :], I["exr"], sems[0], pw=True)
                vswv = self.scr["vsw"].v(self.scr["vsw"].ap.rearrange("(t p) c -> p t c", p=128))
                fcnt = [0]
                for g in range(2):
                    sg = g % 2
                    kb.dma(ksT[sg][0:64, :], self.scr["kswT"][0, g], sems[1 + sg], pw=True)
                    kb.dma(kwT[sg][0:64, :], self.scr["kswT"][1, g], sems[1 + sg], pw=True)
                    kb.dma(vsa[sg][:, :, 0:64], vswv[:, :, g * 64:(g + 1) * 64], sems[1 + sg], pw=True)
                    kb.dma(vwa[sg][:, :, 0:64], vswv[:, :, 128 + g * 64:128 + (g + 1) * 64], sems[1 + sg], pw=True)
                    kb.memset("pool", vsa[sg][:, :, 64:65], 1.0)
                    kb.memset("pool", vwa[sg][:, :, 64:65], 1.0)
                    for h in range(4):
                        kb.dma(ssel[h], self.hankel(0, 4 * g + h, 1, 2688), sems[3], pw=True)
                        kb.dma(swin[h], self.hankel(1, 4 * g + h, 1, 1408), sems[3], pw=True)
                    qsrc = self.scr["qaT"].v(self.scr["qaT"].ap[4 * g:4 * g + 4].rearrange("h d s -> d h s"))
                    def qload(QG, g=g, qsrc=qsrc):
                        sq = QG % 2
                        qs = slice(QG * 512, (QG + 1) * 512)
                        kb.dma(qT4[sq][0:64, :, :], qsrc[:, :, qs], sems[4 + sq], pw=True)
                        kb.op("pool", (lambda e, o=qT4[sq].ap[64:128, :, :], i=bc_mid(selT.ap[64:128, g, qs], 4): e.tensor_copy(out=o, in_=i)), [], [selT], pw=[qT4[sq]])
                        kb.dma(oct_[sq], OCv[:, QG * 4:QG * 4 + 4, g * 256:(g + 1) * 256], sems[6 + sq])
                    qload(0)
                    for QG in range(8):
                        sq = QG % 2
                        qs = slice(QG * 512, (QG + 1) * 512)
                        nK = 4 * QG + 4
                        if QG + 1 < 8:
                            qload(QG + 1)
                        acc = oct_[sq]
                        items = []
                        for h in range(4):
                            hd = 4 * g + h
                            for br in range(2):
                                k0 = 0 if br == 0 else max(0, 4 * QG - 4)
                                pvb = bk[4 + br][0:65, :]

                                def fin(h=h, hd=hd, br=br, pvb=pvb, acc=acc, QG=QG):
                                    f = fcnt[0]
                                    fcnt[0] += 1
                                    o = osb[f % 2]
                                    kb.copy("dve", o, pvb)
                                    tbk = bk[6 + f % 2]
                                    for qt in range(4):
                                        kb.tr(tbk[:, qt * 65:qt * 65 + 65], o[:, qt * 128:(qt + 1) * 128], self.ident_f[0:65, 0:65])
                                    ta = tbk.ap
                                    dens = tbk.v(AP(ta.tensor, ta.offset + 64, [list(ta.ap[0]), [65, 4]]))
                                    vals = tbk.v(AP(ta.tensor, ta.offset, [list(ta.ap[0]), [65, 4], [1, 64]]))
                                    r4, s4, tm = rd4[f % 2], sc4[f % 2], tmp4[f % 2]
                                    kb.recip(r4, dens)
                                    kb.tt("dve", s4, r4, gates[:, QG * 4:QG * 4 + 4, hd * 3 + 1 + br], ALU.mult)
                                    kb.tt("dve", tm, vals, s4.v(bc_last(s4.ap, 64)), ALU.mult)
                                    av = acc[:, :, h * 64:(h + 1) * 64]
                                    kb.tt("pool", av, av, tm, ALU.add)
                                for Kt in range(k0, nK):
                                    if br == 0:
                                        c0 = min(4 * QG - Kt + 3, 16) * 128
                                        items.append(dict(kT=ksT[sg][:, Kt * 128:(Kt + 1) * 128], q=qT4[sq][:, h, :], strip=ssel[h][:, c0:c0 + 512],
                                                          mask=None, vaug=vsa[sg][:, Kt, :], pv=pvb, first=(Kt == k0), last=(Kt == nK - 1), fin=fin,
                                                          qa=max(0, Kt - 4 * QG), qb=4))
                                    else:
                                        c0 = (4 * QG - Kt + 3) * 128
                                        items.append(dict(kT=kwT[sg][:, Kt * 128:(Kt + 1) * 128], q=qT4[sq][:, h, :], strip=swin[h][:, c0:c0 + 512],
                                                          mask=None, vaug=vwa[sg][:, Kt, :], pv=pvb, first=(Kt == k0), last=(Kt == nK - 1), fin=fin,
                                                          qa=max(0, Kt - 4 * QG), qb=min(4, Kt - 4 * QG + 5)))
                        self.run_attn(items, eb, tb, pb)
                        kb.dma(Ov[:, QG * 4:QG * 4 + 4, g * 256:(g + 1) * 256], acc, sems[8 + sq], pw=True)
                kb.c.barrier()
                kb.c.release(sems)
            kb.c.release([sem0])

    def phase_dil(self, layer):
        kb = self.kb
        bk = self.banks
        with contextlib.ExitStack() as es:
            kT = kb.sbn(es, 2, [128, S], BF16, "kbT")
            qT = kb.sbn(es, 2, [128, S], BF16, "qbT")
            for t_ in (kT[0], kT[1], qT[0], qT[1]):
                kb.memset("pool", t_[64:128, :], 0.0)
            va = kb.sbn(es, 2, [128, NT, 65], BF16, "vba")
            sd = kb.sbn(es, 2, [128, 2944], BF16, "sdil")
            eb = kb.sbn(es, 5, [128, 512], BF16, "eb")
            pb = kb.sbn(es, 5, [128, 512], BF16, "pb")
            osb = kb.sbn(es, 2, [65, 512], F32, "osb")
            rd4 = kb.sbn(es, 2, [128, 4], F32, "rd4")
            obs = kb.sbn(es, 2, [128, 4, 64], F32, "obs")
            sems = [kb.c.dma_sem() for _ in range(4)]
            vbv = self.scr["vb"].v(self.scr["vb"].ap.rearrange("(t p) c -> p t c", p=128))
            Ov = self.scr["O"].v(self.scr["O"].ap.rearrange("(t p) c -> p t c", p=128))
            fcnt = [0]

            def load(hd):
                s = hd % 2
                kb.dma(kT[s][0:64, :], self.scr["kbT"][hd], sems[s], pw=True)
                kb.dma(qT[s][0:64, :], self.scr["qbT"][hd], sems[s], pw=True)
                kb.dma(va[s][:, :, 0:64], vbv[:, :, hd * 64:(hd + 1) * 64], sems[s], pw=True)
                kb.memset("pool", va[s][:, :, 64:65], 1.0)
                kb.dma(sd[s], self.hankel(2, 8 + hd, 1, 2944), sems[s])
            load(0)
            for hd in range(8):
                s = hd % 2
                if hd + 1 < 8:
                    load(hd + 1)
                items = []
                for QG in range(8):
                    k0 = max(0, 4 * QG - 16)
                    nK = 4 * QG + 4
                    pvb = bk[4 + QG % 2][0:65, :]

                    def fin(hd=hd, QG=QG, pvb=pvb):
                        f = fcnt[0]
                        fcnt[0] += 1
                        o = osb[f % 2]
                        kb.copy("dve", o, pvb)
                        tbk = bk[6 + f % 2]
                        ob = obs[f % 2]
                        for qt in range(4):
                            kb.tr(tbk[:, qt * 65:qt * 65 + 65], o[:, qt * 128:(qt + 1) * 128], self.ident_f[0:65, 0:65])
                        ta = tbk.ap
                        dens = tbk.v(AP(ta.tensor, ta.offset + 64, [list(ta.ap[0]), [65, 4]]))
                        vals = tbk.v(AP(ta.tensor, ta.offset, [list(ta.ap[0]), [65, 4], [1, 64]]))
                        r4 = rd4[f % 2]
                        kb.recip(r4, dens)
                        kb.tt("dve", ob, vals, r4.v(bc_last(r4.ap, 64)), ALU.mult)
                        kb.dma(Ov[:, QG * 4:QG * 4 + 4, 512 + hd * 64:512 + (hd + 1) * 64], ob, sems[2 + f % 2], pw=True)
                    for Kt in range(k0, nK):
                        c0 = (4 * QG - Kt + 3) * 128
                        items.append(dict(kT=kT[s][:, Kt * 128:(Kt + 1) * 128], q=qT[s][:, QG * 512:(QG + 1) * 512], strip=sd[s][:, c0:c0 + 512],
                                          mask=None, vaug=va[s][:, Kt, :], pv=pvb, first=(Kt == k0), last=(Kt == nK - 1), fin=fin,
                                          qa=max(0, Kt - 4 * QG), qb=min(4, Kt - 4 * QG + 17)))
                self.run_attn(items, eb, None, pb)
            kb.c.barrier()
            kb.c.release(sems)


    def phase_ffn(self, layer, hin_dram, hout_dram):
        kb = self.kb
        I = self.inp
        bk = self.banks
        moe = (layer % 2 == 1)
        li = layer // 2
        dff = DFE if moe else DFF
        nfc = dff // 128
        nfb = dff // 256
        with contextlib.ExitStack() as es:
            Wo = kb.sb(es, [128, 8, D], BF16, "Wo")
            gO = kb.sb(es, [128, D], F32, "gO")
            gF = kb.sb(es, [128, D], F32, "gF")
            hp = kb.sb(es, [128, 4, D], F32, "hp")
            hpt = [Tl(hp.ap[:, i, :]) for i in range(4)]
            vT = kb.sb(es, [128, 8, 512], BF16, "vT")
            hid = kb.sb(es, [128, nfc, 512], BF16, "hid")
            hidc = [Tl(hid.ap[:, i, :]) for i in range(nfc)]
            stg = kb.sbn(es, 2, [128, 8, 256], F32, "stg")
            wgu = kb.sbn(es, 4, [128, 8, 256], BF16, "wgu")
            stgd = kb.sbn(es, 2, [128, 4, 512], F32, "stgd")
            wd = kb.sbn(es, 2, [128, 4, 512], BF16, "wd")
            Ot = kb.sbn(es, 2, [128, D], F32, "Ot")
            ht = kb.sbn(es, 2, [128, D], F32, "ht")
            junk = kb.sb(es, [128, D], F32, "junk")
            on = kb.sbn(es, 2, [128, D], BF16, "on")
            onT = kb.sbn(es, 2, [128, 8, 128], BF16, "onT")
            vb_ = kb.sbn(es, 2, [128, D], BF16, "vb_")
            ss2 = kb.sbn(es, 2, [128, 2], F32, "ss2")
            rm2 = kb.sbn(es, 2, [128, 2], F32, "rm2")
            rs2 = kb.sbn(es, 2, [128, 2], F32, "rs2")
            ss1 = kb.sbn(es, 2, [128, 1], F32, "ss1")
            rm1 = kb.sbn(es, 2, [128, 1], F32, "rm1")
            rs1 = kb.sbn(es, 2, [128, 1], F32, "rs1")
            sg = kb.sbn(es, 2, [128, 512], F32, "sg")
            sems = [kb.c.dma_sem() for _ in range(12)]
            if moe:
                v32 = kb.sbn(es, 2, [128, D], F32, "v32")
                v32T = kb.sb(es, [128, 8, 128], F32, "v32T")
                rw = kb.sb(es, [128, 8, NE], F32, "rw")
                gate = kb.sb(es, [128, 4, NE], F32, "gate")
                lg = kb.sbn(es, 2, [128, NE], F32, "lg")
                m8 = kb.sbn(es, 2, [128, 8], F32, "m8")
                msk = kb.sbn(es, 2, [128, NE], F32, "msk")
                nm1 = kb.sbn(es, 2, [128, 1], F32, "nm1")
                ex = kb.sbn(es, 2, [128, NE], F32, "ex")
                gu = kb.sbn(es, 2, [128, NE], F32, "gu")
                dn = kb.sbn(es, 2, [128, 1], F32, "dn")
                rdn = kb.sbn(es, 2, [128, 1], F32, "rdn")
                kb.dma(rw, I["router_w"].v(I["router_w"].ap[li].rearrange("(kc p) e -> p kc e", p=128)), sems[0])
            wov = I["w_out"].v(I["w_out"].ap[layer].rearrange("(kc p) n -> p kc n", p=128))
            for kc in range(8):
                kb.dma(Wo[:, kc, :], wov[:, kc, :], sems[0], q="pool", pw=True)
            kb.dma(gO, Tl(I["out_norm"].ap[layer].partition_broadcast(128), I["out_norm"].buf), sems[0])
            kb.dma(gF, Tl(I["ffn_norm"].ap[layer].partition_broadcast(128), I["ffn_norm"].buf), sems[0])
            kb.c.barrier()
            Ov = self.scr["O"].v(self.scr["O"].ap.rearrange("(t p) c -> t p c", p=128))
            hv = hin_dram.v(hin_dram.ap.rearrange("(t p) d -> t p d", p=128))
            hov = hout_dram.v(hout_dram.ap.rearrange("(t p) d -> p t d", p=128))
            if moe:
                experts = [(I["exp_w_gate"][li, e], I["exp_w_up"][li, e], I["exp_w_down"][li, e], e) for e in range(NE)]
            else:
                experts = [(I["ffn_w_gate"][li], I["ffn_w_up"][li], I["ffn_w_down"][li], None)]
            cnt = {"ld": 0, "ldd": 0, "fc": 0}
            for blk in range(8):
                def tile_body(ti, blk=blk):
                    t = blk * 4 + ti
                    s = t % 2
                    kb.dma(Ot[s], Ov[t], sems[1 + s])
                    kb.dma(ht[s], hv[t], sems[3 + s])
                    o = Ot[s]
                    kb.act(junk[:, 0:512], o[:, 0:512], AF.Square, accum=ss2[s][:, 0:1])
                    kb.act(junk[:, 512:1024], o[:, 512:1024], AF.Square, accum=ss2[s][:, 1:2])
                    kb.act(rm2[s], ss2[s], AF.Sqrt, bias=kb.epsb, scale=1.0 / 512)
                    kb.recip(rs2[s], rm2[s])
                    for gq in range(2):
                        cs = slice(gq * 512, (gq + 1) * 512)
                        kb.stt(on[s][:, cs], o[:, cs], rs2[s][:, gq:gq + 1], gO[:, cs], ALU.mult, ALU.mult)
                    b0 = bk[s].v(bk[s].ap.bitcast(BF16))
                    for kc in range(8):
                        kb.tr(b0[:, kc * 128:(kc + 1) * 128], on[s][:, kc * 128:(kc + 1) * 128], self.ident_bf)
                    kb.copy("act", onT[s], b0.v(b0.ap.rearrange("p (a b) -> p a b", b=128)))
                    for half in range(2):
                        bank = bk[2 + 2 * s + half]
                        for kc in range(8):
                            kb.mm(bank, onT[s][:, kc, :], Wo[:, kc, half * 512:(half + 1) * 512], start=(kc == 0), stop=(kc == 7))
                        kb.tt("dve", hpt[ti][:, half * 512:(half + 1) * 512], bank, ht[s][:, half * 512:(half + 1) * 512], ALU.add)
                    kb.act(junk, hpt[ti], AF.Square, accum=ss1[s])
                    kb.act(rm1[s], ss1[s], AF.Sqrt, bias=kb.epsb, scale=1.0 / D)
                    kb.recip(rs1[s], rm1[s])
                    if moe:
                        kb.stt(v32[s], hpt[ti], rs1[s], gF, ALU.mult, ALU.mult)
                        kb.copy("pool", vb_[s], v32[s])
                    else:
                        kb.stt(vb_[s], hpt[ti], rs1[s], gF, ALU.mult, ALU.mult)
                    for kc in range(8):
                        kb.tr(b0[:, kc * 128:(kc + 1) * 128], vb_[s][:, kc * 128:(kc + 1) * 128], self.ident_bf)
                    kb.copy("act", vT[:, :, ti * 128:(ti + 1) * 128], b0.v(b0.ap.rearrange("p (a b) -> p a b", b=128)))
                    if moe:
                        for kc in range(8):
                            kb.tr(bk[2 + kc // 4][:, (kc % 4) * 128:(kc % 4 + 1) * 128], v32[s][:, kc * 128:(kc + 1) * 128], self.ident_f)
                        kb.copy("dve", v32T[:, 0:4, :], bk[2].v(bk[2].ap.rearrange("p (a b) -> p a b", b=128)))
                        kb.copy("act", v32T[:, 4:8, :], bk[3].v(bk[3].ap.rearrange("p (a b) -> p a b", b=128)))
                        for kc in range(8):
                            kb.mm(bk[1][:, 0:NE], v32T[:, kc, :], rw[:, kc, :], start=(kc == 0), stop=(kc == 7))
                        kb.copy("dve", lg[s], bk[1][:, 0:NE])
                        a_, b_ = lg[s].ap, m8[s].ap
                        kb.op("dve", lambda e, a_=a_, b_=b_: e.max(out=b_, in_=a_), [m8[s]], [lg[s]])
                        kb.ts("dve", msk[s], lg[s], m8[s][:, 1:2], ALU.is_ge)
                        kb.ts("dve", nm1[s], m8[s][:, 0:1], -1.0, ALU.mult)
                        kb.act(ex[s], lg[s], AF.Exp, bias=nm1[s])
                        kb.tt("dve", gu[s], ex[s], msk[s], ALU.mult)
                        kb.reduce(dn[s], gu[s])
                        kb.recip(rdn[s], dn[s])
                        kb.ts("dve", gate[:, ti, :], gu[s], rdn[s], ALU.mult)
                for pa in (0, 2):
                    la = kb.c.record(lambda: tile_body(pa))
                    lb = kb.c.record(lambda: tile_body(pa + 1))
                    kb.c.play(la, lb)
                for (Wg, Wu, Wd, e) in experts:
                    stages = []
                    wgv = Wg.v(Wg.ap.rearrange("(kc p) f -> p kc f", p=128))
                    wuv = Wu.v(Wu.ap.rearrange("(kc p) f -> p kc f", p=128))
                    wdv = Wd.v(Wd.ap.rearrange("(fc p) n -> p fc n", p=128))
                    for fb in range(nfb):
                        def ld(fb=fb, wgv=wgv, wuv=wuv):
                            res = []
                            for src in (wgv, wuv):
                                k = cnt["ld"]
                                cnt["ld"] += 1
                                st = stg[k % 2]
                                w = wgu[k % 4]
                                kb.dma(st, src[:, :, fb * 256:(fb + 1) * 256], sems[5 + k % 2])
                                kb.copy("act" if k % 2 == 0 else "dve", w, st)
                                res.append(w)
                            return res

                        def comp(ws, fb=fb):
                            wg_, wu_ = ws
                            for j in range(2):
                                fc = fb * 2 + j
                                k = cnt["fc"]
                                cnt["fc"] += 1
                                gb, ub = bk[(k % 2) * 2], bk[(k % 2) * 2 + 1]
                                for kc in range(8):
                                    kb.mm(gb, wg_[:, kc, j * 128:(j + 1) * 128], vT[:, kc, :], start=(kc == 0), stop=(kc == 7))
                                for kc in range(8):
                                    kb.mm(ub, wu_[:, kc, j * 128:(j + 1) * 128], vT[:, kc, :], start=(kc == 0), stop=(kc == 7))
                                kb.act(sg[k % 2], gb, AF.Silu)
                                kb.tt("dve", hidc[fc], sg[k % 2], ub, ALU.mult)
                        stages.append((ld, comp))
                    ngr = (nfc + 3) // 4
                    for half in range(2):
                        for fg in range(ngr):
                            f0 = fg * 4
                            nf = min(4, nfc - f0)

                            def ld(half=half, f0=f0, nf=nf, wdv=wdv):
                                k = cnt["ldd"]
                                cnt["ldd"] += 1
                                st = stgd[k % 2]
                                w = wd[k % 2]
                                kb.dma(st[:, 0:nf, :], wdv[:, f0:f0 + nf, half * 512:(half + 1) * 512], sems[7 + k % 2])
                                kb.copy("act" if k % 2 == 0 else "dve", w[:, 0:nf, :], st[:, 0:nf, :])
                                return w

                            def comp(w, half=half, f0=f0, nf=nf, fg=fg, e=e):
                                for j in range(nf):
                                    fc = f0 + j
                                    for ti in range(4):
                                        kb.mm(bk[4 + ti], hidc[fc][:, ti * 128:(ti + 1) * 128], w[:, j, :], start=(fc == 0), stop=(fc == nfc - 1))
                                if fg == ngr - 1:
                                    for ti in range(4):
                                        dst = hpt[ti][:, half * 512:(half + 1) * 512]
                                        if e is None:
                                            kb.tt("dve", dst, bk[4 + ti], dst, ALU.add)
                                        else:
                                            kb.stt(dst, bk[4 + ti], gate[:, ti, e:e + 1], dst, ALU.mult, ALU.add)
                            stages.append((ld, comp))
                    nxt = stages[0][0]()
                    for i in range(len(stages)):
                        cur = nxt
                        if i + 1 < len(stages):
                            nxt = stages[i + 1][0]()
                        stages[i][1](cur)
                for ti in range(4):
                    kb.dma(hov[:, blk * 4 + ti, :], hpt[ti], sems[9], q="pool", pw=True)
            kb.c.barrier()
            kb.c.release(sems)

    def phase_moe(self, layer, hin_dram, hout_dram):
        kb = self.kb
        c = kb.c
        I = self.inp
        bk = self.banks
        li = layer // 2
        dff = DFE
        nfc = dff // 128
        nfb = dff // 256
        XE = self.scr["XE"]
        YE = self.scr["YE"]
        HP = self.scr["HP"]
        with contextlib.ExitStack() as es0:
            idx_all = kb.sb(es0, [128, NT, 2], I32, "idx_all")
            g_all = kb.sb(es0, [128, NT, 2], F32, "g_all")
            run = kb.sb(es0, [128, NE], F32, "run")
            flags_i = kb.sb(es0, [128, NE * 8], I32, "flags_i")
            HPv = HP.v(HP.ap.rearrange("(t p) d -> t p d", p=128))
            with contextlib.ExitStack() as es:
                Wo = kb.sb(es, [128, 8, D], BF16, "Wo")
                gO = kb.sb(es, [128, D], F32, "gO")
                gF = kb.sb(es, [128, D], F32, "gF")
                ecst = kb.sb(es, [128, NE], F32, "ecst")
                utri = kb.sb(es, [128, 128], BF16, "utri")
                ones_bf = kb.sb(es, [128, 128], BF16, "ones_bf")
                jthr = kb.sb(es, [128, NE * 8], F32, "jthr")
                rw = kb.sb(es, [128, 8, NE], F32, "rw")
                Ot = kb.sbn(es, 4, [128, D], F32, "Ot")
                ht = kb.sbn(es, 4, [128, D], F32, "ht")
                hpt = kb.sbn(es, 2, [128, D], F32, "hpt")
                junk = kb.sb(es, [128, D], F32, "junk")
                on = kb.sbn(es, 2, [128, D], BF16, "on")
                onT = kb.sbn(es, 2, [128, 8, 128], BF16, "onT")
                vb_ = kb.sbn(es, 2, [128, D], BF16, "vb_")
                v32 = kb.sbn(es, 2, [128, D], F32, "v32")
                v32T = kb.sbn(es, 2, [128, 8, 128], F32, "v32T")
                ss2 = kb.sbn(es, 2, [128, 2], F32, "ss2")
                rm2 = kb.sbn(es, 2, [128, 2], F32, "rm2")
                rs2 = kb.sbn(es, 2, [128, 2], F32, "rs2")
                ss1 = kb.sbn(es, 2, [128, 1], F32, "ss1")
                rm1 = kb.sbn(es, 2, [128, 1], F32, "rm1")
                rs1 = kb.sbn(es, 2, [128, 1], F32, "rs1")
                lg = kb.sbn(es, 2, [128, NE], F32, "lg")
                m8 = kb.sbn(es, 2, [128, 8], F32, "m8")
                msk = kb.sbn(es, 2, [128, NE], F32, "msk")
                mskb = kb.sbn(es, 2, [128, NE], BF16, "mskb")
                nm1 = kb.sbn(es, 2, [128, 1], F32, "nm1")
                ex = kb.sbn(es, 2, [128, NE], F32, "ex")
                gu = kb.sbn(es, 2, [128, NE], F32, "gu")
                gate = kb.sbn(es, 2, [128, NE], F32, "gate")
                dn = kb.sbn(es, 2, [128, 1], F32, "dn")
                rdn = kb.sbn(es, 2, [128, 1], F32, "rdn")
                flat = kb.sbn(es, 2, [128, NE], F32, "flat")
                A = kb.sbn(es, 2, [128, NE], F32, "A")
                Bm = kb.sbn(es, 2, [128, NE], F32, "Bm")
                eq = kb.sbn(es, 2, [128, NE], F32, "eq")
                f2 = kb.sbn(es, 2, [128, 2], F32, "f2")
                amax = kb.sbn(es, 2, [128, 1], F32, "amax")
                bmax = kb.sbn(es, 2, [128, 1], F32, "bmax")
                nrep = kb.sb(es, [128, NE * 8], F32, "nrep")
                flags_f = kb.sb(es, [128, NE * 8], F32, "flags_f")
                sems = [kb.c.dma_sem() for _ in range(10)]
                wov = I["w_out"].v(I["w_out"].ap[layer].rearrange("(kc p) n -> p kc n", p=128))
                for kc in range(8):
                    kb.dma(Wo[:, kc, :], wov[:, kc, :], sems[0], q="pool", pw=True)
                kb.dma(gO, Tl(I["out_norm"].ap[layer].partition_broadcast(128), I["out_norm"].buf), sems[0])
                kb.dma(gF, Tl(I["ffn_norm"].ap[layer].partition_broadcast(128), I["ffn_norm"].buf), sems[0])
                kb.dma(rw, I["router_w"].v(I["router_w"].ap[li].rearrange("(kc p) e -> p kc e", p=128)), sems[0])
                kb.dma(ecst, I["ecst"], sems[0])
                kb.dma(utri, I["utri"], sems[0])
                kb.dma(ones_bf, I["ones_bf"], sems[0])
                kb.dma(jthr, I["jthr"], sems[0])
                kb.memset("dve", run, 0.0)
                kb.c.barrier()
                Ov = self.scr["O"].v(self.scr["O"].ap.rearrange("(t p) c -> t p c", p=128))
                hv = hin_dram.v(hin_dram.ap.rearrange("(t p) d -> t p d", p=128))
                lsem = [kb.c.dma_sem() for _ in range(8)]

                def tload(t):
                    kb.dma(Ot[t % 4], Ov[t], lsem[t % 4])
                    kb.dma(ht[t % 4], hv[t], lsem[4 + t % 4])

                def part1(t):
                    s = t % 2
                    o = Ot[t % 4]
                    kb.act(junk[:, 0:512], o[:, 0:512], AF.Square, accum=ss2[s][:, 0:1])
                    kb.act(junk[:, 512:1024], o[:, 512:1024], AF.Square, accum=ss2[s][:, 1:2])
                    kb.act(rm2[s], ss2[s], AF.Sqrt, bias=kb.epsb, scale=1.0 / 512)
                    kb.recip(rs2[s], rm2[s])
                    for gq in range(2):
                        cs = slice(gq * 512, (gq + 1) * 512)
                        kb.stt(on[s][:, cs], o[:, cs], rs2[s][:, gq:gq + 1], gO[:, cs], ALU.mult, ALU.mult)
                    b0 = bk[s].v(bk[s].ap.bitcast(BF16))
                    for kc in range(8):
                        kb.tr(b0[:, kc * 128:(kc + 1) * 128], on[s][:, kc * 128:(kc + 1) * 128], self.ident_bf)
                    kb.copy("act", onT[s], b0.v(b0.ap.rearrange("p (a b) -> p a b", b=128)))
                    hp = hpt[s]
                    for half in range(2):
                        bank = bk[2 + s]
                        for kc in range(8):
                            kb.mm(bank, onT[s][:, kc, :], Wo[:, kc, half * 512:(half + 1) * 512], start=(kc == 0), stop=(kc == 7))
                        kb.tt("dve", hp[:, half * 512:(half + 1) * 512], bank, ht[t % 4][:, half * 512:(half + 1) * 512], ALU.add)
                    kb.dma(HPv[t], hp, sems[5 + s], q="pool")
                    kb.act(junk, hp, AF.Square, accum=ss1[s])
                    kb.act(rm1[s], ss1[s], AF.Sqrt, bias=kb.epsb, scale=1.0 / D)
                    kb.recip(rs1[s], rm1[s])
                    kb.stt(v32[s], hp, rs1[s], gF, ALU.mult, ALU.mult)
                    kb.copy("pool", vb_[s], v32[s])
                    for kc in range(8):
                        kb.tr(bk[4 + s][:, (kc % 4) * 128:(kc % 4 + 1) * 128], v32[s][:, kc * 128:(kc + 1) * 128], self.ident_f)
                        if kc % 4 == 3:
                            kb.copy("dve" if kc == 3 else "act", v32T[s][:, kc - 3:kc + 1, :], bk[4 + s].v(bk[4 + s].ap.rearrange("p (a b) -> p a b", b=128)))
                    for kc in range(8):
                        kb.mm(bk[6 + s][:, 0:NE], v32T[s][:, kc, :], rw[:, kc, :], start=(kc == 0), stop=(kc == 7))
                    kb.copy("dve", lg[s], bk[6 + s][:, 0:NE])
                    a_, b_ = lg[s].ap, m8[s].ap
                    kb.op("dve", lambda e, a_=a_, b_=b_: e.max(out=b_, in_=a_), [m8[s]], [lg[s]])
                    kb.ts("dve", msk[s], lg[s], m8[s][:, 1:2], ALU.is_ge)
                    kb.copy("dve", mskb[s], msk[s])
                    kb.ts("dve", nm1[s], m8[s][:, 0:1], -1.0, ALU.mult)
                    kb.act(ex[s], lg[s], AF.Exp, bias=nm1[s])
                    kb.tt("dve", gu[s], ex[s], msk[s], ALU.mult)
                    kb.reduce(dn[s], gu[s])
                    kb.recip(rdn[s], dn[s])
                    kb.ts("dve", gate[s], gu[s], rdn[s], ALU.mult)
                def part2(t):
                    s = t % 2
                    kb.mm(bk[6][:, 0:NE], utri, mskb[s])
                    kb.mm(bk[7][:, 0:NE], ones_bf, mskb[s])
                    kb.tt("dve", flat[s], bk[6][:, 0:NE], run, ALU.add)
                    kb.tt("dve", run, bk[7][:, 0:NE], run, ALU.add)
                    kb.tt("dve", flat[s], flat[s], ecst, ALU.add)
                    kb.stt(A[s], flat[s], 1.0, msk[s], ALU.add, ALU.mult)
                    kb.reduce(amax[s], A[s], op=ALU.max)
                    kb.ts("dve", f2[s][:, 0:1], amax[s], -1.0, ALU.add)
                    kb.ts("dve", Bm[s], flat[s], -1.0, ALU.mult, 40000.0, ALU.add)
                    kb.tt("dve", Bm[s], Bm[s], msk[s], ALU.mult)
                    kb.reduce(bmax[s], Bm[s], op=ALU.max)
                    kb.ts("dve", f2[s][:, 1:2], bmax[s], -1.0, ALU.mult, 40000.0, ALU.add)
                    kb.copy("dve", idx_all[:, t, :], f2[s])
                    kb.ts("dve", eq[s], A[s], amax[s], ALU.is_equal)
                    kb.tt("dve", eq[s], eq[s], gate[s], ALU.mult)
                    kb.reduce(g_all[:, t, 0:1], eq[s])
                    kb.ts("dve", g_all[:, t, 1:2], g_all[:, t, 0:1], -1.0, ALU.mult, 1.0, ALU.add)
                    for k in range(2):
                        xo, ia, va = XE.ap, idx_all.ap[:, t, k:k + 1], vb_[s].ap
                        c.emit("pool", lambda e, xo=xo, ia=ia, va=va: e.indirect_dma_start(
                            out=xo, out_offset=bass.IndirectOffsetOnAxis(ap=ia, axis=0), in_=va, in_offset=None),
                            reads=[idx_all.buf, vb_[s].buf], pwrites=[XE.buf], dsem=sems[7 + s])
                tload(0)
                tload(1)
                for t in range(0, NT, 2):
                    if t + 2 < NT:
                        tload(t + 2)
                        tload(t + 3)
                    la = kb.c.record(lambda: part1(t))
                    lb = kb.c.record(lambda: part1(t + 1))
                    kb.c.play(la, lb)
                    part2(t)
                    part2(t + 1)
                kb.copy("dve", nrep.v(nrep.ap.rearrange("p (e j) -> p e j", j=8)), run.v(bc_last(run.ap, 8)))
                kb.tt("dve", flags_f, nrep, jthr, ALU.is_gt)
                kb.copy("dve", flags_i, flags_f)
                kb.c.barrier()
                kb.c.release(sems)
                kb.c.release(lsem)
            with contextlib.ExitStack() as es:
                xt_ = kb.sbn(es, 4, [128, D], BF16, "xt_")
                XT = kb.sb(es, [128, 8, 512], BF16, "XT")
                hid = kb.sb(es, [128, nfc, 512], BF16, "hid")
                hidc = [Tl(hid.ap[:, i, :]) for i in range(nfc)]
                stg = kb.sbn(es, 2, [128, 8, 256], F32, "stg")
                wgu = kb.sbn(es, 4, [128, 8, 256], BF16, "wgu")
                stgd = kb.sbn(es, 2, [128, 4, 512], F32, "stgd")
                wd = kb.sbn(es, 2, [128, 4, 512], BF16, "wd")
                sg = kb.sbn(es, 2, [128, 512], F32, "sg")
                yst = kb.sbn(es, 2, [128, 4, 512], F32, "yst")
                sems = [kb.c.dma_sem() for _ in range(8)]
                cnt = {"ld": 0, "ldd": 0, "fc": 0, "x": 0, "y": 0}
                XEv = XE.v(XE.ap.rearrange("(t p) d -> t p d", p=128))
                YEv = YE.v(YE.ap.rearrange("(t p) d -> p t d", p=128))
                for e in range(NE):
                    Wg, Wu, Wd = I["exp_w_gate"][li, e], I["exp_w_up"][li, e], I["exp_w_down"][li, e]
                    wgv = Wg.v(Wg.ap.rearrange("(kc p) f -> p kc f", p=128))
                    wuv = Wu.v(Wu.ap.rearrange("(kc p) f -> p kc f", p=128))
                    wdv = Wd.v(Wd.ap.rearrange("(fc p) n -> p fc n", p=128))
                    for j in range(8):
                        c.cond_begin(flags_i.ap[0:1, e * 8 + j:e * 8 + j + 1], flags_i.buf)
                        t0 = (e * S + j * 512) // 128
                        b0 = bk[0].v(bk[0].ap.bitcast(BF16))
                        for ti in range(4):
                            kb.dma(xt_[ti], XEv[t0 + ti], sems[ti % 2])
                        for ti in range(4):
                            bt = bk[ti % 2].v(bk[ti % 2].ap.bitcast(BF16))
                            for kc in range(8):
                                kb.tr(bt[:, kc * 128:(kc + 1) * 128], xt_[ti][:, kc * 128:(kc + 1) * 128], self.ident_bf)
                            kb.copy("act" if ti % 2 == 0 else "dve", XT[:, :, ti * 128:(ti + 1) * 128], bt.v(bt.ap.rearrange("p (a b) -> p a b", b=128)))
                        stages = []
                        for fb in range(nfb):
                            def ld(fb=fb, wgv=wgv, wuv=wuv):
                                res = []
                                for src in (wgv, wuv):
                                    k = cnt["ld"]
                                    cnt["ld"] += 1
                                    st = stg[k % 2]
                                    w = wgu[k % 4]
                                    kb.dma(st, src[:, :, fb * 256:(fb + 1) * 256], sems[2 + k % 2])
                                    kb.copy("act" if k % 2 == 0 else "dve", w, st)
                                    res.append(w)
                                return res

                            def comp(ws, fb=fb):
                                wg_, wu_ = ws
                                for jj in range(2):
                                    fc = fb * 2 + jj
                                    k = cnt["fc"]
                                    cnt["fc"] += 1
                                    gb, ub = bk[(k % 2) * 2], bk[(k % 2) * 2 + 1]
                                    for kc in range(8):
                                        kb.mm(gb, wg_[:, kc, jj * 128:(jj + 1) * 128], XT[:, kc, :], start=(kc == 0), stop=(kc == 7))
                                    for kc in range(8):
                                        kb.mm(ub, wu_[:, kc, jj * 128:(jj + 1) * 128], XT[:, kc, :], start=(kc == 0), stop=(kc == 7))
                                    kb.act(sg[k % 2], gb, AF.Silu)
                                    kb.tt("dve", hidc[fc], sg[k % 2], ub, ALU.mult)
                            stages.append((ld, comp))
                        ngr = (nfc + 3) // 4
                        for half in range(2):
                            for fg in range(ngr):
                                f0 = fg * 4
                                nf = min(4, nfc - f0)

                                def ld(half=half, f0=f0, nf=nf, wdv=wdv):
                                    k = cnt["ldd"]
                                    cnt["ldd"] += 1
                                    st = stgd[k % 2]
                                    w = wd[k % 2]
                                    kb.dma(st[:, 0:nf, :], wdv[:, f0:f0 + nf, half * 512:(half + 1) * 512], sems[4 + k % 2])
                                    kb.copy("act" if k % 2 == 0 else "dve", w[:, 0:nf, :], st[:, 0:nf, :])
                                    return w

                                def comp(w, half=half, f0=f0, nf=nf, fg=fg, t0=t0):
                                    for jj in range(nf):
                                        fc = f0 + jj
                                        for ti in range(4):
                                            kb.mm(bk[4 + ti], hidc[fc][:, ti * 128:(ti + 1) * 128], w[:, jj, :], start=(fc == 0), stop=(fc == nfc - 1))
                                    if fg == ngr - 1:
                                        k = cnt["y"]
                                        cnt["y"] += 1
                                        y = yst[k % 2]
                                        for ti in range(4):
                                            kb.copy("act" if ti % 2 == 0 else "dve", y[:, ti, :], bk[4 + ti])
                                        kb.dma(YEv[:, t0:t0 + 4, half * 512:(half + 1) * 512], y, sems[6 + k % 2], q="pool", pw=True)
                                stages.append((ld, comp))
                        nxt = stages[0][0]()
                        for i in range(len(stages)):
                            cur = nxt
                            if i + 1 < len(stages):
                                nxt = stages[i + 1][0]()
                            stages[i][1](cur)
                        c.cond_end()
                kb.c.barrier()
                kb.c.release(sems)
            with contextlib.ExitStack() as es:
                hpb = kb.sbn(es, 2, [128, D], F32, "hpb")
                yh = kb.sbn(es, 2, [128, D], F32, "yh")
                yl = kb.sbn(es, 2, [128, D], F32, "yl")
                ob = kb.sbn(es, 2, [128, D], F32, "ob")
                sems = [kb.c.dma_sem() for _ in range(8)]
                hov = hout_dram.v(hout_dram.ap.rearrange("(t p) d -> t p d", p=128))
                for t in range(NT):
                    s = t % 2
                    kb.dma(hpb[s], HPv[t], sems[s])
                    for k, dst in enumerate((yh[s], yl[s])):
                        yi, ia, da = YE.ap, idx_all.ap[:, t, k:k + 1], dst.ap
                        c.emit("pool", lambda e, yi=yi, ia=ia, da=da: e.indirect_dma_start(
                            out=da, out_offset=None, in_=yi, in_offset=bass.IndirectOffsetOnAxis(ap=ia, axis=0)),
                            reads=[idx_all.buf, YE.buf], writes=[dst.buf], dsem=sems[2 + 2 * k + s])
                    kb.stt(ob[s], yh[s], g_all[:, t, 0:1], hpb[s], ALU.mult, ALU.add)
                    kb.stt(ob[s], yl[s], g_all[:, t, 1:2], ob[s], ALU.mult, ALU.add)
                    kb.dma(hov[t], ob[s], sems[6 + s], q="act", pw=True)
                kb.c.barrier()
                kb.c.release(sems)

    def build_all(self):
        self.declare()
        self.setup_globals()
        self.phase_tables()
        hin = self.inp["x"]
        for layer in range(DEPTH):
            hout = self.scr["H1"] if layer == 0 else self.out
            self.phase_inproj(layer, hin)
            self.phase_compress(layer)
            self.phase_nsa(layer)
            self.phase_dil(layer)
            if layer % 2 == 1:
                self.phase_moe(layer, hin, hout)
            else:
                self.phase_ffn(layer, hin, hout)
            hin = hout
        self.kb.c.barrier()
        self.kb.c.flush()
        return self.nc


INPUT_NAMES = ["x", "rel_bias", "attn_norm", "w_in", "nsa_q_norm", "nsa_k_norm", "cmp_pos", "cmp_w1", "cmp_b1", "cmp_w2",
               "dil_q_norm", "dil_k_norm", "out_norm", "w_out", "ffn_norm", "ffn_w_gate", "ffn_w_up", "ffn_w_down",
               "router_w", "exp_w_gate", "exp_w_up", "exp_w_down"]


def make_in_maps(inputs, cores):
    cst = host_consts()
    maps = []
    shared = {k: np.ascontiguousarray(np.asarray(inputs[k], dtype=np.float32)) for k in INPUT_NAMES if k != "x"}
    x = np.asarray(inputs["x"], dtype=np.float32)
    for b in cores:
        m = dict(shared)
        m["x"] = np.ascontiguousarray(x[b])
        m.update(cst)
        maps.append(m)
    return maps


_CACHE = {}


def kernel(**inputs):
    cores = list(range(8))
    if "nc" not in _CACHE:
        _CACHE["nc"] = Prog().build_all()
    nc = _CACHE["nc"]
    maps = make_in_maps(inputs, cores)
    res = run_bass_kernel_spmd(nc, maps, core_ids=cores)
    out = np.stack([np.asarray(r["out"], dtype=np.float32) for r in res.results], axis=0)
    return out
```
